# Optimizing a Trainium2 kernel written in Bass

```python
import math
import jax
import jax.numpy as jnp
from jax import lax
import numpy as np

D_MODEL = 1024
BATCH = 8
SEQ = 4096
DEPTH = 2

CHUNK = 64
Q_BLOCK = 128
D_MIX = D_MODEL
D_FF = 2816
CONV_K = 4
EPS = 1e-6
ROPE_THETA = 10000.0

GDN_HEADS = 4
GDN_DK = 64
GDN_DV = 64
GDN_QK_W = GDN_HEADS * GDN_DK
GDN_W = GDN_HEADS * GDN_DV

DIFF_HEADS = 4
DIFF_DH = 32
DIFF_DV = 2 * DIFF_DH
DIFF_QK_W = 2 * DIFF_HEADS * DIFF_DH
DIFF_W = DIFF_HEADS * DIFF_DV

SSD_HEADS = 4
SSD_P = 64
SSD_N = 128
SSD_G = 2
SSD_W = SSD_HEADS * SSD_P

MLSTM_HEADS = 4
MLSTM_DH = 64
MLSTM_W = MLSTM_HEADS * MLSTM_DH

IN_SPLITS = (GDN_QK_W, GDN_QK_W, GDN_W, GDN_W, GDN_HEADS, GDN_HEADS,
             DIFF_QK_W, DIFF_QK_W, DIFF_W,
             SSD_W, SSD_W + 2 * SSD_G * SSD_N, SSD_HEADS,
             MLSTM_W, MLSTM_W, MLSTM_W, MLSTM_W, MLSTM_HEADS, MLSTM_HEADS)
IN_COLS = sum(IN_SPLITS)

kernel_name = 'hybrid_parallel_head_group_encoder'


def rmsnorm(x, g):
    xf = x.astype(jnp.float32)
    y = xf * lax.rsqrt(jnp.mean(xf * xf, axis=-1, keepdims=True) + EPS)
    return (y * g.astype(jnp.float32)).astype(x.dtype)


def l2norm(t):
    return t * lax.rsqrt(jnp.sum(t * t, axis=-1, keepdims=True) + EPS)


def swiglu(h, w_gate, w_up, w_down):
    return (jax.nn.silu(h @ w_gate) * (h @ w_up)) @ w_down


def causal_dwconv(u, w):
    ch = u.shape[-1]
    return lax.conv_general_dilated(u, w.astype(u.dtype)[:, None, :], window_strides=(1,),
                                    padding=[(CONV_K - 1, 0)],
                                    dimension_numbers=('NWC', 'WIO', 'NWC'),
                                    feature_group_count=ch)


def causal_tril():
    return jnp.tril(jnp.ones((CHUNK, CHUNK), dtype=bool))


def masked_decay(cum):
    diff = cum[..., :, None] - cum[..., None, :]
    return jnp.exp(jnp.where(causal_tril(), diff, -jnp.inf))


def heads_to_chunks(t):
    b, s = t.shape[:2]
    t = t.reshape((b, s // CHUNK, CHUNK) + t.shape[2:])
    perm = (1, 0, 3, 2) + tuple(range(4, t.ndim))
    return t.transpose(perm)


def chunks_to_heads(t):
    n, b, h, c, d = t.shape
    return t.transpose(1, 0, 3, 2, 4).reshape(b, n * c, h, d)


def rope_tables(seq, dim):
    inv_freq = ROPE_THETA ** (-jnp.arange(0, dim, 2, dtype=jnp.float32) / dim)
    ang = jnp.arange(seq, dtype=jnp.float32)[:, None] * inv_freq[None, :]
    return jnp.cos(ang), jnp.sin(ang)


def apply_rope(t, cos, sin):
    half = t.shape[-1] // 2
    t1, t2 = t[..., :half], t[..., half:]
    c, s = cos[None, :, None, :], sin[None, :, None, :]
    return jnp.concatenate([t1 * c - t2 * s, t2 * c + t1 * s], axis=-1)


def gated_deltanet(q, k, v, gate, beta_raw, a_raw, conv_w, a_log, dt_bias, norm_g):
    bsz, seq, _ = q.shape
    qkv = jax.nn.silu(causal_dwconv(jnp.concatenate([q, k, v], axis=-1), conv_w))
    q, k, v = jnp.split(qkv, [GDN_QK_W, 2 * GDN_QK_W], axis=-1)
    q = l2norm(q.reshape(bsz, seq, GDN_HEADS, GDN_DK)) * (GDN_DK ** -0.5)
    k = l2norm(k.reshape(bsz, seq, GDN_HEADS, GDN_DK))
    v = v.reshape(bsz, seq, GDN_HEADS, GDN_DV)
    beta = jax.nn.sigmoid(beta_raw)
    g = -jnp.exp(a_log) * jax.nn.softplus(a_raw + dt_bias)
    qc, kc, vc = heads_to_chunks(q), heads_to_chunks(k), heads_to_chunks(v)
    gc = jnp.cumsum(heads_to_chunks(g), axis=-1)
    bc = heads_to_chunks(beta)[..., None]
    decay = masked_decay(gc)
    eye = jnp.eye(CHUNK, dtype=jnp.float32)
    kb = kc * bc
    lower = jnp.einsum('nbhcd,nbhmd->nbhcm', kb, kc) * decay * (1.0 - eye)
    t_mat = lax.linalg.triangular_solve(eye + lower, jnp.broadcast_to(eye, lower.shape),
                                        left_side=True, lower=True, unit_diagonal=True)
    u = t_mat @ (vc * bc)
    w = t_mat @ (kb * jnp.exp(gc)[..., None])
    attn = jnp.einsum('nbhcd,nbhmd->nbhcm', qc, kc) * decay
    q_dec = qc * jnp.exp(gc)[..., None]
    k_dec = kc * jnp.exp(gc[..., -1:] - gc)[..., None]
    g_last = jnp.exp(gc[..., -1])

    def step(state, inp):
        q_i, k_i, u_i, w_i, a_i, gl_i = inp
        v_new = u_i - w_i @ state
        o_i = q_i @ state + a_i @ v_new
        state = state * gl_i[..., None, None] + jnp.einsum('bhcd,bhce->bhde', k_i, v_new)
        return state, o_i

    s0 = jnp.zeros((bsz, GDN_HEADS, GDN_DK, GDN_DV), jnp.float32)
    _, o = lax.scan(step, s0, (q_dec, k_dec, u, w, attn, g_last))
    o = chunks_to_heads(o)
    o = rmsnorm(o, norm_g) * jax.nn.silu(gate.reshape(bsz, seq, GDN_HEADS, GDN_DV))
    return o.reshape(bsz, seq, GDN_W)


def diff_attention(q, k, v, lam_q1, lam_k1, lam_q2, lam_k2, norm_g, lambda_init, cos, sin):
    bsz, seq, _ = q.shape
    q = apply_rope(q.reshape(bsz, seq, 2 * DIFF_HEADS, DIFF_DH), cos, sin)
    k = apply_rope(k.reshape(bsz, seq, 2 * DIFF_HEADS, DIFF_DH), cos, sin)
    v = v.reshape(bsz, seq, DIFF_HEADS, DIFF_DV)
    lam = (jnp.exp(jnp.sum(lam_q1 * lam_k1)) - jnp.exp(jnp.sum(lam_q2 * lam_k2))
           + lambda_init).astype(jnp.float32)
    nb = seq // Q_BLOCK
    qb = q.reshape(bsz, nb, Q_BLOCK, 2 * DIFF_HEADS, DIFF_DH).transpose(1, 0, 3, 2, 4) * (DIFF_DH ** -0.5)
    kt = k.transpose(0, 2, 1, 3)
    vt = v.transpose(0, 2, 1, 3)
    key_chunk = jnp.arange(seq) // CHUNK

    def block(args):
        q_blk, blk = args
        scores = jnp.einsum('bhqd,bhkd->bhqk', q_blk, kt)
        q_chunk = (blk * Q_BLOCK + jnp.arange(Q_BLOCK)) // CHUNK
        allowed = key_chunk[None, :] <= q_chunk[:, None]
        p = jax.nn.softmax(jnp.where(allowed, scores, -jnp.inf), axis=-1)
        p = p.reshape(bsz, DIFF_HEADS, 2, Q_BLOCK, seq)
        return jnp.einsum('bhqk,bhkd->bhqd', p[:, :, 0] - lam * p[:, :, 1], vt)

    o = lax.map(block, (qb, jnp.arange(nb)))
    o = o.transpose(1, 0, 3, 2, 4).reshape(bsz, seq, DIFF_HEADS, DIFF_DV)
    o = rmsnorm(o, norm_g) * (1.0 - lambda_init)
    return o.reshape(bsz, seq, DIFF_W)


def ssd_mixer(z, xbc, dt_raw, conv_w, conv_b, a_log, dt_bias, d_skip, norm_g):
    bsz, seq, _ = z.shape
    xbc = jax.nn.silu(causal_dwconv(xbc, conv_w) + conv_b)
    xs, bm, cm = jnp.split(xbc, [SSD_W, SSD_W + SSD_G * SSD_N], axis=-1)
    x = xs.reshape(bsz, seq, SSD_HEADS, SSD_P)
    rep = SSD_HEADS // SSD_G
    bm = jnp.repeat(bm.reshape(bsz, seq, SSD_G, SSD_N), rep, axis=2)
    cm = jnp.repeat(cm.reshape(bsz, seq, SSD_G, SSD_N), rep, axis=2)
    dt = jax.nn.softplus(dt_raw + dt_bias)
    da = dt * (-jnp.exp(a_log))
    nch = seq // CHUNK
    xc = (x * dt[..., None]).reshape(bsz, nch, CHUNK, SSD_HEADS, SSD_P)
    bc = bm.reshape(bsz, nch, CHUNK, SSD_HEADS, SSD_N)
    cc = cm.reshape(bsz, nch, CHUNK, SSD_HEADS, SSD_N)
    acs = jnp.cumsum(da.reshape(bsz, nch, CHUNK, SSD_HEADS).transpose(0, 3, 1, 2), axis=-1)
    l_mat = masked_decay(acs)
    y_diag = jnp.einsum('bclhn,bcmhn,bhclm,bcmhp->bclhp', cc, bc, l_mat, xc)
    decay_states = jnp.exp(acs[..., -1:] - acs)
    states = jnp.einsum('bclhn,bhcl,bclhp->bchpn', bc, decay_states, xc)

    def step(st, inp):
        s_c, d_c = inp
        return st * d_c[..., None, None] + s_c, st

    s0 = jnp.zeros((bsz, SSD_HEADS, SSD_P, SSD_N), jnp.float32)
    _, prev = lax.scan(step, s0, (states.transpose(1, 0, 2, 3, 4),
                                  jnp.exp(acs[..., -1]).transpose(2, 0, 1)))
    prev = prev.transpose(1, 0, 2, 3, 4)
    y_off = jnp.einsum('bclhn,bchpn,bhcl->bclhp', cc, prev, jnp.exp(acs))
    y = (y_diag + y_off).reshape(bsz, seq, SSD_HEADS, SSD_P) + x * d_skip[:, None]
    y = y.reshape(bsz, seq, SSD_W) * jax.nn.silu(z)
    gw = SSD_W // SSD_G
    y = rmsnorm(y.reshape(bsz, seq, SSD_G, gw), norm_g.reshape(SSD_G, gw))
    return y.reshape(bsz, seq, SSD_W)


def mlstm(q, k, v, o_raw, i_raw, f_raw, i_bias, f_bias, norm_g):
    bsz, seq, _ = q.shape
    shp = (bsz, seq, MLSTM_HEADS, MLSTM_DH)
    qc = heads_to_chunks(q.reshape(shp))
    kc = heads_to_chunks(k.reshape(shp) * (MLSTM_DH ** -0.5))
    vc = heads_to_chunks(v.reshape(shp))
    li = heads_to_chunks(i_raw + i_bias)
    lf = heads_to_chunks(jax.nn.log_sigmoid(f_raw + f_bias))
    b = jnp.cumsum(lf, axis=-1)
    d_log = jnp.where(causal_tril(), b[..., :, None] - b[..., None, :] + li[..., None, :], -jnp.inf)
    d_max = jnp.max(d_log, axis=-1)
    qk = jnp.einsum('nbhcd,nbhmd->nbhcm', qc, kc)
    g_end = b[..., -1:] - b + li

    def step(carry, inp):
        c_st, n_st, m_st = carry
        q_i, k_i, v_i, b_i, dl_i, dm_i, qk_i, ge_i = inp
        m_t = jnp.maximum(b_i + m_st[..., None], dm_i)
        w_st = jnp.exp(b_i + m_st[..., None] - m_t)
        s_i = qk_i * jnp.exp(dl_i - m_t[..., None])
        num = w_st[..., None] * (q_i @ c_st) + s_i @ v_i
        den = w_st * jnp.einsum('bhcd,bhd->bhc', q_i, n_st) + jnp.sum(s_i, axis=-1)
        h_i = num / jnp.maximum(jnp.abs(den), jnp.exp(-m_t))[..., None]
        m_new = jnp.maximum(b_i[..., -1] + m_st, jnp.max(ge_i, axis=-1))
        w_old = jnp.exp(b_i[..., -1] + m_st - m_new)
        k_w = k_i * jnp.exp(ge_i - m_new[..., None])[..., None]
        c_st = c_st * w_old[..., None, None] + jnp.einsum('bhcd,bhce->bhde', k_w, v_i)
        n_st = n_st * w_old[..., None] + jnp.sum(k_w, axis=-2)
        return (c_st, n_st, m_new), h_i

    init = (jnp.zeros((bsz, MLSTM_HEADS, MLSTM_DH, MLSTM_DH), jnp.float32),
            jnp.zeros((bsz, MLSTM_HEADS, MLSTM_DH), jnp.float32),
            jnp.zeros((bsz, MLSTM_HEADS), jnp.float32))
    _, h = lax.scan(step, init, (qc, kc, vc, b, d_log, d_max, qk, g_end))
    h = chunks_to_heads(h) * jax.nn.sigmoid(o_raw.reshape(shp))
    h = rmsnorm(h, norm_g.reshape(MLSTM_HEADS, MLSTM_DH))
    return h.reshape(bsz, seq, MLSTM_W)


def hybrid_mixer(h, w_in, w_out, gdn_conv_w, gdn_a_log, gdn_dt_bias, gdn_norm_g,
                 diff_lam_q1, diff_lam_k1, diff_lam_q2, diff_lam_k2, diff_norm_g,
                 ssd_conv_w, ssd_conv_b, ssd_a_log, ssd_dt_bias, ssd_d, ssd_norm_g,
                 mlstm_i_bias, mlstm_f_bias, mlstm_norm_g, lambda_init, cos, sin):
    proj = (h @ w_in).astype(jnp.float32)
    points = np.cumsum(np.array(IN_SPLITS))[:-1].tolist()
    (a_q, a_k, a_v, a_g, a_beta, a_a,
     b_q, b_k, b_v,
     c_z, c_xbc, c_dt,
     d_q, d_k, d_v, d_o, d_i, d_f) = jnp.split(proj, points, axis=-1)
    out_a = gated_deltanet(a_q, a_k, a_v, a_g, a_beta, a_a, gdn_conv_w, gdn_a_log, gdn_dt_bias, gdn_norm_g)
    out_b = diff_attention(b_q, b_k, b_v, diff_lam_q1, diff_lam_k1, diff_lam_q2, diff_lam_k2,
                           diff_norm_g, lambda_init, cos, sin)
    out_c = ssd_mixer(c_z, c_xbc, c_dt, ssd_conv_w, ssd_conv_b, ssd_a_log, ssd_dt_bias, ssd_d, ssd_norm_g)
    out_d = mlstm(d_q, d_k, d_v, d_o, d_i, d_f, mlstm_i_bias, mlstm_f_bias, mlstm_norm_g)
    mixed = jnp.concatenate([out_a, out_b, out_c, out_d], axis=-1)
    return mixed.astype(h.dtype) @ w_out


def setup_inputs(seed: int = 0) -> dict:
    key = jax.random.key(seed)
    keys = list(jax.random.split(key, 48))
    f32 = jnp.float32
    L = DEPTH

    def nxt():
        return keys.pop()

    def normal(shape, scale):
        return jax.random.normal(nxt(), shape, f32) * scale

    def gain(n):
        return 1.0 + normal((L, n), 0.02)

    def a_log_init(n):
        return jnp.log(jax.random.uniform(nxt(), (L, n), f32, 1.0, 16.0))

    def dt_bias_init(n):
        dt = jnp.exp(jax.random.uniform(nxt(), (L, n), f32, math.log(1e-3), math.log(1e-1)))
        return dt + jnp.log(-jnp.expm1(-dt))

    xbc_w = SSD_W + 2 * SSD_G * SSD_N
    return {
        'x': normal((BATCH, SEQ, D_MODEL), 1.0),
        'ffn1_pre_g': gain(D_MODEL),
        'ffn1_w_gate': normal((L, D_MODEL, D_FF), D_MODEL ** -0.5),
        'ffn1_w_up': normal((L, D_MODEL, D_FF), D_MODEL ** -0.5),
        'ffn1_w_down': normal((L, D_FF, D_MODEL), D_FF ** -0.5),
        'ffn1_post_g': gain(D_MODEL),
        'mix_pre_g': gain(D_MODEL),
        'w_in': normal((L, D_MODEL, IN_COLS), D_MODEL ** -0.5),
        'gdn_conv_w': normal((L, CONV_K, 2 * GDN_QK_W + GDN_W), CONV_K ** -0.5),
        'gdn_a_log': a_log_init(GDN_HEADS),
        'gdn_dt_bias': dt_bias_init(GDN_HEADS),
        'gdn_norm_g': gain(GDN_DV),
        'diff_lam_q1': normal((L, DIFF_DH), 0.1),
        'diff_lam_k1': normal((L, DIFF_DH), 0.1),
        'diff_lam_q2': normal((L, DIFF_DH), 0.1),
        'diff_lam_k2': normal((L, DIFF_DH), 0.1),
        'diff_norm_g': gain(DIFF_DV),
        'ssd_conv_w': normal((L, CONV_K, xbc_w), CONV_K ** -0.5),
        'ssd_conv_b': normal((L, xbc_w), 0.02),
        'ssd_a_log': a_log_init(SSD_HEADS),
        'ssd_dt_bias': dt_bias_init(SSD_HEADS),
        'ssd_d': 1.0 + normal((L, SSD_HEADS), 0.1),
        'ssd_norm_g': gain(SSD_W),
        'mlstm_i_bias': normal((L, MLSTM_HEADS), 0.1),
        'mlstm_f_bias': jnp.linspace(3.0, 6.0, MLSTM_HEADS, dtype=f32)[None, :] + normal((L, MLSTM_HEADS), 0.1),
        'mlstm_norm_g': gain(MLSTM_W),
        'w_out': normal((L, D_MIX, D_MODEL), D_MIX ** -0.5),
        'mix_post_g': gain(D_MODEL),
        'ffn2_pre_g': gain(D_MODEL),
        'ffn2_w_gate': normal((L, D_MODEL, D_FF), D_MODEL ** -0.5),
        'ffn2_w_up': normal((L, D_MODEL, D_FF), D_MODEL ** -0.5),
        'ffn2_w_down': normal((L, D_FF, D_MODEL), D_FF ** -0.5),
        'ffn2_post_g': gain(D_MODEL),
    }


def reference(x, ffn1_pre_g, ffn1_w_gate, ffn1_w_up, ffn1_w_down, ffn1_post_g,
              mix_pre_g, w_in,
              gdn_conv_w, gdn_a_log, gdn_dt_bias, gdn_norm_g,
              diff_lam_q1, diff_lam_k1, diff_lam_q2, diff_lam_k2, diff_norm_g,
              ssd_conv_w, ssd_conv_b, ssd_a_log, ssd_dt_bias, ssd_d, ssd_norm_g,
              mlstm_i_bias, mlstm_f_bias, mlstm_norm_g,
              w_out, mix_post_g,
              ffn2_pre_g, ffn2_w_gate, ffn2_w_up, ffn2_w_down, ffn2_post_g):
    cos, sin = rope_tables(x.shape[1], DIFF_DH)
    for l in range(DEPTH):
        lambda_init = 0.8 - 0.6 * math.exp(-0.3 * l)
        h = rmsnorm(x, ffn1_pre_g[l])
        x = x + 0.5 * rmsnorm(swiglu(h, ffn1_w_gate[l], ffn1_w_up[l], ffn1_w_down[l]), ffn1_post_g[l])
        h = rmsnorm(x, mix_pre_g[l])
        m = hybrid_mixer(h, w_in[l], w_out[l],
                         gdn_conv_w[l], gdn_a_log[l], gdn_dt_bias[l], gdn_norm_g[l],
                         diff_lam_q1[l], diff_lam_k1[l], diff_lam_q2[l], diff_lam_k2[l], diff_norm_g[l],
                         ssd_conv_w[l], ssd_conv_b[l], ssd_a_log[l], ssd_dt_bias[l], ssd_d[l], ssd_norm_g[l],
                         mlstm_i_bias[l], mlstm_f_bias[l], mlstm_norm_g[l],
                         lambda_init, cos, sin)
        x = x + rmsnorm(m, mix_post_g[l])
        h = rmsnorm(x, ffn2_pre_g[l])
        x = x + 0.5 * rmsnorm(swiglu(h, ffn2_w_gate[l], ffn2_w_up[l], ffn2_w_down[l]), ffn2_post_g[l])
    return x
```

```python
import math
from contextlib import ExitStack

import numpy as np
import ml_dtypes
import concourse.bass as bass
import concourse.mybir as mybir
from concourse.bass_utils import run_bass_kernel_spmd

F32 = mybir.dt.float32
BF16 = mybir.dt.bfloat16
AF = mybir.ActivationFunctionType
ALU = mybir.AluOpType
AX = mybir.AxisListType

S_LEN = 4096
D = 1024
DFF = 2816
INC = 3860
NB = S_LEN // 128
EPS = 1e-6
NEG = -30000.0


class Res:
    __slots__ = ("lw", "rd", "excl", "pe")

    def __init__(self):
        self.lw = None
        self.rd = []
        self.excl = False
        self.pe = None


class V:
    __slots__ = ("ap", "r")

    def __init__(self, ap, r=None):
        self.ap = ap
        self.r = r if r is not None else Res()

    def __getitem__(self, k):
        return V(self.ap[k], self.r)

    def re(self, pat, **kw):
        return V(self.ap.rearrange(pat, **kw), self.r)

    def bc(self, axis, shape):
        return V(self.ap.unsqueeze(axis).broadcast_to(list(shape)), self.r)


class Sched:
    ENG = ("tensor", "vector", "scalar", "gpsimd", "sync")
    NSLOT = 8

    def __init__(self, nc, es):
        self.nc = nc
        self.eng = {e: getattr(nc, e) for e in self.ENG}
        self.sem = {e: es.enter_context(nc.semaphore("s_" + e)) for e in self.ENG}
        self.cnt = {e: 0 for e in self.ENG}
        self.known = {e: {} for e in self.ENG}
        self.semobj = {}
        self.maxv = {}
        self.dq = {}
        for q in ("sync", "gpsimd", "scalar"):
            self.dq[q] = dict(
                sems=[es.enter_context(nc.semaphore("d_%s%d" % (q, i))) for i in range(self.NSLOT)], n=0)

    def _wait(self, e, tok):
        sem, val = tok
        k = self.known[e]
        if k.get(id(sem), 0) >= val:
            return
        k[id(sem)] = val
        self.eng[e].wait_ge(sem, val)

    def _deps(self, e, reads, writes):
        mysem = self.sem.get(e)
        for r in reads:
            if r.lw is not None:
                if not (e == "tensor" and r.lw[0] is mysem):
                    self._wait(e, r.lw)
        for w in writes:
            if w.lw is not None:
                if not (e == "tensor" and w.lw[0] is mysem):
                    self._wait(e, w.lw)
            for t in w.rd:
                if not (e == "tensor" and t[0] is mysem):
                    self._wait(e, t)

    def _commit(self, tok, reads, writes):
        self.semobj[id(tok[0])] = tok[0]
        self.maxv[id(tok[0])] = max(self.maxv.get(id(tok[0]), 0), tok[1])
        for r in reads:
            r.rd.append(tok)
            if len(r.rd) > 12:
                best = {}
                for t in r.rd:
                    if t[1] > best.get(id(t[0]), (None, -1))[1]:
                        best[id(t[0])] = t
                r.rd = list(best.values())
        for w in writes:
            w.lw = tok
            w.rd = []

    def op(self, e, fn, reads=(), writes=(), force=()):
        writes = list(writes) + [r for r in reads if r.excl]
        reads = [r for r in reads if not r.excl]
        for t in force:
            self._wait(e, t)
        self._deps(e, reads, writes)
        ins = fn(self.eng[e])
        self.cnt[e] += 1
        ins.then_inc(self.sem[e], 1)
        tok = (self.sem[e], self.cnt[e])
        self._commit(tok, reads, writes)
        return tok

    def dma(self, q, out, in_, reads=(), writes=(), **kw):
        d = self.dq[q]
        n = d["n"]
        sem = d["sems"][n % self.NSLOT]
        if n >= self.NSLOT:
            self._wait(q, (sem, 16 * (n // self.NSLOT)))
        self._deps(q, reads, writes)
        ins = self.eng[q].dma_start(out=out, in_=in_, **kw)
        ins.then_inc(sem, 16)
        d["n"] = n + 1
        tok = (sem, 16 * (n // self.NSLOT + 1))
        self._commit(tok, reads, writes)
        return tok

    def barrier(self, skip_queues=("gpsimd",)):
        skip = set()
        for q in skip_queues:
            for s in self.dq[q]["sems"]:
                skip.add(id(s))
        for e in self.ENG:
            for sid, v in self.maxv.items():
                if sid in skip:
                    continue
                self._wait(e, (self.semobj[sid], v))

    def wait_res(self, e, ress):
        for r in ress:
            if r.lw is not None:
                self._wait(e, r.lw)
            for t in r.rd:
                self._wait(e, t)


class KB:
    def __init__(self, nc, es):
        self.nc = nc
        self.S = Sched(nc, es)
        self.ncnt = 0

    def _nm(self, n):
        self.ncnt += 1
        return "%s_%d" % (n, self.ncnt)

    def sb(self, es, name, shape, dt):
        t = es.enter_context(self.nc.sbuf_tensor(self._nm(name), list(shape), dt))
        return V(t[:])

    def ps(self, es, name, shape, dt):
        t = es.enter_context(self.nc.psum_tensor(self._nm(name), list(shape), dt))
        v = V(t[:])
        v.r.excl = True
        return v

    def dram(self, name, shape, dt, kind=None):
        if kind is None:
            t = self.nc.dram_tensor(name, list(shape), dt)
        else:
            t = self.nc.dram_tensor(name, list(shape), dt, kind=kind)
        return V(t.ap())

    def mm(self, out, lhsT, rhs, start=True, stop=True, skip=False):
        def _v(x):
            return x() if callable(x) else x
        rb = (_v(lhsT.ap.base_partition), _v(lhsT.ap.partition_size))
        prev = out.r.pe
        force = [prev[1]] if (prev is not None and prev[0] != rb) else []
        if skip:
            tok = self.S.op("tensor", lambda e: e.matmul(out.ap, lhsT=lhsT.ap, rhs=rhs.ap, start=start, stop=stop, skip_group_check=True),
                            [lhsT.r, rhs.r], [out.r], force=force)
        else:
            tok = self.S.op("tensor", lambda e: e.matmul(out.ap, lhsT=lhsT.ap, rhs=rhs.ap, start=start, stop=stop),
                            [lhsT.r, rhs.r], [out.r], force=force)
        out.r.pe = (rb, tok)

    def tp(self, out, in_, ident):
        def _v(x):
            return x() if callable(x) else x
        rb = (_v(in_.ap.base_partition), _v(in_.ap.partition_size))
        prev = out.r.pe
        force = [prev[1]] if (prev is not None and prev[0] != rb) else []
        tok = self.S.op("tensor", lambda e: e.transpose(out=out.ap, in_=in_.ap, identity=ident.ap),
                        [in_.r, ident.r], [out.r], force=force)
        out.r.pe = (rb, tok)

    def act(self, out, in_, func, scale=1.0, bias=None, accum=None):
        rd = [in_.r]
        wr = [out.r]
        kw = {}
        if bias is not None:
            if isinstance(bias, V):
                kw["bias"] = bias.ap
                rd.append(bias.r)
            else:
                kw["bias"] = bias
        if isinstance(scale, V):
            rd.append(scale.r)
            kw["scale"] = scale.ap
        else:
            kw["scale"] = scale
        if accum is not None:
            kw["accum_out"] = accum.ap
            wr.append(accum.r)
        self.S.op("scalar", lambda e: e.activation(out=out.ap, in_=in_.ap, func=func, **kw), rd, wr)

    def tt(self, eng, out, a, b, op):
        self.S.op(eng, lambda e: e.tensor_tensor(out=out.ap, in0=a.ap, in1=b.ap, op=op), [a.r, b.r], [out.r])

    def ts(self, eng, out, a, s1, s2=None, op0=ALU.mult, op1=None):
        rd = [a.r]
        s1a = s1
        if isinstance(s1, V):
            rd.append(s1.r)
            s1a = s1.ap
        s2a = s2
        if isinstance(s2, V):
            rd.append(s2.r)
            s2a = s2.ap
        if op1 is None:
            self.S.op(eng, lambda e: e.tensor_scalar(out=out.ap, in0=a.ap, scalar1=s1a, scalar2=None, op0=op0), rd, [out.r])
        else:
            self.S.op(eng, lambda e: e.tensor_scalar(out=out.ap, in0=a.ap, scalar1=s1a, scalar2=s2a, op0=op0, op1=op1), rd, [out.r])

    def stt(self, out, a, scalar, b, op0, op1):
        rd = [a.r, b.r]
        sa = scalar
        if isinstance(scalar, V):
            rd.append(scalar.r)
            sa = scalar.ap
        self.S.op("vector", lambda e: e.scalar_tensor_tensor(out=out.ap, in0=a.ap, scalar=sa, in1=b.ap, op0=op0, op1=op1), rd, [out.r])

    def cp(self, eng, out, in_):
        if eng == "scalar":
            self.S.op("scalar", lambda e: e.copy(out=out.ap, in_=in_.ap), [in_.r], [out.r])
        else:
            self.S.op(eng, lambda e: e.tensor_copy(out=out.ap, in_=in_.ap), [in_.r], [out.r])

    def red(self, out, in_, op=ALU.add):
        self.S.op("vector", lambda e: e.tensor_reduce(out=out.ap, in_=in_.ap, axis=AX.X, op=op), [in_.r], [out.r])

    def recip(self, out, in_):
        self.S.op("vector", lambda e: e.reciprocal(out=out.ap, in_=in_.ap), [in_.r], [out.r])

    def memset(self, eng, out, val):
        self.S.op(eng, lambda e: e.memset(out.ap, val), [], [out.r])

    def dma(self, q, out, in_, **kw):
        self.S.dma(q, out.ap, in_.ap, reads=[in_.r], writes=[out.r], **kw)

    def rstd(self, out, ss, inv_n, epsv, tmp):
        self.act(tmp, ss, AF.Ln, scale=inv_n, bias=epsv)
        self.act(out, tmp, AF.Exp, scale=-0.5)


def host_consts():
    c = {}
    idx = np.arange(128)
    same = (idx[:, None] // 64) == (idx[None, :] // 64)
    c["c_ident"] = np.eye(128, dtype=np.float32)
    c["c_tri"] = (same & (idx[:, None] <= idx[None, :])).astype(np.float32)
    c["c_su"] = (same & (idx[:, None] > idx[None, :])).astype(np.float32)
    c["c_onesbd"] = same.astype(np.float32)
    mbt = np.where(same & (idx[:, None] <= idx[None, :]), 0.0, NEG).astype(np.float32)
    mbs = np.where(same & (idx[None, :] < idx[:, None]), 0.0, NEG).astype(np.float32)
    c["c_mbt4"] = np.tile(mbt, (1, 4))
    c["c_mbs4"] = np.tile(mbs, (1, 4))
    c["c_ident4"] = np.tile(np.eye(128, dtype=np.float32), (1, 4))
    c["c_bo"] = same.astype(np.float32)
    chi = np.zeros((128, 2), np.float32)
    chi[:64, 0] = 1
    chi[64:, 1] = 1
    c["c_chi"] = chi
    chj = np.zeros((2, 128, 128), np.float32)
    chj[0, :64, :] = 1
    chj[1, 64:, :] = 1
    c["c_chj"] = chj
    p = np.arange(64)
    dloc = p % 32
    perm = np.where(dloc < 16, p + 16, p - 16)
    prot = np.zeros((64, 64), np.float32)
    prot[perm, p] = 1.0
    c["c_prot"] = prot
    inv_freq = (10000.0 ** (-(np.arange(0, 32, 2, dtype=np.float32)) / np.float32(32))).astype(np.float32)
    ang = (np.arange(S_LEN, dtype=np.float32)[:, None] * inv_freq[None, :]).astype(np.float32)
    cos = np.cos(ang).astype(np.float32)
    sin = np.sin(ang).astype(np.float32)
    cosT = np.zeros((64, S_LEN), np.float32)
    sinT = np.zeros((64, S_LEN), np.float32)
    for pp in range(64):
        dl = pp % 32
        cosT[pp] = cos[:, dl % 16]
        sinT[pp] = -sin[:, dl] if dl < 16 else sin[:, dl - 16]
    c["c_cos"] = cosT
    c["c_sin"] = sinT
    return c


W_SPECS = [
    ("ffn1_pre_g", [2, 1024]), ("ffn1_w_gate", [2, 1024, 2816]), ("ffn1_w_up", [2, 1024, 2816]),
    ("ffn1_w_down", [2, 2816, 1024]), ("ffn1_post_g", [2, 1024]), ("mix_pre_g", [2, 1024]),
    ("w_in", [2, 1024, 3860]), ("gdn_conv_w", [2, 4, 768]), ("gdn_a_log", [2, 4]), ("gdn_dt_bias", [2, 4]),
    ("gdn_norm_g", [2, 64]), ("diff_lam_q1", [2, 32]), ("diff_lam_k1", [2, 32]), ("diff_lam_q2", [2, 32]),
    ("diff_lam_k2", [2, 32]), ("diff_norm_g", [2, 64]), ("ssd_conv_w", [2, 4, 768]), ("ssd_conv_b", [2, 768]),
    ("ssd_a_log", [2, 4]), ("ssd_dt_bias", [2, 4]), ("ssd_d", [2, 4]), ("ssd_norm_g", [2, 256]),
    ("mlstm_i_bias", [2, 4]), ("mlstm_f_bias", [2, 4]), ("mlstm_norm_g", [2, 256]), ("w_out", [2, 1024, 1024]),
    ("mix_post_g", [2, 1024]), ("ffn2_pre_g", [2, 1024]), ("ffn2_w_gate", [2, 1024, 2816]),
    ("ffn2_w_up", [2, 1024, 2816]), ("ffn2_w_down", [2, 2816, 1024]), ("ffn2_post_g", [2, 1024]),
]


def build(nlayers=2, stages=("ffn1", "mixa", "mixb", "ffn2"), dbg=False, nblk=NB, upto=99):
    nc = bass.Bass("TRN2", target_bir_lowering=False)
    es0 = ExitStack()
    kb = KB(nc, es0)
    S = kb.S
    x_in = kb.dram("x", [S_LEN, D], F32, "ExternalInput")
    out = kb.dram("out", [S_LEN, D], F32, "ExternalOutput")
    W = {n: kb.dram(n, s, F32, "ExternalInput") for n, s in W_SPECS}
    hc = host_consts()
    C = {n: kb.dram(n, list(a.shape), F32, "ExternalInput") for n, a in hc.items()}
    mixA = kb.dram("mixA", [S_LEN, 768], BF16)
    dbg_mixed = kb.dram("dbg_mixed", [S_LEN, D], F32, "ExternalOutput") if dbg else None

    Wb = {}
    for l in range(nlayers):
        for f in (1, 2):
            for nm, shp in (("w_gate", [D, DFF]), ("w_up", [D, DFF]), ("w_down", [DFF, D])):
                Wb[(l, "ffn%d_%s" % (f, nm))] = kb.dram("wb_%d_ffn%d_%s" % (l, f, nm), shp, BF16)
        Wb[(l, "w_in")] = kb.dram("wb_%d_w_in" % l, [D, INC], BF16)
        Wb[(l, "w_out")] = kb.dram("wb_%d_w_out" % l, [D, D], BF16)

    def cast_weight(l, name):
        src = W[name]
        dst = Wb[(l, name)]
        rows = src.ap.shape[1]
        for r0 in range(0, rows, 256):
            r1 = min(rows, r0 + 256)
            kb.dma("gpsimd", dst[r0:r1, :], V(src.ap[l], src.r)[r0:r1, :])

    for l in range(nlayers):
        for name in ("ffn1_w_gate", "ffn1_w_up", "ffn1_w_down", "w_in", "w_out", "ffn2_w_gate", "ffn2_w_up", "ffn2_w_down"):
            cast_weight(l, name)

    cs = {}
    for n in ("c_ident", "c_tri", "c_su", "c_onesbd", "c_bo"):
        cs[n] = kb.sb(es0, n, [128, 128], F32)
        kb.dma("sync", cs[n], C[n])
    for n in ("c_mbt4", "c_mbs4", "c_ident4"):
        cs[n] = kb.sb(es0, n, [128, 512], F32)
        kb.dma("sync", cs[n], C[n])
    cs["c_chi"] = kb.sb(es0, "c_chi", [128, 2], F32)
    kb.dma("sync", cs["c_chi"], C["c_chi"])
    cs["c_chj"] = kb.sb(es0, "c_chj", [128, 2, 128], F32)
    kb.dma("sync", cs["c_chj"], C["c_chj"].re("j k c -> k j c"))
    identb = kb.sb(es0, "identb", [128, 128], BF16)
    kb.cp("vector", identb, cs["c_ident"])
    protf = kb.sb(es0, "protf", [64, 64], F32)
    kb.dma("sync", protf, C["c_prot"])
    protb = kb.sb(es0, "protb", [64, 64], BF16)
    kb.cp("vector", protb, protf)
    epsv = kb.sb(es0, "epsv", [128, 1], F32)
    kb.memset("vector", epsv, EPS)
    ident = cs["c_ident"]

    def bload(es, name, src_ap_v, n, scale=None):
        t = kb.sb(es, name, [128, n], F32)
        kb.S.dma("sync", t.ap, src_ap_v.ap.partition_broadcast(128), reads=[src_ap_v.r], writes=[t.r])
        if scale is not None:
            kb.ts("vector", t, t, scale, None, ALU.mult)
        return t

    def wl(name, l):
        w = W[name]
        return V(w.ap[l], w.r)

    def prenorm_T(xt, g1b, xn, hT_dst, pT, ss, rs, tmp1, junk):
        kb.memset("gpsimd", ss, 0.0)
        kb.act(junk, xt, AF.Square, accum=ss)
        kb.rstd(rs, ss, 1.0 / D, epsv, tmp1)
        kb.stt(xn, xt, rs[:, 0:1], g1b, ALU.mult, ALU.mult)
        for k in range(8):
            kb.tp(pT[:, k * 128:(k + 1) * 128], xn[:, k * 128:(k + 1) * 128], identb)
        kb.cp("scalar", hT_dst, pT.re("p (k n) -> p k n", k=8))

    def post_res(ya, yb, xt, g2b, tmp, q2, rs2, tmp1, junk):
        kb.memset("gpsimd", q2, 0.0)
        kb.act(junk[:, 0:512], ya, AF.Square, accum=q2[:, 0:1])
        kb.act(junk[:, 512:1024], yb, AF.Square, accum=q2[:, 1:2])
        kb.tt("vector", q2[:, 0:1], q2[:, 0:1], q2[:, 1:2], ALU.add)
        kb.rstd(rs2, q2[:, 0:1], 1.0 / D, epsv, tmp1)
        kb.tt("vector", tmp[:, 0:512], ya, g2b[:, 0:512], ALU.mult)
        kb.tt("vector", tmp[:, 512:1024], yb, g2b[:, 512:1024], ALU.mult)
        kb.stt(xt, tmp, rs2[:, 0:1], xt, ALU.mult, ALU.add)

    def ffn_phase(l, f, src, dst):
        pre = "ffn%d_" % f
        with ExitStack() as es:
            g1b = bload(es, "g1b", wl(pre + "pre_g", l), D)
            g2b = bload(es, "g2b", wl(pre + "post_g", l), D, scale=0.5)
            xt = [[kb.sb(es, "xt", [128, D], F32) for s in range(4)] for par in range(2)]
            xn = kb.sb(es, "xn", [128, D], BF16)
            junk = kb.sb(es, "junk", [128, D], BF16)
            tmp = kb.sb(es, "tmp", [128, D], F32)
            hT = kb.sb(es, "hT", [128, 8, 512], BF16)
            actb = kb.sb(es, "actb", [128, 22, 512], BF16)
            slabs = [kb.sb(es, "slab", [128, 11264], BF16) for i in range(4)]
            sg = [kb.sb(es, "sg", [128, 512], F32) for i in range(2)]
            ysb = [kb.sb(es, "ysb", [128, 512], F32) for i in range(4)]
            ss = kb.sb(es, "ss", [128, 1], F32)
            rs = kb.sb(es, "rs", [128, 1], F32)
            t1 = kb.sb(es, "t1", [128, 1], F32)
            q2 = kb.sb(es, "q2", [128, 2], F32)
            pT = [kb.ps(es, "pT", [128, 1024], BF16) for i in range(2)]
            pG = [kb.ps(es, "pG", [128, 512], F32) for i in range(2)]
            pU = [kb.ps(es, "pU", [128, 512], F32) for i in range(2)]
            pY = [kb.ps(es, "pY", [128, 512], F32) for i in range(2)]
            wg = Wb[(l, pre + "w_gate")].re("(k p) n -> p k n", p=128)
            wu = Wb[(l, pre + "w_up")].re("(k p) n -> p k n", p=128)
            wd = Wb[(l, pre + "w_down")].re("(f p) n -> p f n", p=128)
            nslab = [0]

            def load_slab(kind, half):
                sl = slabs[nslab[0] % 4]
                nslab[0] += 1
                if kind == "g":
                    v = sl.re("p (k n) -> p k n", k=8)
                    kb.dma("sync", v, wg[:, :, half * 1408:(half + 1) * 1408])
                elif kind == "u":
                    v = sl.re("p (k n) -> p k n", k=8)
                    kb.dma("sync", v, wu[:, :, half * 1408:(half + 1) * 1408])
                else:
                    v = sl.re("p (f n) -> p f n", f=22)
                    kb.dma("sync", v, wd[:, :, half * 512:(half + 1) * 512])
                return v

            def load_x(t):
                for s in range(4):
                    r0 = (t * 4 + s) * 128
                    kb.dma("scalar", xt[t % 2][s], src[r0:r0 + 128, :])

            load_x(0)
            NT = S_LEN // 512
            for t in range(NT):
                par = t % 2
                sg0 = load_slab("g", 0)
                su0 = load_slab("u", 0)
                for s in range(4):
                    prenorm_T(xt[par][s], g1b, xn, hT[:, :, s * 128:(s + 1) * 128], pT[s % 2], ss, rs, t1, junk)
                if t + 1 < NT:
                    load_x(t + 1)
                sg1 = load_slab("g", 1)
                su1 = load_slab("u", 1)
                for half, (sgw, suw) in enumerate(((sg0, su0), (sg1, su1))):
                    for fi in range(11):
                        fc = half * 11 + fi
                        pg = pG[fc % 2]
                        pu = pU[fc % 2]
                        for k in range(8):
                            kb.mm(pg, sgw[:, k, fi * 128:(fi + 1) * 128], hT[:, k, :], start=(k == 0), stop=(k == 7))
                        for k in range(8):
                            kb.mm(pu, suw[:, k, fi * 128:(fi + 1) * 128], hT[:, k, :], start=(k == 0), stop=(k == 7))
                        kb.act(sg[fc % 2], pg, AF.Silu)
                        kb.tt("vector", actb[:, fc, :], sg[fc % 2], pu, ALU.mult)
                    if half == 0:
                        sd0 = load_slab("d", 0)
                sd1 = load_slab("d", 1)
                for s in range(4):
                    py = pY[s % 2]
                    for fc in range(22):
                        kb.mm(py, actb[:, fc, s * 128:(s + 1) * 128], sd0[:, fc, :], start=(fc == 0), stop=(fc == 21))
                    kb.cp("scalar", ysb[s], py)
                for s in range(4):
                    py = pY[s % 2]
                    for fc in range(22):
                        kb.mm(py, actb[:, fc, s * 128:(s + 1) * 128], sd1[:, fc, :], start=(fc == 0), stop=(fc == 21))
                    post_res(ysb[s], py, xt[par][s], g2b, tmp, q2, rs, t1, junk)
                    r0 = (t * 4 + s) * 128
                    kb.dma("scalar", dst[r0:r0 + 128, :], xt[par][s])
            S.barrier()

    def mixa_phase(l, xs):
        with ExitStack() as es:
            sb = lambda n, shp, dt=F32: kb.sb(es, n, shp, dt)
            g1b = bload(es, "g1b", wl("mix_pre_g", l), D)
            winb = sb("winb", [128, 8, INC], BF16)
            wsrc = Wb[(l, "w_in")].re("(k p) n -> p k n", p=128)
            for k in range(8):
                kb.dma("sync", winb[:, k, :], wsrc[:, k, :])
            gcw = sb("gcw", [128, 6, 4])
            scw = sb("scw", [128, 6, 4])
            scb = sb("scb", [128, 6])
            for j in range(4):
                kb.dma("sync", gcw[:, :, j], wl("gdn_conv_w", l)[j].re("(c p) -> p c", p=128), allow_slow_non_contiguous=True)
                kb.dma("sync", scw[:, :, j], wl("ssd_conv_w", l)[j].re("(c p) -> p c", p=128), allow_slow_non_contiguous=True)
            kb.dma("sync", scb, wl("ssd_conv_b", l).re("(c p) -> p c", p=128), allow_slow_non_contiguous=True)
            negA_g = bload(es, "negA_g", wl("gdn_a_log", l), 4)
            kb.act(negA_g, negA_g, AF.Exp)
            kb.ts("vector", negA_g, negA_g, -1.0, None, ALU.mult)
            negA_s = bload(es, "negA_s", wl("ssd_a_log", l), 4)
            kb.act(negA_s, negA_s, AF.Exp)
            kb.ts("vector", negA_s, negA_s, -1.0, None, ALU.mult)
            dtb_g = bload(es, "dtb_g", wl("gdn_dt_bias", l), 4)
            dtb_s = bload(es, "dtb_s", wl("ssd_dt_bias", l), 4)
            dsk = bload(es, "dsk", wl("ssd_d", l), 4)
            ibias = bload(es, "ibias", wl("mlstm_i_bias", l), 4)
            fbias = bload(es, "fbias", wl("mlstm_f_bias", l), 4)
            gng = bload(es, "gng", wl("gdn_norm_g", l), 64)
            gssd = bload(es, "gssd", wl("ssd_norm_g", l), 256)
            gml = bload(es, "gml", wl("mlstm_norm_g", l), 256)

            TRI, SU, ONESBD, BO = cs["c_tri"], cs["c_su"], cs["c_onesbd"], cs["c_bo"]
            NI4 = sb("NI4", [128, 512])
            kb.ts("vector", NI4, cs["c_ident4"], -1.0, 1.0, ALU.mult, ALU.add)
            MBT4, MBS4, ID4, CHI, CHJ = cs["c_mbt4"], cs["c_mbs4"], cs["c_ident4"], cs["c_chi"], cs["c_chj"]

            xt = sb("xt", [128, D])
            xn = sb("xn", [128, D], BF16)
            junk = sb("junk", [128, D], BF16)
            hT = sb("hT", [128, 8, 128], BF16)
            ss = sb("ss", [128, 1]); rs = sb("rs", [128, 1]); t1s = sb("t1s", [128, 1])
            rawG = sb("rawG", [128, 6, 131]); rawS = sb("rawS", [128, 6, 131])
            kb.memset("vector", rawG, 0.0)
            kb.memset("vector", rawS, 0.0)
            acc = [sb("acc", [128, 128]) for i in range(2)]
            gF = sb("gF", [128, 4, 128])
            sq4 = sb("sq4", [128, 4, 128]); rn4 = sb("rn4", [128, 4, 128])
            qn = sb("qn", [128, 2, 128], BF16); kn = sb("kn", [128, 2, 128], BF16); vFb = sb("vFb", [128, 2, 128], BF16)
            sgate = sb("sgate", [128, 256])
            beta = sb("beta", [128, 4]); a4 = sb("a4", [128, 4]); glog = sb("glog", [128, 4]); cbg = sb("cbg", [128, 4])
            dt4 = sb("dt4", [128, 4]); cdt = sb("cdt", [128, 4])
            E = sb("E", [128, 12]); G_all = sb("G_all", [128, 512]); DT_all = sb("DT_all", [128, 512]); Ds_all = sb("Ds_all", [128, 512])
            Kbg = sb("Kbg", [128, 256]); Vb = sb("Vb", [128, 256]); kdec = sb("kdec", [128, 256], BF16)
            Lt = sb("Lt", [128, 512])
            P = [sb("P", [128, 512]) for j in range(6)]
            PT = [sb("PT", [128, 512]) for j in range(5)]
            Y = sb("Y", [128, 512])
            U_sb = sb("U_sb", [128, 256]); WT_sb = sb("WT_sb", [128, 2, 128], BF16)
            attnT = sb("attnT", [128, 512], BF16)
            g_rep = sb("g_rep", [128, 256]); eP = sb("eP", [128, 4])
            vnew = sb("vnew", [128, 256], BF16)
            S32 = sb("S32", [128, 2, 64]); Sbf = sb("Sbf", [128, 2, 64], BF16)
            kb.memset("vector", S32, 0.0); kb.memset("vector", Sbf, 0.0)
            t1 = sb("t1", [128, 256]); o_ = sb("o_", [128, 256]); sq = sb("sq", [128, 256])
            ss4 = sb("ss4", [128, 4]); rs4 = sb("rs4", [128, 4]); tm4 = sb("tm4", [128, 4])
            sF = sb("sF", [128, 6, 128], BF16)
            xdt = sb("xdt", [128, 256], BF16); xdec = sb("xdec", [128, 256], BF16); xskip = sb("xskip", [128, 256])
            B_tm = sb("B_tm", [128, 256], BF16); MT = sb("MT", [128, 512], BF16)
            ST32 = sb("ST32", [128, 256]); STbf = sb("STbf", [128, 256], BF16); tmpS = sb("tmpS", [128, 256])
            kb.memset("vector", ST32, 0.0); kb.memset("vector", STbf, 0.0)
            etotN = sb("etotN", [128, 8]); sz = sb("sz", [128, 256]); y_ = sb("y_", [128, 256])
            ss2 = sb("ss2", [128, 2]); rs2 = sb("rs2", [128, 2]); tm2 = sb("tm2", [128, 2])
            mqk = sb("mqk", [128, 4, 128], BF16)
            li4 = sb("li4", [128, 4]); eli8 = sb("eli8", [128, 4]); f4 = sb("f4", [128, 4])
            vli = sb("vli", [128, 4, 66], BF16); STm = sb("STm", [128, 512], BF16)
            CS32 = sb("CS32", [128, 2, 66]); CSbf = sb("CSbf", [128, 2, 66], BF16)
            kb.memset("vector", CS32, 0.0); kb.memset("vector", CSbf, 0.0)
            t2 = sb("t2", [128, 4, 65]); d4 = sb("d4", [128, 4]); so = sb("so", [128, 256]); hm = sb("hm", [128, 256])
            mixo = sb("mixo", [128, 768], BF16)
            pjA = kb.ps(es, "pjA", [128, 512], F32); pjB = kb.ps(es, "pjB", [128, 512], F32)
            ptp = kb.ps(es, "ptp", [128, 1024], BF16)
            pg1 = kb.ps(es, "pg1", [128, 512], F32); pg2 = kb.ps(es, "pg2", [128, 512], F32); pg3 = kb.ps(es, "pg3", [128, 512], F32)
            po1 = kb.ps(es, "po1", [128, 512], F32); po2 = kb.ps(es, "po2", [128, 512], F32)

            v3 = lambda v, h=4: v.re("p (h d) -> p h d", h=h)

            def proj_fm(pout, col0, M=128):
                for k in range(8):
                    kb.mm(pout, winb[:, k, col0:col0 + M], hT[:, k, :], start=(k == 0), stop=(k == 7))

            def proj_tm(pout, col0, n):
                for k in range(8):
                    kb.mm(pout, hT[:, k, :], winb[:, k, col0:col0 + n], start=(k == 0), stop=(k == 7))

            def conv_chunk(praw, raw, w, bias, ac, outv):
                kb.cp("scalar", raw[:, 3:131], praw)
                if bias is None:
                    kb.ts("vector", ac, raw[:, 3:131], w[:, 3:4], None, ALU.mult)
                else:
                    kb.ts("vector", ac, raw[:, 3:131], w[:, 3:4], bias, ALU.mult, ALU.add)
                for j in (2, 1, 0):
                    kb.stt(ac, raw[:, j:j + 128], w[:, j:j + 1], ac, ALU.mult, ALU.add)
                kb.cp("gpsimd", raw[:, 0:3], raw[:, 128:131])
                kb.act(outv, ac, AF.Silu)

            def decay_prep(strict):
                kb.mm(pg1[:, 0:4], TRI, glog)
                kb.mm(pg1[:, 4:8], SU, glog)
                kb.mm(pg1[:, 8:12], ONESBD, glog)
                if upto < 4.05:
                    return
                kb.tt("vector", v3(G_all), TRI.bc(1, [128, 4, 128]), glog.bc(2, [128, 4, 128]), ALU.mult)
                if upto < 4.1:
                    return
                kb.mm(pg2, SU, G_all, start=True, stop=False)
                kb.mm(pg2, ident, MBT4, start=False, stop=True)
                if upto < 4.15:
                    return
                if strict:
                    kb.mm(pg3, ident, MBS4, start=True, stop=False)
                    for h in range(4):
                        kb.mm(pg3[:, h * 128:(h + 1) * 128], G_all[:, h * 128:(h + 1) * 128], SU, start=False, stop=(h == 3))

            def decay_exp(strict):
                kb.act(E, pg1[:, 0:12], AF.Exp)
                kb.act(DT_all, pg2, AF.Exp)
                if strict:
                    kb.act(Ds_all, pg3, AF.Exp)

            def head_norm(src, nh, dst, gain, ssv, rsv, tmv):
                kb.tt("gpsimd", sq, src, src, ALU.mult)
                kb.red(ssv, v3(sq, nh))
                kb.rstd(rsv, ssv, float(nh) / 256.0, epsv, tmv)
                kb.tt("vector", v3(src, nh), v3(src, nh), rsv.bc(2, [128, nh, 256 // nh]), ALU.mult)
                kb.tt("vector", dst, src, gain, ALU.mult)

            for b in range(nblk):
                r0 = b * 128
                kb.dma("sync", xt, xs[r0:r0 + 128, :])
                prenorm_T(xt, g1b, xn, hT, ptp, ss, rs, t1s, junk)
                if upto < 1:
                    continue
                for c in range(6):
                    pj = pjA if c % 2 == 0 else pjB
                    proj_fm(pj[:, 0:128], c * 128)
                    outv = gF[:, c, :] if c < 4 else vFb[:, c - 4, :]
                    conv_chunk(pj[:, 0:128], rawG[:, c, :], gcw[:, c, :], None, acc[c % 2], outv)
                if upto < 2:
                    continue
                kb.tt("gpsimd", sq4, gF, gF, ALU.mult)
                for i in range(4):
                    kb.mm(pg1[:, i * 128:(i + 1) * 128], BO, sq4[:, i, :])
                rn4f = rn4.re("p a b -> p (a b)")
                kb.act(rn4f, pg1, AF.Ln, bias=epsv)
                kb.act(rn4f, rn4f, AF.Exp, scale=-0.5)
                kb.stt(qn, gF[:, 0:2, :], 0.125, rn4[:, 0:2, :], ALU.mult, ALU.mult)
                kb.tt("vector", kn, gF[:, 2:4, :], rn4[:, 2:4, :], ALU.mult)
                kb.tp(ptp[:, 0:128], kn[:, 0, :], identb)
                kb.tp(ptp[:, 128:256], kn[:, 1, :], identb)
                kb.tp(ptp[:, 256:384], vFb[:, 0, :], identb)
                kb.tp(ptp[:, 384:512], vFb[:, 1, :], identb)
                kTM = v3(ptp[:, 0:256]); vTM = v3(ptp[:, 256:512])
                if upto < 3:
                    continue
                proj_tm(pjA[:, 0:264], 768, 264)
                kb.act(sgate, pjA[:, 0:256], AF.Silu)
                kb.act(beta, pjA[:, 256:260], AF.Sigmoid)
                kb.tt("vector", a4, pjA[:, 260:264], dtb_g, ALU.add)
                kb.act(a4, a4, AF.Exp)
                kb.act(a4, a4, AF.Ln, bias=1.0)
                kb.tt("vector", glog, a4, negA_g, ALU.mult)
                if upto < 4:
                    continue
                decay_prep(False)
                if upto < 4.3:
                    continue
                decay_exp(False)
                if upto < 4.6:
                    continue
                kb.tt("vector", cbg, beta, E[:, 0:4], ALU.mult)
                kb.tt("vector", v3(Kbg), kTM, cbg.bc(2, [128, 4, 64]), ALU.mult)
                kb.tt("vector", v3(Vb), vTM, beta.bc(2, [128, 4, 64]), ALU.mult)
                kb.tt("vector", v3(kdec), kTM, E[:, 4:8].bc(2, [128, 4, 64]), ALU.mult)
                if upto < 5:
                    continue
                hs = lambda h: slice(h * 128, (h + 1) * 128)
                hp = lambda h: slice((h % 2) * 64, (h % 2) * 64 + 64)
                for h in range(4):
                    kb.mm(pg1[:, hs(h)], kn[hp(h), h // 2, :], kn[hp(h), h // 2, :])
                kb.tt("vector", Lt, pg1, DT_all, ALU.mult)
                kb.tt("vector", Lt, Lt, NI4, ALU.mult)
                for h in range(4):
                    kb.tp(pg3[:, hs(h)], Lt[:, hs(h)], ident)
                kb.tt("vector", v3(P[0]), v3(pg3), beta.bc(2, [128, 4, 128]), ALU.mult)
                if upto < 6:
                    continue
                for h in range(4):
                    kb.tp(pg2[:, hs(h)], P[0][:, hs(h)], ident)
                kb.cp("scalar", PT[0], pg2)
                for j in range(5):
                    for h in range(4):
                        kb.mm(pg1[:, hs(h)], PT[j][:, hs(h)], P[j][:, hs(h)])
                    if j < 4:
                        for h in range(4):
                            kb.mm(pg2[:, hs(h)], P[j][:, hs(h)], PT[j][:, hs(h)])
                    kb.cp("scalar", P[j + 1], pg1)
                    if j < 4:
                        kb.cp("vector", PT[j + 1], pg2)
                kb.tt("vector", Y, ID4, PT[0], ALU.subtract)
                for j in range(1, 6):
                    for h in range(4):
                        kb.mm(pg3[:, hs(h)], P[j][:, hs(h)], Y[:, hs(h)])
                    kb.tt("vector", Y, Y, pg3, ALU.add)
                if upto < 7:
                    continue
                for h in range(4):
                    kb.mm(po1[:, h * 64:(h + 1) * 64], Y[:, hs(h)], Vb[:, h * 64:(h + 1) * 64])
                for h in range(4):
                    kb.mm(po2[hp(h), (h // 2) * 128:(h // 2) * 128 + 128], Kbg[:, h * 64:(h + 1) * 64], Y[:, hs(h)])
                kb.cp("scalar", U_sb, po1[:, 0:256])
                kb.cp("vector", WT_sb.re("p a b -> p (a b)"), po2[:, 0:256])
                for h in range(4):
                    kb.mm(pg1[:, hs(h)], kn[hp(h), h // 2, :], qn[hp(h), h // 2, :])
                kb.tt("vector", attnT, pg1, DT_all, ALU.mult)
                if upto < 8:
                    continue
                kb.cp("gpsimd", v3(g_rep), glog.bc(2, [128, 4, 64]))
                for pair in range(2):
                    kb.mm(pg2[:, pair * 2:pair * 2 + 2], g_rep[:, pair * 128:(pair + 1) * 128], CHI)
                kb.act(eP, pg2[:, 0:4], AF.Exp)
                for j in range(2):
                    r = slice(j * 64, j * 64 + 64)
                    for h in range(4):
                        kb.mm(pg3[r, h * 64:(h + 1) * 64], WT_sb[hp(h), h // 2, r], Sbf[hp(h), h // 2, :])
                    kb.tt("vector", vnew[r, :], U_sb[r, :], pg3[r, 0:256], ALU.subtract)
                    for h in range(4):
                        kb.mm(po1[r, h * 64:(h + 1) * 64], qn[hp(h), h // 2, r], Sbf[hp(h), h // 2, :])
                    for h in range(4):
                        kb.mm(po2[hp(h), (h // 2) * 64:(h // 2) * 64 + 64], kdec[r, h * 64:(h + 1) * 64], vnew[r, h * 64:(h + 1) * 64])
                    for pair in range(2):
                        kb.stt(S32[:, pair, :], S32[:, pair, :], eP[:, pair * 2 + j:pair * 2 + j + 1],
                               po2[:, pair * 64:(pair + 1) * 64], ALU.mult, ALU.add)
                    kb.cp("scalar", Sbf, S32)
                for h in range(4):
                    kb.mm(pg1[:, h * 64:(h + 1) * 64], attnT[:, hs(h)], vnew[:, h * 64:(h + 1) * 64])
                kb.tt("vector", v3(t1), v3(po1[:, 0:256]), E[:, 0:4].bc(2, [128, 4, 64]), ALU.mult)
                kb.tt("vector", o_, t1, pg1[:, 0:256], ALU.add)
                kb.tt("gpsimd", sq, o_, o_, ALU.mult)
                kb.red(ss4, v3(sq))
                kb.rstd(rs4, ss4, 1.0 / 64, epsv, tm4)
                kb.tt("vector", v3(o_), v3(o_), rs4.bc(2, [128, 4, 64]), ALU.mult)
                kb.tt("vector", v3(o_), v3(o_), gng.bc(1, [128, 4, 64]), ALU.mult)
                kb.tt("vector", mixo[:, 0:256], o_, sgate, ALU.mult)

                if upto < 9:
                    continue
                for c in range(6):
                    pj = pjA if c % 2 == 0 else pjB
                    proj_fm(pj[:, 0:128], 2056 + c * 128)
                    conv_chunk(pj[:, 0:128], rawS[:, c, :], scw[:, c, :], scb[:, c:c + 1], acc[c % 2], sF[:, c, :])
                for i in range(4):
                    kb.tp(ptp[:, i * 128:(i + 1) * 128], sF[:, i, :], identb)
                xsTM = v3(ptp[:, 0:256])
                proj_tm(pjA[:, 0:256], 1800, 256)
                proj_tm(pjA[:, 256:260], 2824, 4)
                kb.act(sz, pjA[:, 0:256], AF.Silu)
                kb.tt("vector", a4, pjA[:, 256:260], dtb_s, ALU.add)
                kb.act(a4, a4, AF.Exp)
                kb.act(dt4, a4, AF.Ln, bias=1.0)
                kb.tt("vector", glog, dt4, negA_s, ALU.mult)
                decay_prep(False)
                for j in range(2):
                    kb.mm(pg3[:, j * 4:(j + 1) * 4], CHJ[:, j, :], glog)
                decay_exp(False)
                kb.act(etotN, pg3[:, 0:8], AF.Exp)
                kb.tt("vector", cdt, dt4, E[:, 4:8], ALU.mult)
                kb.tt("vector", v3(xdt), xsTM, dt4.bc(2, [128, 4, 64]), ALU.mult)
                kb.tt("vector", v3(xdec), xsTM, cdt.bc(2, [128, 4, 64]), ALU.mult)
                kb.tt("vector", v3(xskip), xsTM, dsk.bc(2, [128, 4, 64]), ALU.mult)
                kb.cp("scalar", B_tm, ptp[:, 256:512])
                for g in range(2):
                    kb.mm(pg1[:, g * 128:(g + 1) * 128], sF[:, 2 + g, :], sF[:, 4 + g, :])
                MT3 = v3(MT); DT3 = v3(DT_all)
                for g in range(2):
                    kb.tt("vector", MT3[:, 2 * g:2 * g + 2, :], pg1[:, g * 128:(g + 1) * 128].bc(1, [128, 2, 128]),
                          DT3[:, 2 * g:2 * g + 2, :], ALU.mult)
                for h in range(4):
                    kb.mm(po1[:, h * 64:(h + 1) * 64], MT[:, hs(h)], xdt[:, h * 64:(h + 1) * 64])
                for j in range(2):
                    r = slice(j * 64, j * 64 + 64)
                    for h in range(4):
                        kb.mm(po2[r, h * 64:(h + 1) * 64], sF[:, 4 + h // 2, r], STbf[:, h * 64:(h + 1) * 64])
                    for h in range(4):
                        kb.mm(pg3[:, h * 64:(h + 1) * 64], B_tm[r, (h // 2) * 128:(h // 2) * 128 + 128], xdec[r, h * 64:(h + 1) * 64])
                    kb.tt("vector", v3(tmpS), v3(ST32), etotN[:, j * 4:(j + 1) * 4].bc(2, [128, 4, 64]), ALU.mult)
                    kb.tt("vector", ST32, tmpS, pg3[:, 0:256], ALU.add)
                    kb.cp("scalar", STbf, ST32)
                kb.tt("vector", v3(t1), v3(po2[:, 0:256]), E[:, 0:4].bc(2, [128, 4, 64]), ALU.mult)
                kb.tt("vector", y_, t1, po1[:, 0:256], ALU.add)
                kb.tt("vector", y_, y_, xskip, ALU.add)
                kb.tt("vector", y_, y_, sz, ALU.mult)
                kb.tt("gpsimd", sq, y_, y_, ALU.mult)
                kb.red(ss2, v3(sq, 2))
                kb.rstd(rs2, ss2, 1.0 / 128, epsv, tm2)
                kb.tt("vector", v3(y_, 2), v3(y_, 2), rs2.bc(2, [128, 2, 128]), ALU.mult)
                kb.tt("vector", mixo[:, 256:512], y_, gssd, ALU.mult)

                if upto < 10:
                    continue
                for i in range(2):
                    proj_fm(pjB[:, i * 128:(i + 1) * 128], 2828 + i * 128)
                    proj_fm(pjB[:, 256 + i * 128:256 + (i + 1) * 128], 3084 + i * 128)
                kb.cp("scalar", mqk.re("p a b -> p (a b)"), pjB)
                proj_tm(pjA, 3084, 512)
                proj_tm(pjB[:, 0:264], 3596, 264)
                kb.act(so, pjB[:, 0:256], AF.Sigmoid)
                kb.tt("vector", li4, pjB[:, 256:260], ibias, ALU.add)
                kb.act(eli8, li4, AF.Exp)
                kb.ts("vector", eli8, eli8, 0.125, None, ALU.mult)
                kb.tt("vector", f4, pjB[:, 260:264], fbias, ALU.add)
                kb.act(f4, f4, AF.Exp, scale=-1.0)
                kb.act(f4, f4, AF.Ln, bias=1.0)
                kb.ts("vector", glog, f4, -1.0, None, ALU.mult)
                decay_prep(False)
                decay_exp(False)
                kb.tt("vector", vli[:, :, 0:64], v3(pjA[:, 256:512]), eli8.bc(2, [128, 4, 64]), ALU.mult)
                kb.cp("vector", vli[:, :, 64:65], eli8.bc(2, [128, 4, 1]))
                kb.tt("vector", v3(kdec), v3(pjA[:, 0:256]), E[:, 4:8].bc(2, [128, 4, 64]), ALU.mult)
                for h in range(4):
                    kb.mm(pg1[:, hs(h)], mqk[hp(h), 2 + h // 2, :], mqk[hp(h), h // 2, :])
                kb.tt("vector", STm, pg1, DT_all, ALU.mult)
                for h in range(4):
                    kb.mm(po1[:, h * 68:h * 68 + 65], STm[:, hs(h)], vli[:, h, 0:65])
                kb.cp("gpsimd", v3(g_rep), glog.bc(2, [128, 4, 64]))
                for pair in range(2):
                    kb.mm(pg2[:, pair * 2:pair * 2 + 2], g_rep[:, pair * 128:(pair + 1) * 128], CHI)
                kb.act(eP, pg2[:, 0:4], AF.Exp)
                for j in range(2):
                    r = slice(j * 64, j * 64 + 64)
                    for h in range(4):
                        kb.mm(po2[r, h * 68:h * 68 + 65], mqk[hp(h), h // 2, r], CSbf[hp(h), h // 2, 0:65])
                    for h in range(4):
                        kb.mm(pg3[hp(h), (h // 2) * 68:(h // 2) * 68 + 65], kdec[r, h * 64:(h + 1) * 64], vli[r, h, 0:65])
                    for pair in range(2):
                        kb.stt(CS32[:, pair, 0:65], CS32[:, pair, 0:65], eP[:, pair * 2 + j:pair * 2 + j + 1],
                               pg3[:, pair * 68:pair * 68 + 65], ALU.mult, ALU.add)
                    kb.cp("scalar", CSbf, CS32)
                kb.tt("vector", t2, po2[:, 0:272].re("p (h d) -> p h d", h=4)[:, :, 0:65], E[:, 0:4].bc(2, [128, 4, 65]), ALU.mult)
                kb.tt("vector", t2, t2, po1[:, 0:272].re("p (h d) -> p h d", h=4)[:, :, 0:65], ALU.add)
                kb.tt("vector", d4, t2[:, :, 64], t2[:, :, 64], ALU.mult)
                kb.ts("vector", d4, d4, 1.0, None, ALU.max)
                kb.act(d4, d4, AF.Ln)
                kb.act(d4, d4, AF.Exp, scale=-0.5)
                kb.tt("vector", v3(hm), t2[:, :, 0:64], d4.bc(2, [128, 4, 64]), ALU.mult)
                kb.tt("vector", hm, hm, so, ALU.mult)
                kb.tt("gpsimd", sq, hm, hm, ALU.mult)
                kb.red(ss4, v3(sq))
                kb.rstd(rs4, ss4, 1.0 / 64, epsv, tm4)
                kb.tt("vector", v3(hm), v3(hm), rs4.bc(2, [128, 4, 64]), ALU.mult)
                kb.tt("vector", mixo[:, 512:768], hm, gml, ALU.mult)
                kb.dma("sync", mixA[r0:r0 + 128, :], mixo)
            S.barrier()

    def mixb_phase(l, xs):
        lambda_init = 0.8 - 0.6 * math.exp(-0.3 * l)
        with ExitStack() as es:
            sb = lambda n, shp, dt=F32: kb.sb(es, n, shp, dt)
            g1b = bload(es, "g1b", wl("mix_pre_g", l), D)
            g2b = bload(es, "g2b", wl("mix_post_g", l), D)
            gdf = bload(es, "gdf", wl("diff_norm_g", l), 64, scale=(1.0 - lambda_init))
            winb = sb("winb", [128, 8, 768], BF16)
            wsrc = Wb[(l, "w_in")].re("(k p) n -> p k n", p=128)
            woutb = sb("woutb", [128, 8, D], BF16)
            wosrc = Wb[(l, "w_out")].re("(k p) n -> p k n", p=128)
            for k in range(8):
                kb.dma("sync", winb[:, k, :], wsrc[:, k, 1032:1800])
                kb.dma("sync", woutb[:, k, :], wosrc[:, k, :])
            lq1 = bload(es, "lq1", wl("diff_lam_q1", l), 32); lk1 = bload(es, "lk1", wl("diff_lam_k1", l), 32)
            lq2 = bload(es, "lq2", wl("diff_lam_q2", l), 32); lk2 = bload(es, "lk2", wl("diff_lam_k2", l), 32)
            lam2 = sb("lam2", [128, 2]); neglam = sb("neglam", [128, 1])
            kb.tt("vector", lq1, lq1, lk1, ALU.mult)
            kb.tt("vector", lq2, lq2, lk2, ALU.mult)
            kb.red(lam2[:, 0:1], lq1)
            kb.red(lam2[:, 1:2], lq2)
            kb.act(lam2, lam2, AF.Exp)
            kb.tt("vector", neglam, lam2[:, 1:2], lam2[:, 0:1], ALU.subtract)
            kb.ts("vector", neglam, neglam, -lambda_init, None, ALU.add)

            xt = sb("xt", [128, D]); xn = sb("xn", [128, D], BF16); junk = sb("junk", [128, D], BF16)
            tmp = sb("tmp", [128, D])
            hT = sb("hT", [128, 8, 128], BF16)
            ss = sb("ss", [128, 1]); rs = sb("rs", [128, 1]); t1s = sb("t1s", [128, 1]); q2 = sb("q2", [128, 2])
            KT = sb("KT", [64, 4, S_LEN], BF16)
            Vaug = sb("Vaug", [128, NB, 4, 66], BF16)
            kb.memset("vector", Vaug.re("p a b c -> p (a b c)"), 1.0)
            cosb = sb("cosb", [64, 128]); sinb = sb("sinb", [64, 128])
            qraw = sb("qraw", [64, 512], BF16)
            r1 = sb("r1", [64, 512]); r2 = sb("r2", [64, 512])
            qr = sb("qr", [64, 4, 128], BF16)
            PTt = [sb("PTt", [128, 512], BF16) for i in range(2)]
            rec8 = sb("rec8", [128, 8]); on = sb("on", [128, 8, 64]); od = sb("od", [128, 256]); sq = sb("sq", [128, 256])
            ss4 = sb("ss4", [128, 4]); rs4 = sb("rs4", [128, 4]); tm4 = sb("tm4", [128, 4])
            mixed = sb("mixed", [128, D], BF16); mT = sb("mT", [128, 8, 128], BF16)
            mdbg = sb("mdbg", [128, D]) if dbg else None
            pT = kb.ps(es, "pT", [128, 1024], BF16)
            pq = kb.ps(es, "pq", [128, 512], F32)
            pS = [kb.ps(es, "pS", [128, 512], F32) for i in range(2)]
            pA = [kb.ps(es, "pA", [128, 512], F32) for i in range(2)]
            pY = [kb.ps(es, "pY", [128, 512], F32) for i in range(2)]
            v3 = lambda v, h=4: v.re("p (h d) -> p h d", h=h)
            sc = 32.0 ** -0.5
            nexp = [0]

            for b in range(nblk):
                r0 = b * 128
                kb.dma("sync", xt, xs[r0:r0 + 128, :])
                kb.dma("sync", cosb, C["c_cos"][:, r0:r0 + 128])
                kb.dma("sync", sinb, C["c_sin"][:, r0:r0 + 128])
                kb.dma("sync", mixed[:, 0:256], mixA[r0:r0 + 128, 0:256])
                kb.dma("sync", mixed[:, 512:1024], mixA[r0:r0 + 128, 256:768])
                prenorm_T(xt, g1b, xn, hT, pT, ss, rs, t1s, junk)
                for which in range(2):
                    for h in range(4):
                        for k in range(8):
                            kb.mm(pq[0:64, h * 128:(h + 1) * 128], winb[:, k, which * 256 + h * 64:which * 256 + (h + 1) * 64],
                                  hT[:, k, :], start=(k == 0), stop=(k == 7))
                    kb.cp("scalar", qraw, pq[0:64, :])
                    kb.mm(pS[0][0:64, :], protb, qraw)
                    kb.tt("vector", v3(r1), v3(qraw), cosb.bc(1, [64, 4, 128]), ALU.mult)
                    kb.tt("vector", v3(r2), v3(pS[0][0:64, :]), sinb.bc(1, [64, 4, 128]), ALU.mult)
                    dst = qr if which == 0 else KT[:, :, r0:r0 + 128]
                    kb.tt("vector", dst, v3(r1), v3(r2), ALU.add)
                for k in range(8):
                    kb.mm(pq[:, 0:256], hT[:, k, :], winb[:, k, 512:768], start=(k == 0), stop=(k == 7))
                kb.cp("scalar", Vaug[:, b, :, 0:64], v3(pq[:, 0:256]))
                kb.memset("vector", pA[0], 0.0)
                kb.memset("vector", pA[1], 0.0)
                for g0 in range(0, b + 1, 4):
                    kbs = list(range(g0, min(b + 1, g0 + 4)))
                    n = len(kbs)
                    for hmi in range(8):
                        h, mp = hmi // 2, hmi % 2
                        ps_ = pS[nexp[0] % 2]
                        pt_ = PTt[nexp[0] % 2]
                        nexp[0] += 1
                        for i, kbi in enumerate(kbs):
                            kb.mm(ps_[:, i * 128:(i + 1) * 128], KT[mp * 32:(mp + 1) * 32, h, kbi * 128:(kbi + 1) * 128],
                                  qr[mp * 32:(mp + 1) * 32, h, :])
                        kb.act(pt_[:, 0:n * 128], ps_[:, 0:n * 128], AF.Exp, scale=sc)
                        if kbs[-1] == b:
                            i = n - 1
                            kb.memset("gpsimd", pt_[64:128, i * 128:i * 128 + 64], 0.0)
                        for i, kbi in enumerate(kbs):
                            kb.mm(pA[hmi // 4][:, (hmi % 4) * 68:(hmi % 4) * 68 + 65], pt_[:, i * 128:(i + 1) * 128],
                                  Vaug[:, kbi, h, 0:65], start=False, stop=(kbi == b), skip=True)
                for i in range(2):
                    a3 = pA[i][:, 0:272].re("p (h d) -> p h d", h=4)
                    kb.recip(rec8[:, i * 4:(i + 1) * 4], a3[:, :, 64])
                    kb.tt("vector", on[:, i * 4:(i + 1) * 4, :], a3[:, :, 0:64], rec8[:, i * 4:(i + 1) * 4].bc(2, [128, 4, 64]), ALU.mult)
                on4 = on.re("p (h m) d -> p h m d", m=2)
                kb.stt(v3(od), on4[:, :, 1, :], neglam[:, 0:1], on4[:, :, 0, :], ALU.mult, ALU.add)
                kb.tt("gpsimd", sq, od, od, ALU.mult)
                kb.red(ss4, v3(sq))
                kb.rstd(rs4, ss4, 1.0 / 64, epsv, tm4)
                kb.tt("vector", v3(od), v3(od), rs4.bc(2, [128, 4, 64]), ALU.mult)
                kb.tt("vector", v3(mixed[:, 256:512]), v3(od), gdf.bc(1, [128, 4, 64]), ALU.mult)
                if dbg:
                    kb.cp("vector", mdbg, mixed)
                    kb.dma("sync", dbg_mixed[r0:r0 + 128, :], mdbg)
                for k in range(8):
                    kb.tp(pT[:, k * 128:(k + 1) * 128], mixed[:, k * 128:(k + 1) * 128], identb)
                kb.cp("scalar", mT, pT.re("p (k n) -> p k n", k=8))
                for c in range(2):
                    for k in range(8):
                        kb.mm(pY[c], mT[:, k, :], woutb[:, k, c * 512:(c + 1) * 512], start=(k == 0), stop=(k == 7))
                post_res(pY[0], pY[1], xt, g2b, tmp, q2, rs, t1s, junk)
                kb.dma("sync", xs[r0:r0 + 128, :], xt)
            S.barrier()

    if "ffn1" not in stages:
        for r0 in range(0, S_LEN, 512):
            kb.dma("sync", out[r0:r0 + 512, :], x_in[r0:r0 + 512, :])
    for l in range(nlayers):
        src = x_in if l == 0 else out
        if "ffn1" in stages:
            ffn_phase(l, 1, src, out)
        if "mixa" in stages:
            mixa_phase(l, out)
        if "mixb" in stages:
            mixb_phase(l, out)
        if "ffn2" in stages:
            ffn_phase(l, 2, out, out)
    S.wait_res("sync", [out.r] + ([dbg_mixed.r] if dbg else []))
    es0.close()
    return nc, hc


_CACHE = {}


def kernel(**inputs):
    x = np.ascontiguousarray(np.asarray(inputs["x"], dtype=np.float32))
    nb = x.shape[0]
    if "nc" not in _CACHE:
        _CACHE["nc"] = build()
    nc, hc = _CACHE["nc"]
    shared = {n: np.ascontiguousarray(np.asarray(inputs[n], dtype=np.float32)) for n, _ in W_SPECS}
    shared.update(hc)
    in_maps = []
    for b in range(nb):
        m = dict(shared)
        m["x"] = x[b]
        in_maps.append(m)
    res = run_bass_kernel_spmd(nc, in_maps, core_ids=list(range(nb)))
    return np.stack([np.asarray(r["out"], dtype=np.float32) for r in res.results], axis=0)
```

```python
import math
from contextlib import ExitStack

import numpy as np
import ml_dtypes
import concourse.bass as bass
import concourse.mybir as mybir
from concourse.bass_utils import run_bass_kernel_spmd

F32 = mybir.dt.float32
BF16 = mybir.dt.bfloat16
AF = mybir.ActivationFunctionType
ALU = mybir.AluOpType
AX = mybir.AxisListType

S_LEN = 4096
D = 1024
DFF = 2816
INC = 3860
NB = S_LEN // 128
EPS = 1e-6
NEG = -30000.0


class Res:
    __slots__ = ("lw", "rd", "excl", "pe")

    def __init__(self):
        self.lw = None
        self.rd = []
        self.excl = False
        self.pe = None


class V:
    __slots__ = ("ap", "r")

    def __init__(self, ap, r=None):
        self.ap = ap
        self.r = r if r is not None else Res()

    def __getitem__(self, k):
        return V(self.ap[k], self.r)

    def re(self, pat, **kw):
        return V(self.ap.rearrange(pat, **kw), self.r)

    def bc(self, axis, shape):
        return V(self.ap.unsqueeze(axis).broadcast_to(list(shape)), self.r)


class Sched:
    ENG = ("tensor", "vector", "scalar", "gpsimd", "sync")
    NSLOT = 8

    def __init__(self, nc, es):
        self.nc = nc
        self.eng = {e: getattr(nc, e) for e in self.ENG}
        self.sem = {e: es.enter_context(nc.semaphore("s_" + e)) for e in self.ENG}
        self.cnt = {e: 0 for e in self.ENG}
        self.known = {e: {} for e in self.ENG}
        self.semobj = {}
        self.maxv = {}
        self.rec = None
        self.dq = {}
        for q in ("sync", "gpsimd", "scalar"):
            self.dq[q] = dict(
                sems=[es.enter_context(nc.semaphore("d_%s%d" % (q, i))) for i in range(self.NSLOT)], n=0)

    def _wait(self, e, tok):
        sem, val = tok
        k = self.known[e]
        if k.get(id(sem), 0) >= val:
            return
        k[id(sem)] = val
        self.eng[e].wait_ge(sem, val)

    def _deps(self, e, reads, writes):
        mysem = self.sem.get(e)
        for r in reads:
            if r.lw is not None:
                if not (e == "tensor" and r.lw[0] is mysem):
                    self._wait(e, r.lw)
        for w in writes:
            if w.lw is not None:
                if not (e == "tensor" and w.lw[0] is mysem):
                    self._wait(e, w.lw)
            for t in w.rd:
                if not (e == "tensor" and t[0] is mysem):
                    self._wait(e, t)

    def _commit(self, tok, reads, writes):
        self.semobj[id(tok[0])] = tok[0]
        self.maxv[id(tok[0])] = max(self.maxv.get(id(tok[0]), 0), tok[1])
        for r in reads:
            r.rd.append(tok)
            if len(r.rd) > 12:
                best = {}
                for t in r.rd:
                    if t[1] > best.get(id(t[0]), (None, -1))[1]:
                        best[id(t[0])] = t
                r.rd = list(best.values())
        for w in writes:
            w.lw = tok
            w.rd = []

    def op(self, e, fn, reads=(), writes=(), pe=None):
        if self.rec is not None:
            self.rec.append(("op", e, fn, reads, writes, pe))
            return None
        writes = list(writes) + [r for r in reads if r.excl]
        reads = [r for r in reads if not r.excl]
        if pe is not None:
            res, rb = pe
            prev = res.pe
            if prev is not None and prev[0] != rb:
                self._wait(e, prev[1])
        self._deps(e, reads, writes)
        ins = fn(self.eng[e])
        self.cnt[e] += 1
        ins.then_inc(self.sem[e], 1)
        tok = (self.sem[e], self.cnt[e])
        self._commit(tok, reads, writes)
        if pe is not None:
            pe[0].pe = (pe[1], tok)
        return tok

    def dma(self, q, out, in_, reads=(), writes=(), **kw):
        if self.rec is not None:
            self.rec.append(("dma", q, out, in_, reads, writes, kw))
            return None
        d = self.dq[q]
        n = d["n"]
        sem = d["sems"][n % self.NSLOT]
        if n >= self.NSLOT:
            self._wait(q, (sem, 16 * (n // self.NSLOT)))
        self._deps(q, reads, writes)
        ins = self.eng[q].dma_start(out=out, in_=in_, **kw)
        ins.then_inc(sem, 16)
        d["n"] = n + 1
        tok = (sem, 16 * (n // self.NSLOT + 1))
        self._commit(tok, reads, writes)
        return tok

    def emit(self, item):
        if item[0] == "op":
            _, e, fn, reads, writes, pe = item
            self.op(e, fn, reads, writes, pe=pe)
        else:
            _, q, out, in_, reads, writes, kw = item
            self.dma(q, out, in_, reads=reads, writes=writes, **kw)

    def interleave(self, lists):
        assert self.rec is None
        lists = [l for l in lists if l]
        pos = [0] * len(lists)
        total = sum(len(l) for l in lists)
        for _ in range(total):
            best, bf = None, None
            for i, l in enumerate(lists):
                if pos[i] < len(l):
                    f = pos[i] / float(len(l))
                    if bf is None or f < bf:
                        best, bf = i, f
            self.emit(lists[best][pos[best]])
            pos[best] += 1

    def barrier(self, skip_queues=("gpsimd",)):
        skip = set()
        for q in skip_queues:
            for s in self.dq[q]["sems"]:
                skip.add(id(s))
        for e in self.ENG:
            for sid, v in self.maxv.items():
                if sid in skip:
                    continue
                self._wait(e, (self.semobj[sid], v))

    def wait_res(self, e, ress):
        for r in ress:
            if r.lw is not None:
                self._wait(e, r.lw)
            for t in r.rd:
                self._wait(e, t)


class KB:
    def __init__(self, nc, es):
        self.nc = nc
        self.S = Sched(nc, es)
        self.ncnt = 0

    def _nm(self, n):
        self.ncnt += 1
        return "%s_%d" % (n, self.ncnt)

    def sb(self, es, name, shape, dt):
        t = es.enter_context(self.nc.sbuf_tensor(self._nm(name), list(shape), dt))
        return V(t[:])

    def ps(self, es, name, shape, dt):
        t = es.enter_context(self.nc.psum_tensor(self._nm(name), list(shape), dt))
        v = V(t[:])
        v.r.excl = True
        return v

    def dram(self, name, shape, dt, kind=None):
        if kind is None:
            t = self.nc.dram_tensor(name, list(shape), dt)
        else:
            t = self.nc.dram_tensor(name, list(shape), dt, kind=kind)
        return V(t.ap())

    def mm(self, out, lhsT, rhs, start=True, stop=True, skip=False):
        def _v(x):
            return x() if callable(x) else x
        rb = (_v(lhsT.ap.base_partition), _v(lhsT.ap.partition_size))
        if skip:
            self.S.op("tensor", lambda e: e.matmul(out.ap, lhsT=lhsT.ap, rhs=rhs.ap, start=start, stop=stop, skip_group_check=True),
                      [lhsT.r, rhs.r], [out.r], pe=(out.r, rb))
        else:
            self.S.op("tensor", lambda e: e.matmul(out.ap, lhsT=lhsT.ap, rhs=rhs.ap, start=start, stop=stop),
                      [lhsT.r, rhs.r], [out.r], pe=(out.r, rb))

    def tp(self, out, in_, ident):
        def _v(x):
            return x() if callable(x) else x
        rb = (_v(in_.ap.base_partition), _v(in_.ap.partition_size))
        self.S.op("tensor", lambda e: e.transpose(out=out.ap, in_=in_.ap, identity=ident.ap),
                  [in_.r, ident.r], [out.r], pe=(out.r, rb))

    def act(self, out, in_, func, scale=1.0, bias=None, accum=None):
        rd = [in_.r]
        wr = [out.r]
        kw = {}
        if bias is not None:
            if isinstance(bias, V):
                kw["bias"] = bias.ap
                rd.append(bias.r)
            else:
                kw["bias"] = bias
        if isinstance(scale, V):
            rd.append(scale.r)
            kw["scale"] = scale.ap
        else:
            kw["scale"] = scale
        if accum is not None:
            kw["accum_out"] = accum.ap
            wr.append(accum.r)
        self.S.op("scalar", lambda e: e.activation(out=out.ap, in_=in_.ap, func=func, **kw), rd, wr)

    def tt(self, eng, out, a, b, op):
        self.S.op(eng, lambda e: e.tensor_tensor(out=out.ap, in0=a.ap, in1=b.ap, op=op), [a.r, b.r], [out.r])

    def ts(self, eng, out, a, s1, s2=None, op0=ALU.mult, op1=None):
        rd = [a.r]
        s1a = s1
        if isinstance(s1, V):
            rd.append(s1.r)
            s1a = s1.ap
        s2a = s2
        if isinstance(s2, V):
            rd.append(s2.r)
            s2a = s2.ap
        if op1 is None:
            self.S.op(eng, lambda e: e.tensor_scalar(out=out.ap, in0=a.ap, scalar1=s1a, scalar2=None, op0=op0), rd, [out.r])
        else:
            self.S.op(eng, lambda e: e.tensor_scalar(out=out.ap, in0=a.ap, scalar1=s1a, scalar2=s2a, op0=op0, op1=op1), rd, [out.r])

    def stt(self, out, a, scalar, b, op0, op1):
        rd = [a.r, b.r]
        sa = scalar
        if isinstance(scalar, V):
            rd.append(scalar.r)
            sa = scalar.ap
        self.S.op("vector", lambda e: e.scalar_tensor_tensor(out=out.ap, in0=a.ap, scalar=sa, in1=b.ap, op0=op0, op1=op1), rd, [out.r])

    def cp(self, eng, out, in_):
        if eng == "scalar":
            self.S.op("scalar", lambda e: e.copy(out=out.ap, in_=in_.ap), [in_.r], [out.r])
        else:
            self.S.op(eng, lambda e: e.tensor_copy(out=out.ap, in_=in_.ap), [in_.r], [out.r])

    def red(self, out, in_, op=ALU.add):
        self.S.op("vector", lambda e: e.tensor_reduce(out=out.ap, in_=in_.ap, axis=AX.X, op=op), [in_.r], [out.r])

    def recip(self, out, in_):
        self.S.op("vector", lambda e: e.reciprocal(out=out.ap, in_=in_.ap), [in_.r], [out.r])

    def memset(self, eng, out, val):
        self.S.op(eng, lambda e: e.memset(out.ap, val), [], [out.r])

    def dma(self, q, out, in_, **kw):
        self.S.dma(q, out.ap, in_.ap, reads=[in_.r], writes=[out.r], **kw)

    def rstd(self, out, ss, inv_n, epsv, tmp):
        self.act(tmp, ss, AF.Ln, scale=inv_n, bias=epsv)
        self.act(out, tmp, AF.Exp, scale=-0.5)


def host_consts():
    c = {}
    idx = np.arange(128)
    same = (idx[:, None] // 64) == (idx[None, :] // 64)
    c["c_ident"] = np.eye(128, dtype=np.float32)
    c["c_tri"] = (same & (idx[:, None] <= idx[None, :])).astype(np.float32)
    c["c_su"] = (same & (idx[:, None] > idx[None, :])).astype(np.float32)
    c["c_onesbd"] = same.astype(np.float32)
    mbt = np.where(same & (idx[:, None] <= idx[None, :]), 0.0, NEG).astype(np.float32)
    mbs = np.where(same & (idx[None, :] < idx[:, None]), 0.0, NEG).astype(np.float32)
    c["c_mbt4"] = np.tile(mbt, (1, 4))
    c["c_mbs4"] = np.tile(mbs, (1, 4))
    c["c_ident4"] = np.tile(np.eye(128, dtype=np.float32), (1, 4))
    c["c_bo"] = same.astype(np.float32)
    chi = np.zeros((128, 2), np.float32)
    chi[:64, 0] = 1
    chi[64:, 1] = 1
    c["c_chi"] = chi
    chj = np.zeros((2, 128, 128), np.float32)
    chj[0, :64, :] = 1
    chj[1, 64:, :] = 1
    c["c_chj"] = chj
    p = np.arange(64)
    dloc = p % 32
    perm = np.where(dloc < 16, p + 16, p - 16)
    prot = np.zeros((64, 64), np.float32)
    prot[perm, p] = 1.0
    c["c_prot"] = prot
    inv_freq = (10000.0 ** (-(np.arange(0, 32, 2, dtype=np.float32)) / np.float32(32))).astype(np.float32)
    ang = (np.arange(S_LEN, dtype=np.float32)[:, None] * inv_freq[None, :]).astype(np.float32)
    cos = np.cos(ang).astype(np.float32)
    sin = np.sin(ang).astype(np.float32)
    cosT = np.zeros((64, S_LEN), np.float32)
    sinT = np.zeros((64, S_LEN), np.float32)
    for pp in range(64):
        dl = pp % 32
        cosT[pp] = cos[:, dl % 16]
        sinT[pp] = -sin[:, dl] if dl < 16 else sin[:, dl - 16]
    c["c_cos"] = cosT
    c["c_sin"] = sinT
    return c


W_SPECS = [
    ("ffn1_pre_g", [2, 1024]), ("ffn1_w_gate", [2, 1024, 2816]), ("ffn1_w_up", [2, 1024, 2816]),
    ("ffn1_w_down", [2, 2816, 1024]), ("ffn1_post_g", [2, 1024]), ("mix_pre_g", [2, 1024]),
    ("w_in", [2, 1024, 3860]), ("gdn_conv_w", [2, 4, 768]), ("gdn_a_log", [2, 4]), ("gdn_dt_bias", [2, 4]),
    ("gdn_norm_g", [2, 64]), ("diff_lam_q1", [2, 32]), ("diff_lam_k1", [2, 32]), ("diff_lam_q2", [2, 32]),
    ("diff_lam_k2", [2, 32]), ("diff_norm_g", [2, 64]), ("ssd_conv_w", [2, 4, 768]), ("ssd_conv_b", [2, 768]),
    ("ssd_a_log", [2, 4]), ("ssd_dt_bias", [2, 4]), ("ssd_d", [2, 4]), ("ssd_norm_g", [2, 256]),
    ("mlstm_i_bias", [2, 4]), ("mlstm_f_bias", [2, 4]), ("mlstm_norm_g", [2, 256]), ("w_out", [2, 1024, 1024]),
    ("mix_post_g", [2, 1024]), ("ffn2_pre_g", [2, 1024]), ("ffn2_w_gate", [2, 1024, 2816]),
    ("ffn2_w_up", [2, 1024, 2816]), ("ffn2_w_down", [2, 2816, 1024]), ("ffn2_post_g", [2, 1024]),
]


def build(nlayers=2, stages=("ffn1", "mixa", "mixb", "ffn2"), dbg=False, nblk=NB, upto=99):
    nc = bass.Bass("TRN2", target_bir_lowering=False)
    es0 = ExitStack()
    kb = KB(nc, es0)
    S = kb.S
    x_in = kb.dram("x", [S_LEN, D], F32, "ExternalInput")
    out = kb.dram("out", [S_LEN, D], F32, "ExternalOutput")
    W = {n: kb.dram(n, s, F32, "ExternalInput") for n, s in W_SPECS}
    hc = host_consts()
    C = {n: kb.dram(n, list(a.shape), F32, "ExternalInput") for n, a in hc.items()}
    mixA = kb.dram("mixA", [S_LEN, 768], BF16)
    dbg_mixed = kb.dram("dbg_mixed", [S_LEN, D], F32, "ExternalOutput") if dbg else None

    Wb = {}
    for l in range(nlayers):
        for f in (1, 2):
            for nm, shp in (("w_gate", [D, DFF]), ("w_up", [D, DFF]), ("w_down", [DFF, D])):
                Wb[(l, "ffn%d_%s" % (f, nm))] = kb.dram("wb_%d_ffn%d_%s" % (l, f, nm), shp, BF16)
        Wb[(l, "w_in")] = kb.dram("wb_%d_w_in" % l, [D, INC], BF16)
        Wb[(l, "w_out")] = kb.dram("wb_%d_w_out" % l, [D, D], BF16)

    def cast_weight(l, name):
        src = W[name]
        dst = Wb[(l, name)]
        rows = src.ap.shape[1]
        for r0 in range(0, rows, 256):
            r1 = min(rows, r0 + 256)
            kb.dma("gpsimd", dst[r0:r1, :], V(src.ap[l], src.r)[r0:r1, :])

    for l in range(nlayers):
        for name in ("ffn1_w_gate", "ffn1_w_up", "ffn1_w_down", "w_in", "w_out", "ffn2_w_gate", "ffn2_w_up", "ffn2_w_down"):
            cast_weight(l, name)

    cs = {}
    for n in ("c_ident", "c_tri", "c_su", "c_onesbd", "c_bo"):
        cs[n] = kb.sb(es0, n, [128, 128], F32)
        kb.dma("sync", cs[n], C[n])
    for n in ("c_mbt4", "c_mbs4", "c_ident4"):
        cs[n] = kb.sb(es0, n, [128, 512], F32)
        kb.dma("sync", cs[n], C[n])
    cs["c_chi"] = kb.sb(es0, "c_chi", [128, 2], F32)
    kb.dma("sync", cs["c_chi"], C["c_chi"])
    cs["c_chj"] = kb.sb(es0, "c_chj", [128, 2, 128], F32)
    kb.dma("sync", cs["c_chj"], C["c_chj"].re("j k c -> k j c"))
    identb = kb.sb(es0, "identb", [128, 128], BF16)
    kb.cp("vector", identb, cs["c_ident"])
    protf = kb.sb(es0, "protf", [64, 64], F32)
    kb.dma("sync", protf, C["c_prot"])
    protb = kb.sb(es0, "protb", [64, 64], BF16)
    kb.cp("vector", protb, protf)
    epsv = kb.sb(es0, "epsv", [128, 1], F32)
    kb.memset("vector", epsv, EPS)
    ident = cs["c_ident"]

    def bload(es, name, src_ap_v, n, scale=None):
        t = kb.sb(es, name, [128, n], F32)
        kb.S.dma("sync", t.ap, src_ap_v.ap.partition_broadcast(128), reads=[src_ap_v.r], writes=[t.r])
        if scale is not None:
            kb.ts("vector", t, t, scale, None, ALU.mult)
        return t

    def wl(name, l):
        w = W[name]
        return V(w.ap[l], w.r)

    def prenorm_T(xt, g1b, xn, hT_dst, pT, ss, rs, tmp1, junk):
        kb.memset("gpsimd", ss, 0.0)
        kb.act(junk, xt, AF.Square, accum=ss)
        kb.rstd(rs, ss, 1.0 / D, epsv, tmp1)
        kb.stt(xn, xt, rs[:, 0:1], g1b, ALU.mult, ALU.mult)
        for k in range(8):
            kb.tp(pT[:, k * 128:(k + 1) * 128], xn[:, k * 128:(k + 1) * 128], identb)
        kb.cp("scalar", hT_dst, pT.re("p (k n) -> p k n", k=8))

    def post_res(ya, yb, xt, g2b, tmp, q2, rs2, tmp1, junk):
        kb.memset("gpsimd", q2, 0.0)
        kb.act(junk[:, 0:512], ya, AF.Square, accum=q2[:, 0:1])
        kb.act(junk[:, 512:1024], yb, AF.Square, accum=q2[:, 1:2])
        kb.tt("vector", q2[:, 0:1], q2[:, 0:1], q2[:, 1:2], ALU.add)
        kb.rstd(rs2, q2[:, 0:1], 1.0 / D, epsv, tmp1)
        kb.tt("vector", tmp[:, 0:512], ya, g2b[:, 0:512], ALU.mult)
        kb.tt("vector", tmp[:, 512:1024], yb, g2b[:, 512:1024], ALU.mult)
        kb.stt(xt, tmp, rs2[:, 0:1], xt, ALU.mult, ALU.add)

    def ffn_phase(l, f, src, dst):
        pre = "ffn%d_" % f
        with ExitStack() as es:
            g1b = bload(es, "g1b", wl(pre + "pre_g", l), D)
            g2b = bload(es, "g2b", wl(pre + "post_g", l), D, scale=0.5)
            xt = [[kb.sb(es, "xt", [128, D], F32) for s in range(4)] for par in range(2)]
            xn = kb.sb(es, "xn", [128, D], BF16)
            junk = kb.sb(es, "junk", [128, D], BF16)
            tmp = kb.sb(es, "tmp", [128, D], F32)
            hT = kb.sb(es, "hT", [128, 8, 512], BF16)
            actb = kb.sb(es, "actb", [128, 22, 512], BF16)
            slabs = [kb.sb(es, "slab", [128, 11264], BF16) for i in range(4)]
            sg = [kb.sb(es, "sg", [128, 512], F32) for i in range(2)]
            ysb = [kb.sb(es, "ysb", [128, 512], F32) for i in range(4)]
            ss = kb.sb(es, "ss", [128, 1], F32)
            rs = kb.sb(es, "rs", [128, 1], F32)
            t1 = kb.sb(es, "t1", [128, 1], F32)
            q2 = kb.sb(es, "q2", [128, 2], F32)
            pT = [kb.ps(es, "pT", [128, 1024], BF16) for i in range(2)]
            pG = [kb.ps(es, "pG", [128, 512], F32) for i in range(2)]
            pU = [kb.ps(es, "pU", [128, 512], F32) for i in range(2)]
            pY = [kb.ps(es, "pY", [128, 512], F32) for i in range(2)]
            wg = Wb[(l, pre + "w_gate")].re("(k p) n -> p k n", p=128)
            wu = Wb[(l, pre + "w_up")].re("(k p) n -> p k n", p=128)
            wd = Wb[(l, pre + "w_down")].re("(f p) n -> p f n", p=128)
            nslab = [0]

            def load_slab(kind, half):
                sl = slabs[nslab[0] % 4]
                nslab[0] += 1
                if kind == "g":
                    v = sl.re("p (k n) -> p k n", k=8)
                    kb.dma("sync", v, wg[:, :, half * 1408:(half + 1) * 1408])
                elif kind == "u":
                    v = sl.re("p (k n) -> p k n", k=8)
                    kb.dma("sync", v, wu[:, :, half * 1408:(half + 1) * 1408])
                else:
                    v = sl.re("p (f n) -> p f n", f=22)
                    kb.dma("sync", v, wd[:, :, half * 512:(half + 1) * 512])
                return v

            def load_x(t):
                for s in range(4):
                    r0 = (t * 4 + s) * 128
                    kb.dma("scalar", xt[t % 2][s], src[r0:r0 + 128, :])

            load_x(0)
            NT = S_LEN // 512
            for t in range(NT):
                par = t % 2
                sg0 = load_slab("g", 0)
                su0 = load_slab("u", 0)
                for s in range(4):
                    prenorm_T(xt[par][s], g1b, xn, hT[:, :, s * 128:(s + 1) * 128], pT[s % 2], ss, rs, t1, junk)
                if t + 1 < NT:
                    load_x(t + 1)
                sg1 = load_slab("g", 1)
                su1 = load_slab("u", 1)
                for half, (sgw, suw) in enumerate(((sg0, su0), (sg1, su1))):
                    for fi in range(11):
                        fc = half * 11 + fi
                        pg = pG[fc % 2]
                        pu = pU[fc % 2]
                        for k in range(8):
                            kb.mm(pg, sgw[:, k, fi * 128:(fi + 1) * 128], hT[:, k, :], start=(k == 0), stop=(k == 7))
                        for k in range(8):
                            kb.mm(pu, suw[:, k, fi * 128:(fi + 1) * 128], hT[:, k, :], start=(k == 0), stop=(k == 7))
                        kb.act(sg[fc % 2], pg, AF.Silu)
                        kb.tt("vector", actb[:, fc, :], sg[fc % 2], pu, ALU.mult)
                    if half == 0:
                        sd0 = load_slab("d", 0)
                sd1 = load_slab("d", 1)
                for s in range(4):
                    py = pY[s % 2]
                    for fc in range(22):
                        kb.mm(py, actb[:, fc, s * 128:(s + 1) * 128], sd0[:, fc, :], start=(fc == 0), stop=(fc == 21))
                    kb.cp("scalar", ysb[s], py)
                for s in range(4):
                    py = pY[s % 2]
                    for fc in range(22):
                        kb.mm(py, actb[:, fc, s * 128:(s + 1) * 128], sd1[:, fc, :], start=(fc == 0), stop=(fc == 21))
                    post_res(ysb[s], py, xt[par][s], g2b, tmp, q2, rs, t1, junk)
                    r0 = (t * 4 + s) * 128
                    kb.dma("scalar", dst[r0:r0 + 128, :], xt[par][s])
            S.barrier()

    def mixa_phase(l, xs):
        with ExitStack() as es:
            sb = lambda n, shp, dt=F32: kb.sb(es, n, shp, dt)
            g1b = bload(es, "g1b", wl("mix_pre_g", l), D)
            winb = sb("winb", [128, 8, INC], BF16)
            wsrc = Wb[(l, "w_in")].re("(k p) n -> p k n", p=128)
            for k in range(8):
                kb.dma("sync", winb[:, k, :], wsrc[:, k, :])
            gcw = sb("gcw", [128, 6, 4])
            scw = sb("scw", [128, 6, 4])
            scb = sb("scb", [128, 6])
            for j in range(4):
                kb.dma("sync", gcw[:, :, j], wl("gdn_conv_w", l)[j].re("(c p) -> p c", p=128), allow_slow_non_contiguous=True)
                kb.dma("sync", scw[:, :, j], wl("ssd_conv_w", l)[j].re("(c p) -> p c", p=128), allow_slow_non_contiguous=True)
            kb.dma("sync", scb, wl("ssd_conv_b", l).re("(c p) -> p c", p=128), allow_slow_non_contiguous=True)
            negA_g = bload(es, "negA_g", wl("gdn_a_log", l), 4)
            kb.act(negA_g, negA_g, AF.Exp)
            kb.ts("vector", negA_g, negA_g, -1.0, None, ALU.mult)
            negA_s = bload(es, "negA_s", wl("ssd_a_log", l), 4)
            kb.act(negA_s, negA_s, AF.Exp)
            kb.ts("vector", negA_s, negA_s, -1.0, None, ALU.mult)
            dtb_g = bload(es, "dtb_g", wl("gdn_dt_bias", l), 4)
            dtb_s = bload(es, "dtb_s", wl("ssd_dt_bias", l), 4)
            dsk = bload(es, "dsk", wl("ssd_d", l), 4)
            ibias = bload(es, "ibias", wl("mlstm_i_bias", l), 4)
            fbias = bload(es, "fbias", wl("mlstm_f_bias", l), 4)
            gng = bload(es, "gng", wl("gdn_norm_g", l), 64)
            gssd = bload(es, "gssd", wl("ssd_norm_g", l), 256)
            gml = bload(es, "gml", wl("mlstm_norm_g", l), 256)

            TRI, SU, ONESBD, BO = cs["c_tri"], cs["c_su"], cs["c_onesbd"], cs["c_bo"]
            NI4 = sb("NI4", [128, 512])
            kb.ts("vector", NI4, cs["c_ident4"], -1.0, 1.0, ALU.mult, ALU.add)
            MBT4, ID4, CHI, CHJ = cs["c_mbt4"], cs["c_ident4"], cs["c_chi"], cs["c_chj"]

            xt = sb("xt", [128, D])
            xn = sb("xn", [128, D], BF16)
            junk = sb("junk", [128, D], BF16)
            hTs = [sb("hT", [128, 8, 128], BF16) for i in range(2)]
            ss = sb("ss", [128, 1]); rs = sb("rs", [128, 1]); t1s = sb("t1s", [128, 1])
            mixos = [sb("mixo", [128, 768], BF16) for i in range(2)]
            ptp = kb.ps(es, "ptp", [128, 1024], BF16)

            def bfv(bank):
                return V(bank.ap.bitcast(BF16), bank.r)

            v3 = lambda v, h=4: v.re("p (h d) -> p h d", h=h)
            hs = lambda h: slice(h * 128, (h + 1) * 128)
            hp = lambda h: slice((h % 2) * 64, (h % 2) * 64 + 64)

            def proj_fm(pout, col0, hT, M=128):
                for k in range(8):
                    kb.mm(pout, winb[:, k, col0:col0 + M], hT[:, k, :], start=(k == 0), stop=(k == 7))

            def proj_tm(pout, col0, n, hT):
                for k in range(8):
                    kb.mm(pout, hT[:, k, :], winb[:, k, col0:col0 + n], start=(k == 0), stop=(k == 7))

            def conv_chunk(praw, raw, w, bias, ac, outv):
                kb.cp("scalar", raw[:, 3:131], praw)
                if bias is None:
                    kb.ts("vector", ac, raw[:, 3:131], w[:, 3:4], None, ALU.mult)
                else:
                    kb.ts("vector", ac, raw[:, 3:131], w[:, 3:4], bias, ALU.mult, ALU.add)
                for j in (2, 1, 0):
                    kb.stt(ac, raw[:, j:j + 128], w[:, j:j + 1], ac, ALU.mult, ALU.add)
                kb.cp("gpsimd", raw[:, 0:3], raw[:, 128:131])
                kb.act(outv, ac, AF.Silu)

            def decay_prep(glog, G_all, psmall, pbig):
                kb.mm(psmall[:, 0:4], TRI, glog)
                kb.mm(psmall[:, 4:8], SU, glog)
                kb.mm(psmall[:, 8:12], ONESBD, glog)
                kb.tt("vector", v3(G_all), TRI.bc(1, [128, 4, 128]), glog.bc(2, [128, 4, 128]), ALU.mult)
                kb.mm(pbig, SU, G_all, start=True, stop=False)
                kb.mm(pbig, ident, MBT4, start=False, stop=True)

            class G:
                pass
            g = G()
            g.A = kb.ps(es, "gA", [128, 512], F32); g.B = kb.ps(es, "gB", [128, 512], F32); g.C = kb.ps(es, "gC", [128, 512], F32)
            g.raw = sb("rawG", [128, 6, 131]); kb.memset("vector", g.raw, 0.0)
            g.acc = [sb("gacc", [128, 128]) for i in range(2)]
            g.gF = sb("gF", [128, 4, 128]); g.sq4 = sb("sq4", [128, 4, 128]); g.rn4 = sb("rn4", [128, 4, 128])
            g.qn = sb("qn", [128, 2, 128], BF16); g.kn = sb("kn", [128, 2, 128], BF16); g.vFb = sb("vFb", [128, 2, 128], BF16)
            g.sgate = sb("sgate", [128, 256]); g.beta = sb("beta", [128, 4]); g.a4 = sb("ga4", [128, 4]); g.glog = sb("gglog", [128, 4])
            g.cbg = sb("cbg", [128, 4]); g.E = sb("gE", [128, 12]); g.G_all = sb("gG_all", [128, 512]); g.DT_all = sb("gDT_all", [128, 512])
            g.Kbg = sb("Kbg", [128, 256]); g.Vb = sb("Vb", [128, 256]); g.kdec = sb("gkdec", [128, 256], BF16)
            g.Lt = sb("Lt", [128, 512])
            g.P = [sb("P", [128, 512]) for j in range(6)]
            g.PT = [sb("PT", [128, 512]) for j in range(5)]
            g.Y = sb("Y", [128, 512])
            g.U_sb = sb("U_sb", [128, 256]); g.WT_sb = sb("WT_sb", [128, 2, 128], BF16); g.attnT = sb("attnT", [128, 512], BF16)
            g.g_rep = sb("gg_rep", [128, 256]); g.eP = sb("geP", [128, 4]); g.vnew = sb("vnew", [128, 256], BF16)
            g.S32 = sb("S32", [128, 2, 64]); g.Sbf = sb("Sbf", [128, 2, 64], BF16)
            kb.memset("vector", g.S32, 0.0); kb.memset("vector", g.Sbf, 0.0)
            g.t1 = sb("gt1", [128, 256]); g.o_ = sb("go_", [128, 256]); g.sq = sb("gsq", [128, 256])
            g.ss4 = sb("gss4", [128, 4]); g.rs4 = sb("grs4", [128, 4]); g.tm4 = sb("gtm4", [128, 4])

            def gdn_block(b, hT, mixo):
                A, B, C = g.A, g.B, g.C
                Bb = bfv(B)
                gF, kn, qn, vFb, beta, glog, E, DT_all, P, PT, Y = g.gF, g.kn, g.qn, g.vFb, g.beta, g.glog, g.E, g.DT_all, g.P, g.PT, g.Y
                for c in range(6):
                    pj = A if c % 2 == 0 else B
                    proj_fm(pj[:, 0:128], c * 128, hT)
                    outv = gF[:, c, :] if c < 4 else vFb[:, c - 4, :]
                    conv_chunk(pj[:, 0:128], g.raw[:, c, :], gcw[:, c, :], None, g.acc[c % 2], outv)
                kb.tt("gpsimd", g.sq4, gF, gF, ALU.mult)
                for i in range(4):
                    kb.mm(A[:, i * 128:(i + 1) * 128], BO, g.sq4[:, i, :])
                rn4f = g.rn4.re("p a b -> p (a b)")
                kb.act(rn4f, A, AF.Ln, bias=epsv)
                kb.act(rn4f, rn4f, AF.Exp, scale=-0.5)
                kb.stt(qn, gF[:, 0:2, :], 0.125, g.rn4[:, 0:2, :], ALU.mult, ALU.mult)
                kb.tt("vector", kn, gF[:, 2:4, :], g.rn4[:, 2:4, :], ALU.mult)
                kb.tp(Bb[:, 0:128], kn[:, 0, :], identb)
                kb.tp(Bb[:, 128:256], kn[:, 1, :], identb)
                kb.tp(Bb[:, 256:384], vFb[:, 0, :], identb)
                kb.tp(Bb[:, 384:512], vFb[:, 1, :], identb)
                kTM = v3(Bb[:, 0:256]); vTM = v3(Bb[:, 256:512])
                proj_tm(A[:, 0:264], 768, 264, hT)
                kb.act(g.sgate, A[:, 0:256], AF.Silu)
                kb.act(beta, A[:, 256:260], AF.Sigmoid)
                kb.tt("vector", g.a4, A[:, 260:264], dtb_g, ALU.add)
                kb.act(g.a4, g.a4, AF.Exp)
                kb.act(g.a4, g.a4, AF.Ln, bias=1.0)
                kb.tt("vector", glog, g.a4, negA_g, ALU.mult)
                decay_prep(glog, g.G_all, C, A)
                kb.act(E, C[:, 0:12], AF.Exp)
                kb.act(DT_all, A, AF.Exp)
                kb.tt("vector", g.cbg, beta, E[:, 0:4], ALU.mult)
                kb.tt("vector", v3(g.Kbg), kTM, g.cbg.bc(2, [128, 4, 64]), ALU.mult)
                kb.tt("vector", v3(g.Vb), vTM, beta.bc(2, [128, 4, 64]), ALU.mult)
                kb.tt("vector", v3(g.kdec), kTM, E[:, 4:8].bc(2, [128, 4, 64]), ALU.mult)
                for h in range(4):
                    kb.mm(B[:, hs(h)], kn[hp(h), h // 2, :], kn[hp(h), h // 2, :])
                kb.tt("vector", g.Lt, B, DT_all, ALU.mult)
                kb.tt("gpsimd", g.Lt, g.Lt, NI4, ALU.mult)
                for h in range(4):
                    kb.tp(C[:, hs(h)], g.Lt[:, hs(h)], ident)
                kb.tt("vector", v3(P[0]), v3(C), beta.bc(2, [128, 4, 128]), ALU.mult)
                for h in range(4):
                    kb.tp(A[:, hs(h)], P[0][:, hs(h)], ident)
                kb.cp("scalar", PT[0], A)
                for j in range(5):
                    for h in range(4):
                        kb.mm(B[:, hs(h)], PT[j][:, hs(h)], P[j][:, hs(h)])
                    if j < 4:
                        for h in range(4):
                            kb.mm(C[:, hs(h)], P[j][:, hs(h)], PT[j][:, hs(h)])
                    kb.cp("scalar", P[j + 1], B)
                    if j < 4:
                        kb.cp("vector", PT[j + 1], C)
                kb.tt("vector", Y, ID4, PT[0], ALU.subtract)
                for j in range(1, 6):
                    for h in range(4):
                        kb.mm(A[:, hs(h)], P[j][:, hs(h)], Y[:, hs(h)])
                    kb.tt("vector", Y, Y, A, ALU.add)
                for h in range(4):
                    kb.mm(B[:, h * 64:(h + 1) * 64], Y[:, hs(h)], g.Vb[:, h * 64:(h + 1) * 64])
                for h in range(4):
                    kb.mm(C[hp(h), (h // 2) * 128:(h // 2) * 128 + 128], g.Kbg[:, h * 64:(h + 1) * 64], Y[:, hs(h)])
                kb.cp("scalar", g.U_sb, B[:, 0:256])
                kb.cp("vector", g.WT_sb.re("p a b -> p (a b)"), C[:, 0:256])
                for h in range(4):
                    kb.mm(A[:, hs(h)], kn[hp(h), h // 2, :], qn[hp(h), h // 2, :])
                kb.tt("vector", g.attnT, A, DT_all, ALU.mult)
                kb.cp("gpsimd", v3(g.g_rep), glog.bc(2, [128, 4, 64]))
                for pair in range(2):
                    kb.mm(B[:, pair * 2:pair * 2 + 2], g.g_rep[:, pair * 128:(pair + 1) * 128], CHI)
                kb.act(g.eP, B[:, 0:4], AF.Exp)
                for j in range(2):
                    r = slice(j * 64, j * 64 + 64)
                    for h in range(4):
                        kb.mm(C[r, h * 64:(h + 1) * 64], g.WT_sb[hp(h), h // 2, r], g.Sbf[hp(h), h // 2, :])
                    kb.tt("vector", g.vnew[r, :], g.U_sb[r, :], C[r, 0:256], ALU.subtract)
                    for h in range(4):
                        kb.mm(A[r, h * 64:(h + 1) * 64], qn[hp(h), h // 2, r], g.Sbf[hp(h), h // 2, :])
                    for h in range(4):
                        kb.mm(B[hp(h), (h // 2) * 64:(h // 2) * 64 + 64], g.kdec[r, h * 64:(h + 1) * 64], g.vnew[r, h * 64:(h + 1) * 64])
                    for pair in range(2):
                        kb.stt(g.S32[:, pair, :], g.S32[:, pair, :], g.eP[:, pair * 2 + j:pair * 2 + j + 1],
                               B[:, pair * 64:(pair + 1) * 64], ALU.mult, ALU.add)
                    kb.cp("scalar", g.Sbf, g.S32)
                for h in range(4):
                    kb.mm(C[:, h * 64:(h + 1) * 64], g.attnT[:, hs(h)], g.vnew[:, h * 64:(h + 1) * 64])
                kb.tt("vector", v3(g.t1), v3(A[:, 0:256]), E[:, 0:4].bc(2, [128, 4, 64]), ALU.mult)
                kb.tt("vector", g.o_, g.t1, C[:, 0:256], ALU.add)
                kb.tt("gpsimd", g.sq, g.o_, g.o_, ALU.mult)
                kb.red(g.ss4, v3(g.sq))
                kb.rstd(g.rs4, g.ss4, 1.0 / 64, epsv, g.tm4)
                kb.tt("vector", v3(g.o_), v3(g.o_), g.rs4.bc(2, [128, 4, 64]), ALU.mult)
                kb.tt("gpsimd", v3(g.o_), v3(g.o_), gng.bc(1, [128, 4, 64]), ALU.mult)
                kb.tt("vector", mixo[:, 0:256], g.o_, g.sgate, ALU.mult)

            s_ = G()
            s_.A = kb.ps(es, "sA", [128, 512], F32); s_.B = kb.ps(es, "sB", [128, 512], F32)
            s_.raw = sb("rawS", [128, 6, 131]); kb.memset("vector", s_.raw, 0.0)
            s_.acc = [sb("sacc", [128, 128]) for i in range(2)]
            s_.sF = sb("sF", [128, 6, 128], BF16)
            s_.xs_tm = sb("xs_tm", [128, 256]); s_.B_tm = sb("B_tm", [128, 256], BF16)
            s_.sz = sb("sz", [128, 256]); s_.a4 = sb("sa4", [128, 4]); s_.dt4 = sb("dt4", [128, 4]); s_.cdt = sb("cdt", [128, 4])
            s_.glog = sb("sglog", [128, 4]); s_.E = sb("sE", [128, 12]); s_.G_all = sb("sG_all", [128, 512]); s_.DT_all = sb("sDT_all", [128, 512])
            s_.etotN = sb("etotN", [128, 8])
            s_.xdt = sb("xdt", [128, 256], BF16); s_.xdec = sb("xdec", [128, 256], BF16); s_.xskip = sb("xskip", [128, 256])
            s_.MT = sb("MT", [128, 512], BF16); s_.y_d = sb("y_d", [128, 256])
            s_.ST32 = sb("ST32", [128, 256]); s_.STbf = sb("STbf", [128, 256], BF16); s_.tmpS = sb("tmpS", [128, 256])
            kb.memset("vector", s_.ST32, 0.0); kb.memset("vector", s_.STbf, 0.0)
            s_.t1 = sb("st1", [128, 256]); s_.y_ = sb("y_", [128, 256]); s_.sq = sb("ssq", [128, 256])
            s_.ss2 = sb("ss2", [128, 2]); s_.rs2 = sb("rs2", [128, 2]); s_.tm2 = sb("tm2", [128, 2])

            def ssd_block(b, hT, mixo):
                A, B = s_.A, s_.B
                Ab = bfv(A)
                sF, E, DT_all, glog = s_.sF, s_.E, s_.DT_all, s_.glog
                for c in range(6):
                    pj = A if c % 2 == 0 else B
                    proj_fm(pj[:, 0:128], 2056 + c * 128, hT)
                    conv_chunk(pj[:, 0:128], s_.raw[:, c, :], scw[:, c, :], scb[:, c:c + 1], s_.acc[c % 2], sF[:, c, :])
                for i in range(4):
                    kb.tp(Ab[:, i * 128:(i + 1) * 128], sF[:, i, :], identb)
                kb.cp("scalar", s_.xs_tm, Ab[:, 0:256])
                kb.cp("vector", s_.B_tm, Ab[:, 256:512])
                proj_tm(B[:, 0:256], 1800, 256, hT)
                proj_tm(B[:, 256:260], 2824, 4, hT)
                kb.act(s_.sz, B[:, 0:256], AF.Silu)
                kb.tt("vector", s_.a4, B[:, 256:260], dtb_s, ALU.add)
                kb.act(s_.a4, s_.a4, AF.Exp)
                kb.act(s_.dt4, s_.a4, AF.Ln, bias=1.0)
                kb.tt("vector", glog, s_.dt4, negA_s, ALU.mult)
                decay_prep(glog, s_.G_all, B, A)
                for j in range(2):
                    kb.mm(B[:, 16 + j * 4:16 + (j + 1) * 4], CHJ[:, j, :], glog)
                kb.act(E, B[:, 0:12], AF.Exp)
                kb.act(s_.etotN, B[:, 16:24], AF.Exp)
                kb.act(DT_all, A, AF.Exp)
                xsTM = v3(s_.xs_tm)
                kb.tt("vector", s_.cdt, s_.dt4, E[:, 4:8], ALU.mult)
                kb.tt("vector", v3(s_.xdt), xsTM, s_.dt4.bc(2, [128, 4, 64]), ALU.mult)
                kb.tt("gpsimd", v3(s_.xdec), xsTM, s_.cdt.bc(2, [128, 4, 64]), ALU.mult)
                kb.tt("gpsimd", v3(s_.xskip), xsTM, dsk.bc(2, [128, 4, 64]), ALU.mult)
                for gi in range(2):
                    kb.mm(A[:, gi * 128:(gi + 1) * 128], sF[:, 2 + gi, :], sF[:, 4 + gi, :])
                MT3 = v3(s_.MT); DT3 = v3(DT_all)
                for gi in range(2):
                    kb.tt("vector", MT3[:, 2 * gi:2 * gi + 2, :], A[:, gi * 128:(gi + 1) * 128].bc(1, [128, 2, 128]),
                          DT3[:, 2 * gi:2 * gi + 2, :], ALU.mult)
                for h in range(4):
                    kb.mm(B[:, h * 64:(h + 1) * 64], s_.MT[:, hs(h)], s_.xdt[:, h * 64:(h + 1) * 64])
                kb.cp("scalar", s_.y_d, B[:, 0:256])
                for j in range(2):
                    r = slice(j * 64, j * 64 + 64)
                    for h in range(4):
                        kb.mm(A[r, h * 64:(h + 1) * 64], sF[:, 4 + h // 2, r], s_.STbf[:, h * 64:(h + 1) * 64])
                    for h in range(4):
                        kb.mm(B[:, h * 64:(h + 1) * 64], s_.B_tm[r, (h // 2) * 128:(h // 2) * 128 + 128], s_.xdec[r, h * 64:(h + 1) * 64])
                    kb.tt("gpsimd", v3(s_.tmpS), v3(s_.ST32), s_.etotN[:, j * 4:(j + 1) * 4].bc(2, [128, 4, 64]), ALU.mult)
                    kb.tt("vector", s_.ST32, s_.tmpS, B[:, 0:256], ALU.add)
                    kb.cp("scalar", s_.STbf, s_.ST32)
                kb.tt("vector", v3(s_.t1), v3(A[:, 0:256]), E[:, 0:4].bc(2, [128, 4, 64]), ALU.mult)
                kb.tt("gpsimd", s_.y_, s_.t1, s_.y_d, ALU.add)
                kb.tt("gpsimd", s_.y_, s_.y_, s_.xskip, ALU.add)
                kb.tt("vector", s_.y_, s_.y_, s_.sz, ALU.mult)
                kb.tt("gpsimd", s_.sq, s_.y_, s_.y_, ALU.mult)
                kb.red(s_.ss2, v3(s_.sq, 2))
                kb.rstd(s_.rs2, s_.ss2, 1.0 / 128, epsv, s_.tm2)
                kb.tt("vector", v3(s_.y_, 2), v3(s_.y_, 2), s_.rs2.bc(2, [128, 2, 128]), ALU.mult)
                kb.tt("vector", mixo[:, 256:512], s_.y_, gssd, ALU.mult)

            m_ = G()
            m_.A = kb.ps(es, "mA", [128, 512], F32); m_.B = kb.ps(es, "mB", [128, 512], F32)
            m_.mqk = sb("mqk", [128, 4, 128], BF16); m_.kv = sb("kv_tm", [128, 512])
            m_.so = sb("so", [128, 256]); m_.li4 = sb("li4", [128, 4]); m_.eli8 = sb("eli8", [128, 4]); m_.f4 = sb("f4", [128, 4])
            m_.glog = sb("mglog", [128, 4]); m_.E = sb("mE", [128, 12]); m_.G_all = sb("mG_all", [128, 512]); m_.DT_all = sb("mDT_all", [128, 512])
            m_.vli = sb("vli", [128, 4, 66], BF16); m_.kdec = sb("mkdec", [128, 256], BF16); m_.STm = sb("STm", [128, 512], BF16)
            m_.intra = sb("intra", [128, 272])
            m_.g_rep = sb("mg_rep", [128, 256]); m_.eP = sb("meP", [128, 4])
            m_.CS32 = sb("CS32", [128, 2, 66]); m_.CSbf = sb("CSbf", [128, 2, 66], BF16)
            kb.memset("vector", m_.CS32, 0.0); kb.memset("vector", m_.CSbf, 0.0)
            m_.t2 = sb("t2", [128, 4, 65]); m_.d4 = sb("d4", [128, 4]); m_.hm = sb("hm", [128, 256]); m_.sq = sb("msq", [128, 256])
            m_.ss4 = sb("mss4", [128, 4]); m_.rs4 = sb("mrs4", [128, 4]); m_.tm4 = sb("mtm4", [128, 4])

            def mlstm_block(b, hT, mixo):
                A, B = m_.A, m_.B
                mqk, E, DT_all, glog, vli, t2, hm = m_.mqk, m_.E, m_.DT_all, m_.glog, m_.vli, m_.t2, m_.hm
                for i in range(2):
                    proj_fm(A[:, i * 128:(i + 1) * 128], 2828 + i * 128, hT)
                    proj_fm(A[:, 256 + i * 128:256 + (i + 1) * 128], 3084 + i * 128, hT)
                kb.cp("scalar", mqk.re("p a b -> p (a b)"), A)
                proj_tm(B, 3084, 512, hT)
                kb.cp("scalar", m_.kv, B)
                proj_tm(A[:, 0:264], 3596, 264, hT)
                kb.act(m_.so, A[:, 0:256], AF.Sigmoid)
                kb.tt("vector", m_.li4, A[:, 256:260], ibias, ALU.add)
                kb.act(m_.eli8, m_.li4, AF.Exp)
                kb.ts("vector", m_.eli8, m_.eli8, 0.125, None, ALU.mult)
                kb.tt("vector", m_.f4, A[:, 260:264], fbias, ALU.add)
                kb.act(m_.f4, m_.f4, AF.Exp, scale=-1.0)
                kb.act(m_.f4, m_.f4, AF.Ln, bias=1.0)
                kb.ts("vector", glog, m_.f4, -1.0, None, ALU.mult)
                decay_prep(glog, m_.G_all, A, B)
                kb.act(E, A[:, 0:12], AF.Exp)
                kb.act(DT_all, B, AF.Exp)
                kb.tt("gpsimd", vli[:, :, 0:64], v3(m_.kv[:, 256:512]), m_.eli8.bc(2, [128, 4, 64]), ALU.mult)
                kb.cp("vector", vli[:, :, 64:65], m_.eli8.bc(2, [128, 4, 1]))
                kb.tt("gpsimd", v3(m_.kdec), v3(m_.kv[:, 0:256]), E[:, 4:8].bc(2, [128, 4, 64]), ALU.mult)
                for h in range(4):
                    kb.mm(A[:, hs(h)], mqk[hp(h), 2 + h // 2, :], mqk[hp(h), h // 2, :])
                kb.tt("vector", m_.STm, A, DT_all, ALU.mult)
                for h in range(4):
                    kb.mm(B[:, h * 68:h * 68 + 65], m_.STm[:, hs(h)], vli[:, h, 0:65])
                kb.cp("scalar", m_.intra, B[:, 0:272])
                kb.cp("gpsimd", v3(m_.g_rep), glog.bc(2, [128, 4, 64]))
                for pair in range(2):
                    kb.mm(A[:, pair * 2:pair * 2 + 2], m_.g_rep[:, pair * 128:(pair + 1) * 128], CHI)
                kb.act(m_.eP, A[:, 0:4], AF.Exp)
                for j in range(2):
                    r = slice(j * 64, j * 64 + 64)
                    for h in range(4):
                        kb.mm(A[r, h * 68:h * 68 + 65], mqk[hp(h), h // 2, r], m_.CSbf[hp(h), h // 2, 0:65])
                    for h in range(4):
                        kb.mm(B[hp(h), (h // 2) * 68:(h // 2) * 68 + 65], m_.kdec[r, h * 64:(h + 1) * 64], vli[r, h, 0:65])
                    for pair in range(2):
                        kb.stt(m_.CS32[:, pair, 0:65], m_.CS32[:, pair, 0:65], m_.eP[:, pair * 2 + j:pair * 2 + j + 1],
                               B[:, pair * 68:pair * 68 + 65], ALU.mult, ALU.add)
                    kb.cp("scalar", m_.CSbf, m_.CS32)
                kb.tt("vector", t2, A[:, 0:272].re("p (h d) -> p h d", h=4)[:, :, 0:65], E[:, 0:4].bc(2, [128, 4, 65]), ALU.mult)
                kb.tt("vector", t2, t2, m_.intra.re("p (h d) -> p h d", h=4)[:, :, 0:65], ALU.add)
                kb.tt("vector", m_.d4, t2[:, :, 64], t2[:, :, 64], ALU.mult)
                kb.ts("vector", m_.d4, m_.d4, 1.0, None, ALU.max)
                kb.act(m_.d4, m_.d4, AF.Ln)
                kb.act(m_.d4, m_.d4, AF.Exp, scale=-0.5)
                kb.tt("vector", v3(hm), t2[:, :, 0:64], m_.d4.bc(2, [128, 4, 64]), ALU.mult)
                kb.tt("gpsimd", hm, hm, m_.so, ALU.mult)
                kb.tt("gpsimd", m_.sq, hm, hm, ALU.mult)
                kb.red(m_.ss4, v3(m_.sq))
                kb.rstd(m_.rs4, m_.ss4, 1.0 / 64, epsv, m_.tm4)
                kb.tt("vector", v3(hm), v3(hm), m_.rs4.bc(2, [128, 4, 64]), ALU.mult)
                kb.tt("vector", mixo[:, 512:768], hm, gml, ALU.mult)

            def pre_block(b):
                r0 = b * 128
                kb.dma("sync", xt, xs[r0:r0 + 128, :])
                prenorm_T(xt, g1b, xn, hTs[b % 2], ptp, ss, rs, t1s, junk)

            pre_block(0)
            for b in range(nblk):
                r0 = b * 128
                hT = hTs[b % 2]
                mixo = mixos[b % 2]
                lists = []
                for fn in (gdn_block, ssd_block, mlstm_block):
                    S.rec = []
                    fn(b, hT, mixo)
                    lists.append(S.rec)
                    S.rec = None
                if b + 1 < nblk:
                    S.rec = []
                    pre_block(b + 1)
                    lists.append(S.rec)
                    S.rec = None
                S.interleave(lists)
                kb.dma("sync", mixA[r0:r0 + 128, :], mixo)
            S.barrier()

    def mixb_phase(l, xs):
        lambda_init = 0.8 - 0.6 * math.exp(-0.3 * l)
        with ExitStack() as es:
            sb = lambda n, shp, dt=F32: kb.sb(es, n, shp, dt)
            g1b = bload(es, "g1b", wl("mix_pre_g", l), D)
            g2b = bload(es, "g2b", wl("mix_post_g", l), D)
            gdf = bload(es, "gdf", wl("diff_norm_g", l), 64, scale=(1.0 - lambda_init))
            winb = sb("winb", [128, 8, 768], BF16)
            wsrc = Wb[(l, "w_in")].re("(k p) n -> p k n", p=128)
            woutb = sb("woutb", [128, 8, D], BF16)
            wosrc = Wb[(l, "w_out")].re("(k p) n -> p k n", p=128)
            for k in range(8):
                kb.dma("sync", winb[:, k, :], wsrc[:, k, 1032:1800])
                kb.dma("sync", woutb[:, k, :], wosrc[:, k, :])
            lq1 = bload(es, "lq1", wl("diff_lam_q1", l), 32); lk1 = bload(es, "lk1", wl("diff_lam_k1", l), 32)
            lq2 = bload(es, "lq2", wl("diff_lam_q2", l), 32); lk2 = bload(es, "lk2", wl("diff_lam_k2", l), 32)
            lam2 = sb("lam2", [128, 2]); neglam = sb("neglam", [128, 1])
            kb.tt("vector", lq1, lq1, lk1, ALU.mult)
            kb.tt("vector", lq2, lq2, lk2, ALU.mult)
            kb.red(lam2[:, 0:1], lq1)
            kb.red(lam2[:, 1:2], lq2)
            kb.act(lam2, lam2, AF.Exp)
            kb.tt("vector", neglam, lam2[:, 1:2], lam2[:, 0:1], ALU.subtract)
            kb.ts("vector", neglam, neglam, -lambda_init, None, ALU.add)

            xt = sb("xt", [128, D]); xn = sb("xn", [128, D], BF16); junk = sb("junk", [128, D], BF16)
            tmp = sb("tmp", [128, D])
            hT = sb("hT", [128, 8, 128], BF16)
            ss = sb("ss", [128, 1]); rs = sb("rs", [128, 1]); t1s = sb("t1s", [128, 1]); q2 = sb("q2", [128, 2])
            KT = sb("KT", [64, 4, S_LEN], BF16)
            Vaug = sb("Vaug", [128, NB, 4, 66], BF16)
            kb.memset("vector", Vaug.re("p a b c -> p (a b c)"), 1.0)
            cosb = sb("cosb", [64, 128]); sinb = sb("sinb", [64, 128])
            qraw = sb("qraw", [64, 512], BF16)
            r1 = sb("r1", [64, 512]); r2 = sb("r2", [64, 512])
            qr = sb("qr", [64, 4, 128], BF16)
            PTt = [sb("PTt", [128, 512], BF16) for i in range(2)]
            rec8 = sb("rec8", [128, 8]); on = sb("on", [128, 8, 64]); od = sb("od", [128, 256]); sq = sb("sq", [128, 256])
            ss4 = sb("ss4", [128, 4]); rs4 = sb("rs4", [128, 4]); tm4 = sb("tm4", [128, 4])
            mixed = sb("mixed", [128, D], BF16); mT = sb("mT", [128, 8, 128], BF16)
            mdbg = sb("mdbg", [128, D]) if dbg else None
            pT = kb.ps(es, "pT", [128, 1024], BF16)
            pq = kb.ps(es, "pq", [128, 512], F32)
            pS = [kb.ps(es, "pS", [128, 512], F32) for i in range(2)]
            pA = [kb.ps(es, "pA", [128, 512], F32) for i in range(2)]
            pY = [kb.ps(es, "pY", [128, 512], F32) for i in range(2)]
            v3 = lambda v, h=4: v.re("p (h d) -> p h d", h=h)
            sc = 32.0 ** -0.5
            nexp = [0]

            for b in range(nblk):
                r0 = b * 128
                kb.dma("sync", xt, xs[r0:r0 + 128, :])
                kb.dma("sync", cosb, C["c_cos"][:, r0:r0 + 128])
                kb.dma("sync", sinb, C["c_sin"][:, r0:r0 + 128])
                kb.dma("sync", mixed[:, 0:256], mixA[r0:r0 + 128, 0:256])
                kb.dma("sync", mixed[:, 512:1024], mixA[r0:r0 + 128, 256:768])
                prenorm_T(xt, g1b, xn, hT, pT, ss, rs, t1s, junk)
                for which in range(2):
                    for h in range(4):
                        for k in range(8):
                            kb.mm(pq[0:64, h * 128:(h + 1) * 128], winb[:, k, which * 256 + h * 64:which * 256 + (h + 1) * 64],
                                  hT[:, k, :], start=(k == 0), stop=(k == 7))
                    kb.cp("scalar", qraw, pq[0:64, :])
                    kb.mm(pS[0][0:64, :], protb, qraw)
                    kb.tt("vector", v3(r1), v3(qraw), cosb.bc(1, [64, 4, 128]), ALU.mult)
                    kb.tt("vector", v3(r2), v3(pS[0][0:64, :]), sinb.bc(1, [64, 4, 128]), ALU.mult)
                    dst = qr if which == 0 else KT[:, :, r0:r0 + 128]
                    kb.tt("vector", dst, v3(r1), v3(r2), ALU.add)
                for k in range(8):
                    kb.mm(pq[:, 0:256], hT[:, k, :], winb[:, k, 512:768], start=(k == 0), stop=(k == 7))
                kb.cp("scalar", Vaug[:, b, :, 0:64], v3(pq[:, 0:256]))
                kb.memset("vector", pA[0], 0.0)
                kb.memset("vector", pA[1], 0.0)
                for g0 in range(0, b + 1, 4):
                    kbs = list(range(g0, min(b + 1, g0 + 4)))
                    n = len(kbs)
                    for hmi in range(8):
                        h, mp = hmi // 2, hmi % 2
                        ps_ = pS[nexp[0] % 2]
                        pt_ = PTt[nexp[0] % 2]
                        nexp[0] += 1
                        for i, kbi in enumerate(kbs):
                            kb.mm(ps_[:, i * 128:(i + 1) * 128], KT[mp * 32:(mp + 1) * 32, h, kbi * 128:(kbi + 1) * 128],
                                  qr[mp * 32:(mp + 1) * 32, h, :])
                        kb.act(pt_[:, 0:n * 128], ps_[:, 0:n * 128], AF.Exp, scale=sc)
                        if kbs[-1] == b:
                            i = n - 1
                            kb.memset("gpsimd", pt_[64:128, i * 128:i * 128 + 64], 0.0)
                        for i, kbi in enumerate(kbs):
                            kb.mm(pA[hmi // 4][:, (hmi % 4) * 68:(hmi % 4) * 68 + 65], pt_[:, i * 128:(i + 1) * 128],
                                  Vaug[:, kbi, h, 0:65], start=False, stop=(kbi == b), skip=True)
                for i in range(2):
                    a3 = pA[i][:, 0:272].re("p (h d) -> p h d", h=4)
                    kb.recip(rec8[:, i * 4:(i + 1) * 4], a3[:, :, 64])
                    kb.tt("vector", on[:, i * 4:(i + 1) * 4, :], a3[:, :, 0:64], rec8[:, i * 4:(i + 1) * 4].bc(2, [128, 4, 64]), ALU.mult)
                on4 = on.re("p (h m) d -> p h m d", m=2)
                kb.stt(v3(od), on4[:, :, 1, :], neglam[:, 0:1], on4[:, :, 0, :], ALU.mult, ALU.add)
                kb.tt("gpsimd", sq, od, od, ALU.mult)
                kb.red(ss4, v3(sq))
                kb.rstd(rs4, ss4, 1.0 / 64, epsv, tm4)
                kb.tt("vector", v3(od), v3(od), rs4.bc(2, [128, 4, 64]), ALU.mult)
                kb.tt("vector", v3(mixed[:, 256:512]), v3(od), gdf.bc(1, [128, 4, 64]), ALU.mult)
                if dbg:
                    kb.cp("vector", mdbg, mixed)
                    kb.dma("sync", dbg_mixed[r0:r0 + 128, :], mdbg)
                for k in range(8):
                    kb.tp(pT[:, k * 128:(k + 1) * 128], mixed[:, k * 128:(k + 1) * 128], identb)
                kb.cp("scalar", mT, pT.re("p (k n) -> p k n", k=8))
                for c in range(2):
                    for k in range(8):
                        kb.mm(pY[c], mT[:, k, :], woutb[:, k, c * 512:(c + 1) * 512], start=(k == 0), stop=(k == 7))
                post_res(pY[0], pY[1], xt, g2b, tmp, q2, rs, t1s, junk)
                kb.dma("sync", xs[r0:r0 + 128, :], xt)
            S.barrier()

    if "ffn1" not in stages:
        for r0 in range(0, S_LEN, 512):
            kb.dma("sync", out[r0:r0 + 512, :], x_in[r0:r0 + 512, :])
    for l in range(nlayers):
        src = x_in if l == 0 else out
        if "ffn1" in stages:
            ffn_phase(l, 1, src, out)
        if "mixa" in stages:
            mixa_phase(l, out)
        if "mixb" in stages:
            mixb_phase(l, out)
        if "ffn2" in stages:
            ffn_phase(l, 2, out, out)
    S.wait_res("sync", [out.r] + ([dbg_mixed.r] if dbg else []))
    es0.close()
    return nc, hc


_CACHE = {}


def kernel(**inputs):
    x = np.ascontiguousarray(np.asarray(inputs["x"], dtype=np.float32))
    nb = x.shape[0]
    if "nc" not in _CACHE:
        _CACHE["nc"] = build()
    nc, hc = _CACHE["nc"]
    shared = {n: np.ascontiguousarray(np.asarray(inputs[n], dtype=np.float32)) for n, _ in W_SPECS}
    shared.update(hc)
    in_maps = []
    for b in range(nb):
        m = dict(shared)
        m["x"] = x[b]
        in_maps.append(m)
    res = run_bass_kernel_spmd(nc, in_maps, core_ids=list(range(nb)))
    return np.stack([np.asarray(r["out"], dtype=np.float32) for r in res.results], axis=0)
```

```python
import math
from contextlib import ExitStack

import numpy as np
import ml_dtypes
import concourse.bass as bass
import concourse.mybir as mybir
from concourse.bass_utils import run_bass_kernel_spmd

F32 = mybir.dt.float32
BF16 = mybir.dt.bfloat16
AF = mybir.ActivationFunctionType
ALU = mybir.AluOpType
AX = mybir.AxisListType

S_LEN = 4096
D = 1024
DFF = 2816
INC = 3860
NB = S_LEN // 128
EPS = 1e-6
NEG = -30000.0


class Res:
    __slots__ = ("lw", "rd", "excl", "pe")

    def __init__(self):
        self.lw = None
        self.rd = []
        self.excl = False
        self.pe = None


class V:
    __slots__ = ("ap", "r")

    def __init__(self, ap, r=None):
        self.ap = ap
        self.r = r if r is not None else Res()

    def __getitem__(self, k):
        return V(self.ap[k], self.r)

    def re(self, pat, **kw):
        return V(self.ap.rearrange(pat, **kw), self.r)

    def bc(self, axis, shape):
        return V(self.ap.unsqueeze(axis).broadcast_to(list(shape)), self.r)


class Sched:
    ENG = ("tensor", "vector", "scalar", "gpsimd", "sync")
    NSLOT = 8

    def __init__(self, nc, es):
        self.nc = nc
        self.eng = {e: getattr(nc, e) for e in self.ENG}
        self.sem = {e: es.enter_context(nc.semaphore("s_" + e)) for e in self.ENG}
        self.cnt = {e: 0 for e in self.ENG}
        self.known = {e: {} for e in self.ENG}
        self.semobj = {}
        self.maxv = {}
        self.rec = None
        self.dq = {}
        for q in ("sync", "gpsimd", "scalar"):
            self.dq[q] = dict(
                sems=[es.enter_context(nc.semaphore("d_%s%d" % (q, i))) for i in range(self.NSLOT)], n=0)

    def _wait(self, e, tok):
        sem, val = tok
        k = self.known[e]
        if k.get(id(sem), 0) >= val:
            return
        k[id(sem)] = val
        self.eng[e].wait_ge(sem, val)

    def _deps(self, e, reads, writes):
        mysem = self.sem.get(e)
        for r in reads:
            if r.lw is not None:
                if not (e == "tensor" and r.lw[0] is mysem):
                    self._wait(e, r.lw)
        for w in writes:
            if w.lw is not None:
                if not (e == "tensor" and w.lw[0] is mysem):
                    self._wait(e, w.lw)
            for t in w.rd:
                if not (e == "tensor" and t[0] is mysem):
                    self._wait(e, t)

    def _commit(self, tok, reads, writes):
        self.semobj[id(tok[0])] = tok[0]
        self.maxv[id(tok[0])] = max(self.maxv.get(id(tok[0]), 0), tok[1])
        for r in reads:
            r.rd.append(tok)
            if len(r.rd) > 12:
                best = {}
                for t in r.rd:
                    if t[1] > best.get(id(t[0]), (None, -1))[1]:
                        best[id(t[0])] = t
                r.rd = list(best.values())
        for w in writes:
            w.lw = tok
            w.rd = []

    def op(self, e, fn, reads=(), writes=(), pe=None):
        if self.rec is not None:
            self.rec.append(("op", e, fn, reads, writes, pe))
            return None
        writes = list(writes) + [r for r in reads if r.excl]
        reads = [r for r in reads if not r.excl]
        if pe is not None:
            res, rb = pe
            prev = res.pe
            if prev is not None and prev[0] != rb:
                self._wait(e, prev[1])
        self._deps(e, reads, writes)
        ins = fn(self.eng[e])
        self.cnt[e] += 1
        ins.then_inc(self.sem[e], 1)
        tok = (self.sem[e], self.cnt[e])
        self._commit(tok, reads, writes)
        if pe is not None:
            pe[0].pe = (pe[1], tok)
        return tok

    def dma(self, q, out, in_, reads=(), writes=(), **kw):
        if self.rec is not None:
            self.rec.append(("dma", q, out, in_, reads, writes, kw))
            return None
        d = self.dq[q]
        n = d["n"]
        sem = d["sems"][n % self.NSLOT]
        if n >= self.NSLOT:
            self._wait(q, (sem, 16 * (n // self.NSLOT)))
        self._deps(q, reads, writes)
        ins = self.eng[q].dma_start(out=out, in_=in_, **kw)
        ins.then_inc(sem, 16)
        d["n"] = n + 1
        tok = (sem, 16 * (n // self.NSLOT + 1))
        self._commit(tok, reads, writes)
        return tok

    def emit(self, item):
        if item[0] == "op":
            _, e, fn, reads, writes, pe = item
            self.op(e, fn, reads, writes, pe=pe)
        else:
            _, q, out, in_, reads, writes, kw = item
            self.dma(q, out, in_, reads=reads, writes=writes, **kw)

    def interleave(self, lists):
        assert self.rec is None
        lists = [l for l in lists if l]
        pos = [0] * len(lists)
        total = sum(len(l) for l in lists)
        for _ in range(total):
            best, bf = None, None
            for i, l in enumerate(lists):
                if pos[i] < len(l):
                    f = pos[i] / float(len(l))
                    if bf is None or f < bf:
                        best, bf = i, f
            self.emit(lists[best][pos[best]])
            pos[best] += 1

    def barrier(self, skip_queues=("gpsimd",)):
        skip = set()
        for q in skip_queues:
            for s in self.dq[q]["sems"]:
                skip.add(id(s))
        for e in self.ENG:
            for sid, v in self.maxv.items():
                if sid in skip:
                    continue
                self._wait(e, (self.semobj[sid], v))

    def wait_res(self, e, ress):
        for r in ress:
            if r.lw is not None:
                self._wait(e, r.lw)
            for t in r.rd:
                self._wait(e, t)


class KB:
    def __init__(self, nc, es):
        self.nc = nc
        self.S = Sched(nc, es)
        self.ncnt = 0

    def _nm(self, n):
        self.ncnt += 1
        return "%s_%d" % (n, self.ncnt)

    def sb(self, es, name, shape, dt):
        t = es.enter_context(self.nc.sbuf_tensor(self._nm(name), list(shape), dt))
        return V(t[:])

    def ps(self, es, name, shape, dt):
        t = es.enter_context(self.nc.psum_tensor(self._nm(name), list(shape), dt))
        v = V(t[:])
        v.r.excl = True
        return v

    def dram(self, name, shape, dt, kind=None):
        if kind is None:
            t = self.nc.dram_tensor(name, list(shape), dt)
        else:
            t = self.nc.dram_tensor(name, list(shape), dt, kind=kind)
        return V(t.ap())

    def mm(self, out, lhsT, rhs, start=True, stop=True, skip=False):
        def _v(x):
            return x() if callable(x) else x
        rb = (_v(lhsT.ap.base_partition), _v(lhsT.ap.partition_size))
        if skip:
            self.S.op("tensor", lambda e: e.matmul(out.ap, lhsT=lhsT.ap, rhs=rhs.ap, start=start, stop=stop, skip_group_check=True),
                      [lhsT.r, rhs.r], [out.r], pe=(out.r, rb))
        else:
            self.S.op("tensor", lambda e: e.matmul(out.ap, lhsT=lhsT.ap, rhs=rhs.ap, start=start, stop=stop),
                      [lhsT.r, rhs.r], [out.r], pe=(out.r, rb))

    def tp(self, out, in_, ident):
        def _v(x):
            return x() if callable(x) else x
        rb = (_v(in_.ap.base_partition), _v(in_.ap.partition_size))
        self.S.op("tensor", lambda e: e.transpose(out=out.ap, in_=in_.ap, identity=ident.ap),
                  [in_.r, ident.r], [out.r], pe=(out.r, rb))

    def act(self, out, in_, func, scale=1.0, bias=None, accum=None):
        rd = [in_.r]
        wr = [out.r]
        kw = {}
        if bias is not None:
            if isinstance(bias, V):
                kw["bias"] = bias.ap
                rd.append(bias.r)
            else:
                kw["bias"] = bias
        if isinstance(scale, V):
            rd.append(scale.r)
            kw["scale"] = scale.ap
        else:
            kw["scale"] = scale
        if accum is not None:
            kw["accum_out"] = accum.ap
            wr.append(accum.r)
        self.S.op("scalar", lambda e: e.activation(out=out.ap, in_=in_.ap, func=func, **kw), rd, wr)

    def tt(self, eng, out, a, b, op):
        self.S.op(eng, lambda e: e.tensor_tensor(out=out.ap, in0=a.ap, in1=b.ap, op=op), [a.r, b.r], [out.r])

    def ts(self, eng, out, a, s1, s2=None, op0=ALU.mult, op1=None):
        rd = [a.r]
        s1a = s1
        if isinstance(s1, V):
            rd.append(s1.r)
            s1a = s1.ap
        s2a = s2
        if isinstance(s2, V):
            rd.append(s2.r)
            s2a = s2.ap
        if op1 is None:
            self.S.op(eng, lambda e: e.tensor_scalar(out=out.ap, in0=a.ap, scalar1=s1a, scalar2=None, op0=op0), rd, [out.r])
        else:
            self.S.op(eng, lambda e: e.tensor_scalar(out=out.ap, in0=a.ap, scalar1=s1a, scalar2=s2a, op0=op0, op1=op1), rd, [out.r])

    def stt(self, out, a, scalar, b, op0, op1):
        rd = [a.r, b.r]
        sa = scalar
        if isinstance(scalar, V):
            rd.append(scalar.r)
            sa = scalar.ap
        self.S.op("vector", lambda e: e.scalar_tensor_tensor(out=out.ap, in0=a.ap, scalar=sa, in1=b.ap, op0=op0, op1=op1), rd, [out.r])

    def cp(self, eng, out, in_):
        if eng == "scalar":
            self.S.op("scalar", lambda e: e.copy(out=out.ap, in_=in_.ap), [in_.r], [out.r])
        else:
            self.S.op(eng, lambda e: e.tensor_copy(out=out.ap, in_=in_.ap), [in_.r], [out.r])

    def red(self, out, in_, op=ALU.add):
        self.S.op("vector", lambda e: e.tensor_reduce(out=out.ap, in_=in_.ap, axis=AX.X, op=op), [in_.r], [out.r])

    def recip(self, out, in_):
        self.S.op("vector", lambda e: e.reciprocal(out=out.ap, in_=in_.ap), [in_.r], [out.r])

    def memset(self, eng, out, val):
        self.S.op(eng, lambda e: e.memset(out.ap, val), [], [out.r])

    def dma(self, q, out, in_, **kw):
        self.S.dma(q, out.ap, in_.ap, reads=[in_.r], writes=[out.r], **kw)

    def rstd(self, out, ss, inv_n, epsv, tmp):
        self.act(tmp, ss, AF.Ln, scale=inv_n, bias=epsv)
        self.act(out, tmp, AF.Exp, scale=-0.5)


def host_consts():
    c = {}
    idx = np.arange(128)
    same = (idx[:, None] // 64) == (idx[None, :] // 64)
    c["c_ident"] = np.eye(128, dtype=np.float32)
    c["c_tri"] = (same & (idx[:, None] <= idx[None, :])).astype(np.float32)
    c["c_su"] = (same & (idx[:, None] > idx[None, :])).astype(np.float32)
    c["c_onesbd"] = same.astype(np.float32)
    mbt = np.where(same & (idx[:, None] <= idx[None, :]), 0.0, NEG).astype(np.float32)
    mbs = np.where(same & (idx[None, :] < idx[:, None]), 0.0, NEG).astype(np.float32)
    c["c_mbt4"] = np.tile(mbt, (1, 4))
    c["c_mbs4"] = np.tile(mbs, (1, 4))
    c["c_ident4"] = np.tile(np.eye(128, dtype=np.float32), (1, 4))
    c["c_bo"] = same.astype(np.float32)
    chi = np.zeros((128, 2), np.float32)
    chi[:64, 0] = 1
    chi[64:, 1] = 1
    c["c_chi"] = chi
    chj = np.zeros((2, 128, 128), np.float32)
    chj[0, :64, :] = 1
    chj[1, 64:, :] = 1
    c["c_chj"] = chj
    p = np.arange(64)
    dloc = p % 32
    perm = np.where(dloc < 16, p + 16, p - 16)
    prot = np.zeros((64, 64), np.float32)
    prot[perm, p] = 1.0
    c["c_prot"] = prot
    inv_freq = (10000.0 ** (-(np.arange(0, 32, 2, dtype=np.float32)) / np.float32(32))).astype(np.float32)
    ang = (np.arange(S_LEN, dtype=np.float32)[:, None] * inv_freq[None, :]).astype(np.float32)
    cos = np.cos(ang).astype(np.float32)
    sin = np.sin(ang).astype(np.float32)
    cosT = np.zeros((64, S_LEN), np.float32)
    sinT = np.zeros((64, S_LEN), np.float32)
    for pp in range(64):
        dl = pp % 32
        cosT[pp] = cos[:, dl % 16]
        sinT[pp] = -sin[:, dl] if dl < 16 else sin[:, dl - 16]
    c["c_cos"] = cosT
    c["c_sin"] = sinT
    return c


W_SPECS = [
    ("ffn1_pre_g", [2, 1024]), ("ffn1_w_gate", [2, 1024, 2816]), ("ffn1_w_up", [2, 1024, 2816]),
    ("ffn1_w_down", [2, 2816, 1024]), ("ffn1_post_g", [2, 1024]), ("mix_pre_g", [2, 1024]),
    ("w_in", [2, 1024, 3860]), ("gdn_conv_w", [2, 4, 768]), ("gdn_a_log", [2, 4]), ("gdn_dt_bias", [2, 4]),
    ("gdn_norm_g", [2, 64]), ("diff_lam_q1", [2, 32]), ("diff_lam_k1", [2, 32]), ("diff_lam_q2", [2, 32]),
    ("diff_lam_k2", [2, 32]), ("diff_norm_g", [2, 64]), ("ssd_conv_w", [2, 4, 768]), ("ssd_conv_b", [2, 768]),
    ("ssd_a_log", [2, 4]), ("ssd_dt_bias", [2, 4]), ("ssd_d", [2, 4]), ("ssd_norm_g", [2, 256]),
    ("mlstm_i_bias", [2, 4]), ("mlstm_f_bias", [2, 4]), ("mlstm_norm_g", [2, 256]), ("w_out", [2, 1024, 1024]),
    ("mix_post_g", [2, 1024]), ("ffn2_pre_g", [2, 1024]), ("ffn2_w_gate", [2, 1024, 2816]),
    ("ffn2_w_up", [2, 1024, 2816]), ("ffn2_w_down", [2, 2816, 1024]), ("ffn2_post_g", [2, 1024]),
]


def build(nlayers=2, stages=("ffn1", "mixa", "mixb", "ffn2"), dbg=False, nblk=NB, upto=99):
    nc = bass.Bass("TRN2", target_bir_lowering=False)
    es0 = ExitStack()
    kb = KB(nc, es0)
    S = kb.S
    x_in = kb.dram("x", [S_LEN, D], F32, "ExternalInput")
    out = kb.dram("out", [S_LEN, D], F32, "ExternalOutput")
    W = {n: kb.dram(n, s, F32, "ExternalInput") for n, s in W_SPECS}
    hc = host_consts()
    C = {n: kb.dram(n, list(a.shape), F32, "ExternalInput") for n, a in hc.items()}
    mixA = kb.dram("mixA", [S_LEN, 768], BF16)
    dbg_mixed = kb.dram("dbg_mixed", [S_LEN, D], F32, "ExternalOutput") if dbg else None

    Wb = {}
    for l in range(nlayers):
        for f in (1, 2):
            for nm, shp in (("w_gate", [D, DFF]), ("w_up", [D, DFF]), ("w_down", [DFF, D])):
                Wb[(l, "ffn%d_%s" % (f, nm))] = kb.dram("wb_%d_ffn%d_%s" % (l, f, nm), shp, BF16)
        Wb[(l, "w_in")] = kb.dram("wb_%d_w_in" % l, [D, INC], BF16)
        Wb[(l, "w_out")] = kb.dram("wb_%d_w_out" % l, [D, D], BF16)

    def cast_weight(l, name):
        src = W[name]
        dst = Wb[(l, name)]
        rows = src.ap.shape[1]
        for r0 in range(0, rows, 256):
            r1 = min(rows, r0 + 256)
            kb.dma("gpsimd", dst[r0:r1, :], V(src.ap[l], src.r)[r0:r1, :])

    for l in range(nlayers):
        for name in ("ffn1_w_gate", "ffn1_w_up", "ffn1_w_down", "w_in", "w_out", "ffn2_w_gate", "ffn2_w_up", "ffn2_w_down"):
            cast_weight(l, name)

    cs = {}
    for n in ("c_ident", "c_tri", "c_su", "c_onesbd", "c_bo"):
        cs[n] = kb.sb(es0, n, [128, 128], F32)
        kb.dma("sync", cs[n], C[n])
    for n in ("c_mbt4", "c_mbs4", "c_ident4"):
        cs[n] = kb.sb(es0, n, [128, 512], F32)
        kb.dma("sync", cs[n], C[n])
    cs["c_chi"] = kb.sb(es0, "c_chi", [128, 2], F32)
    kb.dma("sync", cs["c_chi"], C["c_chi"])
    cs["c_chj"] = kb.sb(es0, "c_chj", [128, 2, 128], F32)
    kb.dma("sync", cs["c_chj"], C["c_chj"].re("j k c -> k j c"))
    identb = kb.sb(es0, "identb", [128, 128], BF16)
    kb.cp("vector", identb, cs["c_ident"])
    protf = kb.sb(es0, "protf", [64, 64], F32)
    kb.dma("sync", protf, C["c_prot"])
    protb = kb.sb(es0, "protb", [64, 64], BF16)
    kb.cp("vector", protb, protf)
    epsv = kb.sb(es0, "epsv", [128, 1], F32)
    kb.memset("vector", epsv, EPS)
    ident = cs["c_ident"]

    def bload(es, name, src_ap_v, n, scale=None):
        t = kb.sb(es, name, [128, n], F32)
        kb.S.dma("sync", t.ap, src_ap_v.ap.partition_broadcast(128), reads=[src_ap_v.r], writes=[t.r])
        if scale is not None:
            kb.ts("vector", t, t, scale, None, ALU.mult)
        return t

    def wl(name, l):
        w = W[name]
        return V(w.ap[l], w.r)

    def prenorm_T(xt, g1b, xn, hT_dst, pT, ss, rs, tmp1, junk):
        kb.memset("gpsimd", ss, 0.0)
        kb.act(junk, xt, AF.Square, accum=ss)
        kb.rstd(rs, ss, 1.0 / D, epsv, tmp1)
        kb.stt(xn, xt, rs[:, 0:1], g1b, ALU.mult, ALU.mult)
        for k in range(8):
            kb.tp(pT[:, k * 128:(k + 1) * 128], xn[:, k * 128:(k + 1) * 128], identb)
        kb.cp("scalar", hT_dst, pT.re("p (k n) -> p k n", k=8))

    def post_res(ya, yb, xt, g2b, tmp, q2, rs2, tmp1, junk):
        kb.memset("gpsimd", q2, 0.0)
        kb.act(junk[:, 0:512], ya, AF.Square, accum=q2[:, 0:1])
        kb.act(junk[:, 512:1024], yb, AF.Square, accum=q2[:, 1:2])
        kb.tt("vector", q2[:, 0:1], q2[:, 0:1], q2[:, 1:2], ALU.add)
        kb.rstd(rs2, q2[:, 0:1], 1.0 / D, epsv, tmp1)
        kb.tt("vector", tmp[:, 0:512], ya, g2b[:, 0:512], ALU.mult)
        kb.tt("vector", tmp[:, 512:1024], yb, g2b[:, 512:1024], ALU.mult)
        kb.stt(xt, tmp, rs2[:, 0:1], xt, ALU.mult, ALU.add)

    def ffn_phase(l, f, src, dst):
        pre = "ffn%d_" % f
        with ExitStack() as es:
            g1b = bload(es, "g1b", wl(pre + "pre_g", l), D)
            g2b = bload(es, "g2b", wl(pre + "post_g", l), D, scale=0.5)
            xt = [[kb.sb(es, "xt", [128, D], F32) for s in range(4)] for par in range(2)]
            xn = kb.sb(es, "xn", [128, D], BF16)
            junk = kb.sb(es, "junk", [128, D], BF16)
            tmp = kb.sb(es, "tmp", [128, D], F32)
            hT = kb.sb(es, "hT", [128, 8, 512], BF16)
            actb = kb.sb(es, "actb", [128, 22, 512], BF16)
            slabs = [kb.sb(es, "slab", [128, 11264], BF16) for i in range(4)]
            sg = [kb.sb(es, "sg", [128, 512], F32) for i in range(2)]
            ysb = [kb.sb(es, "ysb", [128, 512], F32) for i in range(4)]
            ss = kb.sb(es, "ss", [128, 1], F32)
            rs = kb.sb(es, "rs", [128, 1], F32)
            t1 = kb.sb(es, "t1", [128, 1], F32)
            q2 = kb.sb(es, "q2", [128, 2], F32)
            pT = [kb.ps(es, "pT", [128, 1024], BF16) for i in range(2)]
            pG = [kb.ps(es, "pG", [128, 512], F32) for i in range(2)]
            pU = [kb.ps(es, "pU", [128, 512], F32) for i in range(2)]
            pY = [kb.ps(es, "pY", [128, 512], F32) for i in range(2)]
            wg = Wb[(l, pre + "w_gate")].re("(k p) n -> p k n", p=128)
            wu = Wb[(l, pre + "w_up")].re("(k p) n -> p k n", p=128)
            wd = Wb[(l, pre + "w_down")].re("(f p) n -> p f n", p=128)
            nslab = [0]

            def load_slab(kind, half):
                sl = slabs[nslab[0] % 4]
                nslab[0] += 1
                if kind == "g":
                    v = sl.re("p (k n) -> p k n", k=8)
                    kb.dma("sync", v, wg[:, :, half * 1408:(half + 1) * 1408])
                elif kind == "u":
                    v = sl.re("p (k n) -> p k n", k=8)
                    kb.dma("sync", v, wu[:, :, half * 1408:(half + 1) * 1408])
                else:
                    v = sl.re("p (f n) -> p f n", f=22)
                    kb.dma("sync", v, wd[:, :, half * 512:(half + 1) * 512])
                return v

            def load_x(t):
                for s in range(4):
                    r0 = (t * 4 + s) * 128
                    kb.dma("scalar", xt[t % 2][s], src[r0:r0 + 128, :])

            hTs = [hT, kb.sb(es, "hT2", [128, 8, 512], BF16)]
            junk2 = kb.sb(es, "junk2", [128, D], BF16)
            ssb = kb.sb(es, "ssb", [128, 1], F32); rsb = kb.sb(es, "rsb", [128, 1], F32); t1b = kb.sb(es, "t1b", [128, 1], F32)

            def prenorm_tile(t):
                for s in range(4):
                    prenorm_T(xt[t % 2][s], g1b, xn, hTs[t % 2][:, :, s * 128:(s + 1) * 128], pT[s % 2], ss, rs, t1, junk)

            load_x(0)
            NT = S_LEN // 512
            prenorm_tile(0)
            for t in range(NT):
                par = t % 2
                hTt = hTs[par]
                sg0 = load_slab("g", 0)
                su0 = load_slab("u", 0)
                if t + 1 < NT:
                    load_x(t + 1)
                sg1 = load_slab("g", 1)
                su1 = load_slab("u", 1)
                for half, (sgw, suw) in enumerate(((sg0, su0), (sg1, su1))):
                    for fi in range(11):
                        fc = half * 11 + fi
                        pg = pG[fc % 2]
                        pu = pU[fc % 2]
                        for k in range(8):
                            kb.mm(pg, sgw[:, k, fi * 128:(fi + 1) * 128], hTt[:, k, :], start=(k == 0), stop=(k == 7))
                        for k in range(8):
                            kb.mm(pu, suw[:, k, fi * 128:(fi + 1) * 128], hTt[:, k, :], start=(k == 0), stop=(k == 7))
                        kb.act(sg[fc % 2], pg, AF.Silu)
                        kb.tt("vector", actb[:, fc, :], sg[fc % 2], pu, ALU.mult)
                    if half == 0:
                        sd0 = load_slab("d", 0)
                sd1 = load_slab("d", 1)
                lists = []
                S.rec = []
                for s in range(4):
                    py = pY[s % 2]
                    for fc in range(22):
                        kb.mm(py, actb[:, fc, s * 128:(s + 1) * 128], sd0[:, fc, :], start=(fc == 0), stop=(fc == 21))
                    kb.cp("scalar", ysb[s], py)
                for s in range(4):
                    py = pY[s % 2]
                    for fc in range(22):
                        kb.mm(py, actb[:, fc, s * 128:(s + 1) * 128], sd1[:, fc, :], start=(fc == 0), stop=(fc == 21))
                    post_res(ysb[s], py, xt[par][s], g2b, tmp, q2, rsb, t1b, junk2)
                    r0 = (t * 4 + s) * 128
                    kb.dma("scalar", dst[r0:r0 + 128, :], xt[par][s])
                lists.append(S.rec)
                S.rec = None
                if t + 1 < NT:
                    S.rec = []
                    prenorm_tile(t + 1)
                    lists.append(S.rec)
                    S.rec = None
                S.interleave(lists)
            S.barrier()

    def mixa_phase(l, xs):
        with ExitStack() as es:
            sb = lambda n, shp, dt=F32: kb.sb(es, n, shp, dt)
            g1b = bload(es, "g1b", wl("mix_pre_g", l), D)
            winb = sb("winb", [128, 8, INC], BF16)
            wsrc = Wb[(l, "w_in")].re("(k p) n -> p k n", p=128)
            for k in range(8):
                kb.dma("sync", winb[:, k, :], wsrc[:, k, :])
            gcw = sb("gcw", [128, 6, 4])
            scw = sb("scw", [128, 6, 4])
            scb = sb("scb", [128, 6])
            for j in range(4):
                kb.dma("sync", gcw[:, :, j], wl("gdn_conv_w", l)[j].re("(c p) -> p c", p=128), allow_slow_non_contiguous=True)
                kb.dma("sync", scw[:, :, j], wl("ssd_conv_w", l)[j].re("(c p) -> p c", p=128), allow_slow_non_contiguous=True)
            kb.dma("sync", scb, wl("ssd_conv_b", l).re("(c p) -> p c", p=128), allow_slow_non_contiguous=True)
            negA_g = bload(es, "negA_g", wl("gdn_a_log", l), 4)
            kb.act(negA_g, negA_g, AF.Exp)
            kb.ts("vector", negA_g, negA_g, -1.0, None, ALU.mult)
            negA_s = bload(es, "negA_s", wl("ssd_a_log", l), 4)
            kb.act(negA_s, negA_s, AF.Exp)
            kb.ts("vector", negA_s, negA_s, -1.0, None, ALU.mult)
            dtb_g = bload(es, "dtb_g", wl("gdn_dt_bias", l), 4)
            dtb_s = bload(es, "dtb_s", wl("ssd_dt_bias", l), 4)
            dsk = bload(es, "dsk", wl("ssd_d", l), 4)
            ibias = bload(es, "ibias", wl("mlstm_i_bias", l), 4)
            fbias = bload(es, "fbias", wl("mlstm_f_bias", l), 4)
            gng = bload(es, "gng", wl("gdn_norm_g", l), 64)
            gssd = bload(es, "gssd", wl("ssd_norm_g", l), 256)
            gml = bload(es, "gml", wl("mlstm_norm_g", l), 256)

            TRI, SU, ONESBD, BO = cs["c_tri"], cs["c_su"], cs["c_onesbd"], cs["c_bo"]
            NI4 = sb("NI4", [128, 512])
            kb.ts("vector", NI4, cs["c_ident4"], -1.0, 1.0, ALU.mult, ALU.add)
            MBT4, ID4, CHI, CHJ = cs["c_mbt4"], cs["c_ident4"], cs["c_chi"], cs["c_chj"]

            xt = sb("xt", [128, D])
            xn = sb("xn", [128, D], BF16)
            junk = sb("junk", [128, D], BF16)
            hTs = [sb("hT", [128, 8, 128], BF16) for i in range(2)]
            ss = sb("ss", [128, 1]); rs = sb("rs", [128, 1]); t1s = sb("t1s", [128, 1])
            mixos = [sb("mixo", [128, 768], BF16) for i in range(2)]
            ptp = kb.ps(es, "ptp", [128, 1024], BF16)

            def bfv(bank):
                return V(bank.ap.bitcast(BF16), bank.r)

            v3 = lambda v, h=4: v.re("p (h d) -> p h d", h=h)
            hs = lambda h: slice(h * 128, (h + 1) * 128)
            hp = lambda h: slice((h % 2) * 64, (h % 2) * 64 + 64)

            def proj_fm(pout, col0, hT, M=128):
                for k in range(8):
                    kb.mm(pout, winb[:, k, col0:col0 + M], hT[:, k, :], start=(k == 0), stop=(k == 7))

            def proj_tm(pout, col0, n, hT):
                for k in range(8):
                    kb.mm(pout, hT[:, k, :], winb[:, k, col0:col0 + n], start=(k == 0), stop=(k == 7))

            def conv_chunk(praw, raw, w, bias, ac, outv):
                kb.cp("scalar", raw[:, 3:131], praw)
                if bias is None:
                    kb.ts("vector", ac, raw[:, 3:131], w[:, 3:4], None, ALU.mult)
                else:
                    kb.ts("vector", ac, raw[:, 3:131], w[:, 3:4], bias, ALU.mult, ALU.add)
                for j in (2, 1, 0):
                    kb.stt(ac, raw[:, j:j + 128], w[:, j:j + 1], ac, ALU.mult, ALU.add)
                kb.cp("gpsimd", raw[:, 0:3], raw[:, 128:131])
                kb.act(outv, ac, AF.Silu)

            def decay_prep(glog, G_all, psmall, pbig):
                kb.mm(psmall[:, 0:4], TRI, glog)
                kb.mm(psmall[:, 4:8], SU, glog)
                kb.mm(psmall[:, 8:12], ONESBD, glog)
                kb.tt("vector", v3(G_all), TRI.bc(1, [128, 4, 128]), glog.bc(2, [128, 4, 128]), ALU.mult)
                kb.mm(pbig, SU, G_all, start=True, stop=False)
                kb.mm(pbig, ident, MBT4, start=False, stop=True)

            class G:
                pass
            g = G()
            g.A = kb.ps(es, "gA", [128, 512], F32); g.B = kb.ps(es, "gB", [128, 512], F32); g.C = kb.ps(es, "gC", [128, 512], F32)
            g.raw = sb("rawG", [128, 6, 131]); kb.memset("vector", g.raw, 0.0)
            g.acc = [sb("gacc", [128, 128]) for i in range(2)]
            g.gF = sb("gF", [128, 4, 128]); g.sq4 = sb("sq4", [128, 4, 128]); g.rn4 = sb("rn4", [128, 4, 128])
            g.qn = sb("qn", [128, 2, 128], BF16); g.kn = sb("kn", [128, 2, 128], BF16); g.vFb = sb("vFb", [128, 2, 128], BF16)
            g.sgate = sb("sgate", [128, 256]); g.beta = sb("beta", [128, 4]); g.a4 = sb("ga4", [128, 4]); g.glog = sb("gglog", [128, 4])
            g.cbg = sb("cbg", [128, 4]); g.E = sb("gE", [128, 12]); g.G_all = sb("gG_all", [128, 512]); g.DT_all = sb("gDT_all", [128, 512])
            g.Kbg = sb("Kbg", [128, 256]); g.Vb = sb("Vb", [128, 256]); g.kdec = sb("gkdec", [128, 256], BF16)
            g.Lt = sb("Lt", [128, 512])
            g.P = [sb("P", [128, 512]) for j in range(6)]
            g.PT = [sb("PT", [128, 512]) for j in range(5)]
            g.Y = sb("Y", [128, 512])
            g.U_sb = sb("U_sb", [128, 256]); g.WT_sb = sb("WT_sb", [128, 2, 128], BF16); g.attnT = sb("attnT", [128, 512], BF16)
            g.g_rep = sb("gg_rep", [128, 256]); g.eP = sb("geP", [128, 4]); g.vnew = sb("vnew", [128, 256], BF16)
            g.S32 = sb("S32", [128, 2, 64]); g.Sbf = sb("Sbf", [128, 2, 64], BF16)
            kb.memset("vector", g.S32, 0.0); kb.memset("vector", g.Sbf, 0.0)
            g.t1 = sb("gt1", [128, 256]); g.o_ = sb("go_", [128, 256]); g.sq = sb("gsq", [128, 256])
            g.ss4 = sb("gss4", [128, 4]); g.rs4 = sb("grs4", [128, 4]); g.tm4 = sb("gtm4", [128, 4])

            def gdn_block(b, hT, mixo):
                A, B, C = g.A, g.B, g.C
                Bb = bfv(B)
                gF, kn, qn, vFb, beta, glog, E, DT_all, P, PT, Y = g.gF, g.kn, g.qn, g.vFb, g.beta, g.glog, g.E, g.DT_all, g.P, g.PT, g.Y
                for c in range(6):
                    pj = A if c % 2 == 0 else B
                    proj_fm(pj[:, 0:128], c * 128, hT)
                    outv = gF[:, c, :] if c < 4 else vFb[:, c - 4, :]
                    conv_chunk(pj[:, 0:128], g.raw[:, c, :], gcw[:, c, :], None, g.acc[c % 2], outv)
                kb.tt("gpsimd", g.sq4, gF, gF, ALU.mult)
                for i in range(4):
                    kb.mm(A[:, i * 128:(i + 1) * 128], BO, g.sq4[:, i, :])
                rn4f = g.rn4.re("p a b -> p (a b)")
                kb.act(rn4f, A, AF.Ln, bias=epsv)
                kb.act(rn4f, rn4f, AF.Exp, scale=-0.5)
                kb.stt(qn, gF[:, 0:2, :], 0.125, g.rn4[:, 0:2, :], ALU.mult, ALU.mult)
                kb.tt("vector", kn, gF[:, 2:4, :], g.rn4[:, 2:4, :], ALU.mult)
                kb.tp(Bb[:, 0:128], kn[:, 0, :], identb)
                kb.tp(Bb[:, 128:256], kn[:, 1, :], identb)
                kb.tp(Bb[:, 256:384], vFb[:, 0, :], identb)
                kb.tp(Bb[:, 384:512], vFb[:, 1, :], identb)
                kTM = v3(Bb[:, 0:256]); vTM = v3(Bb[:, 256:512])
                proj_tm(A[:, 0:264], 768, 264, hT)
                kb.act(g.sgate, A[:, 0:256], AF.Silu)
                kb.act(beta, A[:, 256:260], AF.Sigmoid)
                kb.tt("vector", g.a4, A[:, 260:264], dtb_g, ALU.add)
                kb.act(g.a4, g.a4, AF.Exp)
                kb.act(g.a4, g.a4, AF.Ln, bias=1.0)
                kb.tt("vector", glog, g.a4, negA_g, ALU.mult)
                decay_prep(glog, g.G_all, C, A)
                kb.act(E, C[:, 0:12], AF.Exp)
                kb.act(DT_all, A, AF.Exp)
                kb.tt("vector", g.cbg, beta, E[:, 0:4], ALU.mult)
                kb.tt("vector", v3(g.Kbg), kTM, g.cbg.bc(2, [128, 4, 64]), ALU.mult)
                kb.tt("vector", v3(g.Vb), vTM, beta.bc(2, [128, 4, 64]), ALU.mult)
                kb.tt("vector", v3(g.kdec), kTM, E[:, 4:8].bc(2, [128, 4, 64]), ALU.mult)
                for h in range(4):
                    kb.mm(B[:, hs(h)], kn[hp(h), h // 2, :], kn[hp(h), h // 2, :])
                kb.tt("vector", g.Lt, B, DT_all, ALU.mult)
                kb.tt("gpsimd", g.Lt, g.Lt, NI4, ALU.mult)
                for h in range(4):
                    kb.tp(C[:, hs(h)], g.Lt[:, hs(h)], ident)
                kb.tt("vector", v3(P[0]), v3(C), beta.bc(2, [128, 4, 128]), ALU.mult)
                for h in range(4):
                    kb.tp(A[:, hs(h)], P[0][:, hs(h)], ident)
                kb.cp("scalar", PT[0], A)
                kb.tt("vector", Y, ID4, PT[0], ALU.subtract)
                for j in range(5):
                    for h in range(4):
                        kb.mm(B[:, hs(h)], PT[j][:, hs(h)], P[j][:, hs(h)])
                    if j < 4:
                        for h in range(4):
                            kb.mm(C[:, hs(h)], P[j][:, hs(h)], PT[j][:, hs(h)])
                    kb.cp("scalar", P[j + 1], B)
                    if j < 4:
                        kb.cp("vector", PT[j + 1], C)
                    for h in range(4):
                        kb.mm(A[:, hs(h)], P[j + 1][:, hs(h)], Y[:, hs(h)])
                    kb.tt("vector", Y, Y, A, ALU.add)
                for h in range(4):
                    kb.mm(B[:, h * 64:(h + 1) * 64], Y[:, hs(h)], g.Vb[:, h * 64:(h + 1) * 64])
                for h in range(4):
                    kb.mm(C[hp(h), (h // 2) * 128:(h // 2) * 128 + 128], g.Kbg[:, h * 64:(h + 1) * 64], Y[:, hs(h)])
                kb.cp("scalar", g.U_sb, B[:, 0:256])
                kb.cp("vector", g.WT_sb.re("p a b -> p (a b)"), C[:, 0:256])
                for h in range(4):
                    kb.mm(A[:, hs(h)], kn[hp(h), h // 2, :], qn[hp(h), h // 2, :])
                kb.tt("vector", g.attnT, A, DT_all, ALU.mult)
                kb.cp("gpsimd", v3(g.g_rep), glog.bc(2, [128, 4, 64]))
                for pair in range(2):
                    kb.mm(B[:, pair * 2:pair * 2 + 2], g.g_rep[:, pair * 128:(pair + 1) * 128], CHI)
                kb.act(g.eP, B[:, 0:4], AF.Exp)
                for j in range(2):
                    r = slice(j * 64, j * 64 + 64)
                    for h in range(4):
                        kb.mm(C[r, h * 64:(h + 1) * 64], g.WT_sb[hp(h), h // 2, r], g.Sbf[hp(h), h // 2, :])
                    kb.tt("vector", g.vnew[r, :], g.U_sb[r, :], C[r, 0:256], ALU.subtract)
                    for h in range(4):
                        kb.mm(A[r, h * 64:(h + 1) * 64], qn[hp(h), h // 2, r], g.Sbf[hp(h), h // 2, :])
                    for h in range(4):
                        kb.mm(B[hp(h), (h // 2) * 64:(h // 2) * 64 + 64], g.kdec[r, h * 64:(h + 1) * 64], g.vnew[r, h * 64:(h + 1) * 64])
                    for pair in range(2):
                        kb.stt(g.S32[:, pair, :], g.S32[:, pair, :], g.eP[:, pair * 2 + j:pair * 2 + j + 1],
                               B[:, pair * 64:(pair + 1) * 64], ALU.mult, ALU.add)
                    kb.cp("scalar", g.Sbf, g.S32)
                for h in range(4):
                    kb.mm(C[:, h * 64:(h + 1) * 64], g.attnT[:, hs(h)], g.vnew[:, h * 64:(h + 1) * 64])
                kb.tt("vector", v3(g.t1), v3(A[:, 0:256]), E[:, 0:4].bc(2, [128, 4, 64]), ALU.mult)
                kb.tt("vector", g.o_, g.t1, C[:, 0:256], ALU.add)
                kb.tt("gpsimd", g.sq, g.o_, g.o_, ALU.mult)
                kb.red(g.ss4, v3(g.sq))
                kb.rstd(g.rs4, g.ss4, 1.0 / 64, epsv, g.tm4)
                kb.tt("vector", v3(g.o_), v3(g.o_), g.rs4.bc(2, [128, 4, 64]), ALU.mult)
                kb.tt("gpsimd", v3(g.o_), v3(g.o_), gng.bc(1, [128, 4, 64]), ALU.mult)
                kb.tt("vector", mixo[:, 0:256], g.o_, g.sgate, ALU.mult)

            s_ = G()
            s_.A = kb.ps(es, "sA", [128, 512], F32); s_.B = kb.ps(es, "sB", [128, 512], F32)
            s_.raw = sb("rawS", [128, 6, 131]); kb.memset("vector", s_.raw, 0.0)
            s_.acc = [sb("sacc", [128, 128]) for i in range(2)]
            s_.sF = sb("sF", [128, 6, 128], BF16)
            s_.xs_tm = sb("xs_tm", [128, 256]); s_.B_tm = sb("B_tm", [128, 256], BF16)
            s_.sz = sb("sz", [128, 256]); s_.a4 = sb("sa4", [128, 4]); s_.dt4 = sb("dt4", [128, 4]); s_.cdt = sb("cdt", [128, 4])
            s_.glog = sb("sglog", [128, 4]); s_.E = sb("sE", [128, 12]); s_.G_all = sb("sG_all", [128, 512]); s_.DT_all = sb("sDT_all", [128, 512])
            s_.etotN = sb("etotN", [128, 8])
            s_.xdt = sb("xdt", [128, 256], BF16); s_.xdec = sb("xdec", [128, 256], BF16); s_.xskip = sb("xskip", [128, 256])
            s_.MT = sb("MT", [128, 512], BF16); s_.y_d = sb("y_d", [128, 256])
            s_.ST32 = sb("ST32", [128, 256]); s_.STbf = sb("STbf", [128, 256], BF16); s_.tmpS = sb("tmpS", [128, 256])
            kb.memset("vector", s_.ST32, 0.0); kb.memset("vector", s_.STbf, 0.0)
            s_.t1 = sb("st1", [128, 256]); s_.y_ = sb("y_", [128, 256]); s_.sq = sb("ssq", [128, 256])
            s_.ss2 = sb("ss2", [128, 2]); s_.rs2 = sb("rs2", [128, 2]); s_.tm2 = sb("tm2", [128, 2])

            def ssd_block(b, hT, mixo):
                A, B = s_.A, s_.B
                Ab = bfv(A)
                sF, E, DT_all, glog = s_.sF, s_.E, s_.DT_all, s_.glog
                for c in range(6):
                    pj = A if c % 2 == 0 else B
                    proj_fm(pj[:, 0:128], 2056 + c * 128, hT)
                    conv_chunk(pj[:, 0:128], s_.raw[:, c, :], scw[:, c, :], scb[:, c:c + 1], s_.acc[c % 2], sF[:, c, :])
                for i in range(4):
                    kb.tp(Ab[:, i * 128:(i + 1) * 128], sF[:, i, :], identb)
                kb.cp("scalar", s_.xs_tm, Ab[:, 0:256])
                kb.cp("vector", s_.B_tm, Ab[:, 256:512])
                proj_tm(B[:, 0:256], 1800, 256, hT)
                proj_tm(B[:, 256:260], 2824, 4, hT)
                kb.act(s_.sz, B[:, 0:256], AF.Silu)
                kb.tt("vector", s_.a4, B[:, 256:260], dtb_s, ALU.add)
                kb.act(s_.a4, s_.a4, AF.Exp)
                kb.act(s_.dt4, s_.a4, AF.Ln, bias=1.0)
                kb.tt("vector", glog, s_.dt4, negA_s, ALU.mult)
                decay_prep(glog, s_.G_all, B, A)
                for j in range(2):
                    kb.mm(B[:, 16 + j * 4:16 + (j + 1) * 4], CHJ[:, j, :], glog)
                kb.act(E, B[:, 0:12], AF.Exp)
                kb.act(s_.etotN, B[:, 16:24], AF.Exp)
                kb.act(DT_all, A, AF.Exp)
                xsTM = v3(s_.xs_tm)
                kb.tt("vector", s_.cdt, s_.dt4, E[:, 4:8], ALU.mult)
                kb.tt("vector", v3(s_.xdt), xsTM, s_.dt4.bc(2, [128, 4, 64]), ALU.mult)
                kb.tt("gpsimd", v3(s_.xdec), xsTM, s_.cdt.bc(2, [128, 4, 64]), ALU.mult)
                kb.tt("gpsimd", v3(s_.xskip), xsTM, dsk.bc(2, [128, 4, 64]), ALU.mult)
                for gi in range(2):
                    kb.mm(A[:, gi * 128:(gi + 1) * 128], sF[:, 2 + gi, :], sF[:, 4 + gi, :])
                MT3 = v3(s_.MT); DT3 = v3(DT_all)
                for gi in range(2):
                    kb.tt("vector", MT3[:, 2 * gi:2 * gi + 2, :], A[:, gi * 128:(gi + 1) * 128].bc(1, [128, 2, 128]),
                          DT3[:, 2 * gi:2 * gi + 2, :], ALU.mult)
                for h in range(4):
                    kb.mm(B[:, h * 64:(h + 1) * 64], s_.MT[:, hs(h)], s_.xdt[:, h * 64:(h + 1) * 64])
                kb.cp("scalar", s_.y_d, B[:, 0:256])
                for j in range(2):
                    r = slice(j * 64, j * 64 + 64)
                    for h in range(4):
                        kb.mm(A[r, h * 64:(h + 1) * 64], sF[:, 4 + h // 2, r], s_.STbf[:, h * 64:(h + 1) * 64])
                    for h in range(4):
                        kb.mm(B[:, h * 64:(h + 1) * 64], s_.B_tm[r, (h // 2) * 128:(h // 2) * 128 + 128], s_.xdec[r, h * 64:(h + 1) * 64])
                    kb.tt("gpsimd", v3(s_.tmpS), v3(s_.ST32), s_.etotN[:, j * 4:(j + 1) * 4].bc(2, [128, 4, 64]), ALU.mult)
                    kb.tt("vector", s_.ST32, s_.tmpS, B[:, 0:256], ALU.add)
                    kb.cp("scalar", s_.STbf, s_.ST32)
                kb.tt("vector", v3(s_.t1), v3(A[:, 0:256]), E[:, 0:4].bc(2, [128, 4, 64]), ALU.mult)
                kb.tt("gpsimd", s_.y_, s_.t1, s_.y_d, ALU.add)
                kb.tt("gpsimd", s_.y_, s_.y_, s_.xskip, ALU.add)
                kb.tt("vector", s_.y_, s_.y_, s_.sz, ALU.mult)
                kb.tt("gpsimd", s_.sq, s_.y_, s_.y_, ALU.mult)
                kb.red(s_.ss2, v3(s_.sq, 2))
                kb.rstd(s_.rs2, s_.ss2, 1.0 / 128, epsv, s_.tm2)
                kb.tt("vector", v3(s_.y_, 2), v3(s_.y_, 2), s_.rs2.bc(2, [128, 2, 128]), ALU.mult)
                kb.tt("vector", mixo[:, 256:512], s_.y_, gssd, ALU.mult)

            m_ = G()
            m_.A = kb.ps(es, "mA", [128, 512], F32); m_.B = kb.ps(es, "mB", [128, 512], F32)
            m_.mqk = sb("mqk", [128, 4, 128], BF16); m_.kv = sb("kv_tm", [128, 512])
            m_.so = sb("so", [128, 256]); m_.li4 = sb("li4", [128, 4]); m_.eli8 = sb("eli8", [128, 4]); m_.f4 = sb("f4", [128, 4])
            m_.glog = sb("mglog", [128, 4]); m_.E = sb("mE", [128, 12]); m_.G_all = sb("mG_all", [128, 512]); m_.DT_all = sb("mDT_all", [128, 512])
            m_.vli = sb("vli", [128, 4, 66], BF16); m_.kdec = sb("mkdec", [128, 256], BF16); m_.STm = sb("STm", [128, 512], BF16)
            m_.intra = sb("intra", [128, 272])
            m_.g_rep = sb("mg_rep", [128, 256]); m_.eP = sb("meP", [128, 4])
            m_.CS32 = sb("CS32", [128, 2, 66]); m_.CSbf = sb("CSbf", [128, 2, 66], BF16)
            kb.memset("vector", m_.CS32, 0.0); kb.memset("vector", m_.CSbf, 0.0)
            m_.t2 = sb("t2", [128, 4, 65]); m_.d4 = sb("d4", [128, 4]); m_.hm = sb("hm", [128, 256]); m_.sq = sb("msq", [128, 256])
            m_.ss4 = sb("mss4", [128, 4]); m_.rs4 = sb("mrs4", [128, 4]); m_.tm4 = sb("mtm4", [128, 4])

            def mlstm_block(b, hT, mixo):
                A, B = m_.A, m_.B
                mqk, E, DT_all, glog, vli, t2, hm = m_.mqk, m_.E, m_.DT_all, m_.glog, m_.vli, m_.t2, m_.hm
                for i in range(2):
                    proj_fm(A[:, i * 128:(i + 1) * 128], 2828 + i * 128, hT)
                    proj_fm(A[:, 256 + i * 128:256 + (i + 1) * 128], 3084 + i * 128, hT)
                kb.cp("scalar", mqk.re("p a b -> p (a b)"), A)
                proj_tm(B, 3084, 512, hT)
                kb.cp("scalar", m_.kv, B)
                proj_tm(A[:, 0:264], 3596, 264, hT)
                kb.act(m_.so, A[:, 0:256], AF.Sigmoid)
                kb.tt("vector", m_.li4, A[:, 256:260], ibias, ALU.add)
                kb.act(m_.eli8, m_.li4, AF.Exp)
                kb.ts("vector", m_.eli8, m_.eli8, 0.125, None, ALU.mult)
                kb.tt("vector", m_.f4, A[:, 260:264], fbias, ALU.add)
                kb.act(m_.f4, m_.f4, AF.Exp, scale=-1.0)
                kb.act(m_.f4, m_.f4, AF.Ln, bias=1.0)
                kb.ts("vector", glog, m_.f4, -1.0, None, ALU.mult)
                decay_prep(glog, m_.G_all, A, B)
                kb.act(E, A[:, 0:12], AF.Exp)
                kb.act(DT_all, B, AF.Exp)
                kb.tt("gpsimd", vli[:, :, 0:64], v3(m_.kv[:, 256:512]), m_.eli8.bc(2, [128, 4, 64]), ALU.mult)
                kb.cp("vector", vli[:, :, 64:65], m_.eli8.bc(2, [128, 4, 1]))
                kb.tt("gpsimd", v3(m_.kdec), v3(m_.kv[:, 0:256]), E[:, 4:8].bc(2, [128, 4, 64]), ALU.mult)
                for h in range(4):
                    kb.mm(A[:, hs(h)], mqk[hp(h), 2 + h // 2, :], mqk[hp(h), h // 2, :])
                kb.tt("vector", m_.STm, A, DT_all, ALU.mult)
                for h in range(4):
                    kb.mm(B[:, h * 68:h * 68 + 65], m_.STm[:, hs(h)], vli[:, h, 0:65])
                kb.cp("scalar", m_.intra, B[:, 0:272])
                kb.cp("gpsimd", v3(m_.g_rep), glog.bc(2, [128, 4, 64]))
                for pair in range(2):
                    kb.mm(A[:, pair * 2:pair * 2 + 2], m_.g_rep[:, pair * 128:(pair + 1) * 128], CHI)
                kb.act(m_.eP, A[:, 0:4], AF.Exp)
                for j in range(2):
                    r = slice(j * 64, j * 64 + 64)
                    for h in range(4):
                        kb.mm(A[r, h * 68:h * 68 + 65], mqk[hp(h), h // 2, r], m_.CSbf[hp(h), h // 2, 0:65])
                    for h in range(4):
                        kb.mm(B[hp(h), (h // 2) * 68:(h // 2) * 68 + 65], m_.kdec[r, h * 64:(h + 1) * 64], vli[r, h, 0:65])
                    for pair in range(2):
                        kb.stt(m_.CS32[:, pair, 0:65], m_.CS32[:, pair, 0:65], m_.eP[:, pair * 2 + j:pair * 2 + j + 1],
                               B[:, pair * 68:pair * 68 + 65], ALU.mult, ALU.add)
                    kb.cp("scalar", m_.CSbf, m_.CS32)
                kb.tt("vector", t2, A[:, 0:272].re("p (h d) -> p h d", h=4)[:, :, 0:65], E[:, 0:4].bc(2, [128, 4, 65]), ALU.mult)
                kb.tt("vector", t2, t2, m_.intra.re("p (h d) -> p h d", h=4)[:, :, 0:65], ALU.add)
                kb.tt("vector", m_.d4, t2[:, :, 64], t2[:, :, 64], ALU.mult)
                kb.ts("vector", m_.d4, m_.d4, 1.0, None, ALU.max)
                kb.act(m_.d4, m_.d4, AF.Ln)
                kb.act(m_.d4, m_.d4, AF.Exp, scale=-0.5)
                kb.tt("vector", v3(hm), t2[:, :, 0:64], m_.d4.bc(2, [128, 4, 64]), ALU.mult)
                kb.tt("gpsimd", hm, hm, m_.so, ALU.mult)
                kb.tt("gpsimd", m_.sq, hm, hm, ALU.mult)
                kb.red(m_.ss4, v3(m_.sq))
                kb.rstd(m_.rs4, m_.ss4, 1.0 / 64, epsv, m_.tm4)
                kb.tt("vector", v3(hm), v3(hm), m_.rs4.bc(2, [128, 4, 64]), ALU.mult)
                kb.tt("vector", mixo[:, 512:768], hm, gml, ALU.mult)

            def pre_block(b):
                r0 = b * 128
                kb.dma("sync", xt, xs[r0:r0 + 128, :])
                prenorm_T(xt, g1b, xn, hTs[b % 2], ptp, ss, rs, t1s, junk)

            pre_block(0)
            for b in range(nblk):
                r0 = b * 128
                hT = hTs[b % 2]
                mixo = mixos[b % 2]
                lists = []
                for fn in (gdn_block, ssd_block, mlstm_block):
                    S.rec = []
                    fn(b, hT, mixo)
                    lists.append(S.rec)
                    S.rec = None
                if b + 1 < nblk:
                    S.rec = []
                    pre_block(b + 1)
                    lists.append(S.rec)
                    S.rec = None
                S.interleave(lists)
                kb.dma("sync", mixA[r0:r0 + 128, :], mixo)
            S.barrier()

    def mixb_phase(l, xs):
        lambda_init = 0.8 - 0.6 * math.exp(-0.3 * l)
        with ExitStack() as es:
            sb = lambda n, shp, dt=F32: kb.sb(es, n, shp, dt)
            g1b = bload(es, "g1b", wl("mix_pre_g", l), D)
            g2b = bload(es, "g2b", wl("mix_post_g", l), D)
            gdf = bload(es, "gdf", wl("diff_norm_g", l), 64, scale=(1.0 - lambda_init))
            winb = sb("winb", [128, 8, 768], BF16)
            wsrc = Wb[(l, "w_in")].re("(k p) n -> p k n", p=128)
            woutb = sb("woutb", [128, 8, D], BF16)
            wosrc = Wb[(l, "w_out")].re("(k p) n -> p k n", p=128)
            for k in range(8):
                kb.dma("sync", winb[:, k, :], wsrc[:, k, 1032:1800])
                kb.dma("sync", woutb[:, k, :], wosrc[:, k, :])
            lq1 = bload(es, "lq1", wl("diff_lam_q1", l), 32); lk1 = bload(es, "lk1", wl("diff_lam_k1", l), 32)
            lq2 = bload(es, "lq2", wl("diff_lam_q2", l), 32); lk2 = bload(es, "lk2", wl("diff_lam_k2", l), 32)
            lam2 = sb("lam2", [128, 2]); neglam = sb("neglam", [128, 1])
            kb.tt("vector", lq1, lq1, lk1, ALU.mult)
            kb.tt("vector", lq2, lq2, lk2, ALU.mult)
            kb.red(lam2[:, 0:1], lq1)
            kb.red(lam2[:, 1:2], lq2)
            kb.act(lam2, lam2, AF.Exp)
            kb.tt("vector", neglam, lam2[:, 1:2], lam2[:, 0:1], ALU.subtract)
            kb.ts("vector", neglam, neglam, -lambda_init, None, ALU.add)

            xt = sb("xt", [128, D]); xn = sb("xn", [128, D], BF16); junk = sb("junk", [128, D], BF16)
            tmp = sb("tmp", [128, D])
            hT = sb("hT", [128, 8, 128], BF16)
            ss = sb("ss", [128, 1]); rs = sb("rs", [128, 1]); t1s = sb("t1s", [128, 1]); q2 = sb("q2", [128, 2])
            KT = sb("KT", [64, 4, S_LEN], BF16)
            Vaug = sb("Vaug", [128, NB, 4, 66], BF16)
            kb.memset("vector", Vaug.re("p a b c -> p (a b c)"), 1.0)
            cosb = sb("cosb", [64, 128]); sinb = sb("sinb", [64, 128])
            qraw = sb("qraw", [64, 512], BF16)
            r1 = sb("r1", [64, 512]); r2 = sb("r2", [64, 512])
            qr = sb("qr", [64, 4, 128], BF16)
            PTt = [sb("PTt", [128, 512], BF16) for i in range(2)]
            rec8 = sb("rec8", [128, 8]); on = sb("on", [128, 8, 64]); od = sb("od", [128, 256]); sq = sb("sq", [128, 256])
            ss4 = sb("ss4", [128, 4]); rs4 = sb("rs4", [128, 4]); tm4 = sb("tm4", [128, 4])
            mT = sb("mT", [128, 8, 128], BF16)
            mixeds = [sb("mixed", [128, D], BF16) for i in range(3)]
            xts = [xt, sb("xt2", [128, D]), sb("xt3", [128, D])]
            qrs = [qr, sb("qr2", [64, 4, 128], BF16)]
            junk2 = sb("junk2", [128, D], BF16)
            rsb = sb("rsb", [128, 1]); t1b = sb("t1b", [128, 1])
            mdbg = sb("mdbg", [128, D]) if dbg else None
            pP = kb.ps(es, "pP", [128, 1024], BF16)
            pPf = V(pP.ap.bitcast(F32), pP.r)
            pq = kb.ps(es, "pq", [128, 512], F32)
            pS = [kb.ps(es, "pS", [128, 512], F32) for i in range(2)]
            pA = [kb.ps(es, "pA", [128, 512], F32) for i in range(2)]
            pY = [kb.ps(es, "pY", [128, 512], F32) for i in range(2)]
            pYb = V(pY[0].ap.bitcast(BF16), pY[0].r)
            v3 = lambda v, h=4: v.re("p (h d) -> p h d", h=h)
            sc = 32.0 ** -0.5
            nexp = [0]

            def stage_P(b):
                r0 = b * 128
                xtb = xts[b % 3]; mixed = mixeds[b % 3]; qrb = qrs[b % 2]
                kb.dma("sync", xtb, xs[r0:r0 + 128, :])
                kb.dma("sync", cosb, C["c_cos"][:, r0:r0 + 128])
                kb.dma("sync", sinb, C["c_sin"][:, r0:r0 + 128])
                kb.dma("sync", mixed[:, 0:256], mixA[r0:r0 + 128, 0:256])
                kb.dma("sync", mixed[:, 512:1024], mixA[r0:r0 + 128, 256:768])
                prenorm_T(xtb, g1b, xn, hT, pP, ss, rs, t1s, junk)
                for which in range(2):
                    for h in range(4):
                        for k in range(8):
                            kb.mm(pq[0:64, h * 128:(h + 1) * 128], winb[:, k, which * 256 + h * 64:which * 256 + (h + 1) * 64],
                                  hT[:, k, :], start=(k == 0), stop=(k == 7))
                    kb.cp("scalar", qraw, pq[0:64, :])
                    kb.mm(pPf[0:64, :], protb, qraw)
                    kb.tt("vector", v3(r1), v3(qraw), cosb.bc(1, [64, 4, 128]), ALU.mult)
                    kb.tt("vector", v3(r2), v3(pPf[0:64, :]), sinb.bc(1, [64, 4, 128]), ALU.mult)
                    dst = qrb if which == 0 else KT[:, :, r0:r0 + 128]
                    kb.tt("gpsimd", dst, v3(r1), v3(r2), ALU.add)
                for k in range(8):
                    kb.mm(pq[:, 0:256], hT[:, k, :], winb[:, k, 512:768], start=(k == 0), stop=(k == 7))
                kb.cp("scalar", Vaug[:, b, :, 0:64], v3(pq[:, 0:256]))

            def stage_A(b):
                mixed = mixeds[b % 3]; qrb = qrs[b % 2]
                kb.memset("vector", pA[0], 0.0)
                kb.memset("vector", pA[1], 0.0)
                for g0 in range(0, b + 1, 4):
                    kbs = list(range(g0, min(b + 1, g0 + 4)))
                    n = len(kbs)
                    for hmi in range(8):
                        h, mp = hmi // 2, hmi % 2
                        ps_ = pS[nexp[0] % 2]
                        pt_ = PTt[nexp[0] % 2]
                        nexp[0] += 1
                        for i, kbi in enumerate(kbs):
                            kb.mm(ps_[:, i * 128:(i + 1) * 128], KT[mp * 32:(mp + 1) * 32, h, kbi * 128:(kbi + 1) * 128],
                                  qrb[mp * 32:(mp + 1) * 32, h, :])
                        kb.act(pt_[:, 0:n * 128], ps_[:, 0:n * 128], AF.Exp, scale=sc)
                        if kbs[-1] == b:
                            i = n - 1
                            kb.memset("gpsimd", pt_[64:128, i * 128:i * 128 + 64], 0.0)
                        for i, kbi in enumerate(kbs):
                            kb.mm(pA[hmi // 4][:, (hmi % 4) * 68:(hmi % 4) * 68 + 65], pt_[:, i * 128:(i + 1) * 128],
                                  Vaug[:, kbi, h, 0:65], start=False, stop=(kbi == b), skip=True)
                for i in range(2):
                    a3 = pA[i][:, 0:272].re("p (h d) -> p h d", h=4)
                    kb.recip(rec8[:, i * 4:(i + 1) * 4], a3[:, :, 64])
                    kb.tt("vector", on[:, i * 4:(i + 1) * 4, :], a3[:, :, 0:64], rec8[:, i * 4:(i + 1) * 4].bc(2, [128, 4, 64]), ALU.mult)
                on4 = on.re("p (h m) d -> p h m d", m=2)
                kb.stt(v3(od), on4[:, :, 1, :], neglam[:, 0:1], on4[:, :, 0, :], ALU.mult, ALU.add)
                kb.tt("gpsimd", sq, od, od, ALU.mult)
                kb.red(ss4, v3(sq))
                kb.rstd(rs4, ss4, 1.0 / 64, epsv, tm4)
                kb.tt("vector", v3(od), v3(od), rs4.bc(2, [128, 4, 64]), ALU.mult)
                kb.tt("vector", v3(mixed[:, 256:512]), v3(od), gdf.bc(1, [128, 4, 64]), ALU.mult)

            def stage_O(b):
                r0 = b * 128
                xtb = xts[b % 3]; mixed = mixeds[b % 3]
                if dbg:
                    kb.cp("vector", mdbg, mixed)
                    kb.dma("sync", dbg_mixed[r0:r0 + 128, :], mdbg)
                for k in range(8):
                    kb.tp(pYb[:, k * 128:(k + 1) * 128], mixed[:, k * 128:(k + 1) * 128], identb)
                kb.cp("scalar", mT, pYb.re("p (k n) -> p k n", k=8))
                for c in range(2):
                    for k in range(8):
                        kb.mm(pY[c], mT[:, k, :], woutb[:, k, c * 512:(c + 1) * 512], start=(k == 0), stop=(k == 7))
                post_res(pY[0], pY[1], xtb, g2b, tmp, q2, rsb, t1b, junk2)
                kb.dma("sync", xs[r0:r0 + 128, :], xtb)

            def rec_of(fn, b):
                S.rec = []
                fn(b)
                l_ = S.rec
                S.rec = None
                return l_

            stage_P(0)
            for b in range(nblk):
                lists = [rec_of(stage_A, b)]
                if b + 1 < nblk:
                    lists.append(rec_of(stage_P, b + 1))
                if b > 0:
                    lists.append(rec_of(stage_O, b - 1))
                S.interleave(lists)
            stage_O(nblk - 1)
            S.barrier()

    if "ffn1" not in stages:
        for r0 in range(0, S_LEN, 512):
            kb.dma("sync", out[r0:r0 + 512, :], x_in[r0:r0 + 512, :])
    for l in range(nlayers):
        src = x_in if l == 0 else out
        if "ffn1" in stages:
            ffn_phase(l, 1, src, out)
        if "mixa" in stages:
            mixa_phase(l, out)
        if "mixb" in stages:
            mixb_phase(l, out)
        if "ffn2" in stages:
            ffn_phase(l, 2, out, out)
    S.wait_res("sync", [out.r] + ([dbg_mixed.r] if dbg else []))
    es0.close()
    return nc, hc


_CACHE = {}


def kernel(**inputs):
    x = np.ascontiguousarray(np.asarray(inputs["x"], dtype=np.float32))
    nb = x.shape[0]
    if "nc" not in _CACHE:
        _CACHE["nc"] = build()
    nc, hc = _CACHE["nc"]
    shared = {n: np.ascontiguousarray(np.asarray(inputs[n], dtype=np.float32)) for n, _ in W_SPECS}
    shared.update(hc)
    in_maps = []
    for b in range(nb):
        m = dict(shared)
        m["x"] = x[b]
        in_maps.append(m)
    res = run_bass_kernel_spmd(nc, in_maps, core_ids=list(range(nb)))
    return np.stack([np.asarray(r["out"], dtype=np.float32) for r in res.results], axis=0)
```

```python
import math
from contextlib import ExitStack

import numpy as np
import ml_dtypes
import concourse.bass as bass
import concourse.mybir as mybir
from concourse.bass_utils import run_bass_kernel_spmd

F32 = mybir.dt.float32
BF16 = mybir.dt.bfloat16
AF = mybir.ActivationFunctionType
ALU = mybir.AluOpType
AX = mybir.AxisListType

S_LEN = 4096
D = 1024
DFF = 2816
INC = 3860
NB = S_LEN // 128
EPS = 1e-6
NEG = -30000.0


class Res:
    __slots__ = ("lw", "rd", "excl", "pe")

    def __init__(self):
        self.lw = None
        self.rd = []
        self.excl = False
        self.pe = None


class V:
    __slots__ = ("ap", "r")

    def __init__(self, ap, r=None):
        self.ap = ap
        self.r = r if r is not None else Res()

    def __getitem__(self, k):
        return V(self.ap[k], self.r)

    def re(self, pat, **kw):
        return V(self.ap.rearrange(pat, **kw), self.r)

    def bc(self, axis, shape):
        return V(self.ap.unsqueeze(axis).broadcast_to(list(shape)), self.r)


class Sched:
    ENG = ("tensor", "vector", "scalar", "gpsimd", "sync")
    NSLOT = 8

    def __init__(self, nc, es):
        self.nc = nc
        self.eng = {e: getattr(nc, e) for e in self.ENG}
        self.sem = {e: es.enter_context(nc.semaphore("s_" + e)) for e in self.ENG}
        self.cnt = {e: 0 for e in self.ENG}
        self.known = {e: {} for e in self.ENG}
        self.semobj = {}
        self.maxv = {}
        self.rec = None
        self.dq = {}
        for q in ("sync", "gpsimd", "scalar"):
            self.dq[q] = dict(
                sems=[es.enter_context(nc.semaphore("d_%s%d" % (q, i))) for i in range(self.NSLOT)], n=0)

    def _wait(self, e, tok):
        sem, val = tok
        k = self.known[e]
        if k.get(id(sem), 0) >= val:
            return
        k[id(sem)] = val
        self.eng[e].wait_ge(sem, val)

    def _deps(self, e, reads, writes):
        mysem = self.sem.get(e)
        for r in reads:
            if r.lw is not None:
                if not (e == "tensor" and r.lw[0] is mysem):
                    self._wait(e, r.lw)
        for w in writes:
            if w.lw is not None:
                if not (e == "tensor" and w.lw[0] is mysem):
                    self._wait(e, w.lw)
            for t in w.rd:
                if not (e == "tensor" and t[0] is mysem):
                    self._wait(e, t)

    def _commit(self, tok, reads, writes):
        self.semobj[id(tok[0])] = tok[0]
        self.maxv[id(tok[0])] = max(self.maxv.get(id(tok[0]), 0), tok[1])
        for r in reads:
            r.rd.append(tok)
            if len(r.rd) > 12:
                best = {}
                for t in r.rd:
                    if t[1] > best.get(id(t[0]), (None, -1))[1]:
                        best[id(t[0])] = t
                r.rd = list(best.values())
        for w in writes:
            w.lw = tok
            w.rd = []

    def op(self, e, fn, reads=(), writes=(), pe=None):
        if self.rec is not None:
            self.rec.append(("op", e, fn, reads, writes, pe))
            return None
        writes = list(writes) + [r for r in reads if r.excl]
        reads = [r for r in reads if not r.excl]
        if pe is not None:
            res, rb = pe
            prev = res.pe
            if prev is not None and prev[0] != rb:
                self._wait(e, prev[1])
        self._deps(e, reads, writes)
        ins = fn(self.eng[e])
        self.cnt[e] += 1
        ins.then_inc(self.sem[e], 1)
        tok = (self.sem[e], self.cnt[e])
        self._commit(tok, reads, writes)
        if pe is not None:
            pe[0].pe = (pe[1], tok)
        return tok

    def dma(self, q, out, in_, reads=(), writes=(), **kw):
        if self.rec is not None:
            self.rec.append(("dma", q, out, in_, reads, writes, kw))
            return None
        d = self.dq[q]
        n = d["n"]
        sem = d["sems"][n % self.NSLOT]
        if n >= self.NSLOT:
            self._wait(q, (sem, 16 * (n // self.NSLOT)))
        self._deps(q, reads, writes)
        ins = self.eng[q].dma_start(out=out, in_=in_, **kw)
        ins.then_inc(sem, 16)
        d["n"] = n + 1
        tok = (sem, 16 * (n // self.NSLOT + 1))
        self._commit(tok, reads, writes)
        return tok

    def emit(self, item):
        if item[0] == "op":
            _, e, fn, reads, writes, pe = item
            self.op(e, fn, reads, writes, pe=pe)
        else:
            _, q, out, in_, reads, writes, kw = item
            self.dma(q, out, in_, reads=reads, writes=writes, **kw)

    def interleave(self, lists):
        assert self.rec is None
        lists = [l for l in lists if l]
        pos = [0] * len(lists)
        total = sum(len(l) for l in lists)
        for _ in range(total):
            best, bf = None, None
            for i, l in enumerate(lists):
                if pos[i] < len(l):
                    f = pos[i] / float(len(l))
                    if bf is None or f < bf:
                        best, bf = i, f
            self.emit(lists[best][pos[best]])
            pos[best] += 1

    def barrier(self, skip_queues=("gpsimd",)):
        skip = set()
        for q in skip_queues:
            for s in self.dq[q]["sems"]:
                skip.add(id(s))
        for e in self.ENG:
            for sid, v in self.maxv.items():
                if sid in skip:
                    continue
                self._wait(e, (self.semobj[sid], v))

    def wait_res(self, e, ress):
        for r in ress:
            if r.lw is not None:
                self._wait(e, r.lw)
            for t in r.rd:
                self._wait(e, t)


class KB:
    def __init__(self, nc, es):
        self.nc = nc
        self.S = Sched(nc, es)
        self.ncnt = 0

    def _nm(self, n):
        self.ncnt += 1
        return "%s_%d" % (n, self.ncnt)

    def sb(self, es, name, shape, dt):
        t = es.enter_context(self.nc.sbuf_tensor(self._nm(name), list(shape), dt))
        return V(t[:])

    def ps(self, es, name, shape, dt):
        t = es.enter_context(self.nc.psum_tensor(self._nm(name), list(shape), dt))
        v = V(t[:])
        v.r.excl = True
        return v

    def dram(self, name, shape, dt, kind=None):
        if kind is None:
            t = self.nc.dram_tensor(name, list(shape), dt)
        else:
            t = self.nc.dram_tensor(name, list(shape), dt, kind=kind)
        return V(t.ap())

    def mm(self, out, lhsT, rhs, start=True, stop=True, skip=False):
        def _v(x):
            return x() if callable(x) else x
        rb = (_v(lhsT.ap.base_partition), _v(lhsT.ap.partition_size))
        if skip:
            self.S.op("tensor", lambda e: e.matmul(out.ap, lhsT=lhsT.ap, rhs=rhs.ap, start=start, stop=stop, skip_group_check=True),
                      [lhsT.r, rhs.r], [out.r], pe=(out.r, rb))
        else:
            self.S.op("tensor", lambda e: e.matmul(out.ap, lhsT=lhsT.ap, rhs=rhs.ap, start=start, stop=stop),
                      [lhsT.r, rhs.r], [out.r], pe=(out.r, rb))

    def tp(self, out, in_, ident):
        def _v(x):
            return x() if callable(x) else x
        rb = (_v(in_.ap.base_partition), _v(in_.ap.partition_size))
        self.S.op("tensor", lambda e: e.transpose(out=out.ap, in_=in_.ap, identity=ident.ap),
                  [in_.r, ident.r], [out.r], pe=(out.r, rb))

    def act(self, out, in_, func, scale=1.0, bias=None, accum=None):
        rd = [in_.r]
        wr = [out.r]
        kw = {}
        if bias is not None:
            if isinstance(bias, V):
                kw["bias"] = bias.ap
                rd.append(bias.r)
            else:
                kw["bias"] = bias
        if isinstance(scale, V):
            rd.append(scale.r)
            kw["scale"] = scale.ap
        else:
            kw["scale"] = scale
        if accum is not None:
            kw["accum_out"] = accum.ap
            wr.append(accum.r)
        self.S.op("scalar", lambda e: e.activation(out=out.ap, in_=in_.ap, func=func, **kw), rd, wr)

    def tt(self, eng, out, a, b, op):
        self.S.op(eng, lambda e: e.tensor_tensor(out=out.ap, in0=a.ap, in1=b.ap, op=op), [a.r, b.r], [out.r])

    def ts(self, eng, out, a, s1, s2=None, op0=ALU.mult, op1=None):
        rd = [a.r]
        s1a = s1
        if isinstance(s1, V):
            rd.append(s1.r)
            s1a = s1.ap
        s2a = s2
        if isinstance(s2, V):
            rd.append(s2.r)
            s2a = s2.ap
        if op1 is None:
            self.S.op(eng, lambda e: e.tensor_scalar(out=out.ap, in0=a.ap, scalar1=s1a, scalar2=None, op0=op0), rd, [out.r])
        else:
            self.S.op(eng, lambda e: e.tensor_scalar(out=out.ap, in0=a.ap, scalar1=s1a, scalar2=s2a, op0=op0, op1=op1), rd, [out.r])

    def stt(self, out, a, scalar, b, op0, op1):
        rd = [a.r, b.r]
        sa = scalar
        if isinstance(scalar, V):
            rd.append(scalar.r)
            sa = scalar.ap
        self.S.op("vector", lambda e: e.scalar_tensor_tensor(out=out.ap, in0=a.ap, scalar=sa, in1=b.ap, op0=op0, op1=op1), rd, [out.r])

    def cp(self, eng, out, in_):
        if eng == "scalar":
            self.S.op("scalar", lambda e: e.copy(out=out.ap, in_=in_.ap), [in_.r], [out.r])
        else:
            self.S.op(eng, lambda e: e.tensor_copy(out=out.ap, in_=in_.ap), [in_.r], [out.r])

    def red(self, out, in_, op=ALU.add):
        self.S.op("vector", lambda e: e.tensor_reduce(out=out.ap, in_=in_.ap, axis=AX.X, op=op), [in_.r], [out.r])

    def recip(self, out, in_):
        self.S.op("vector", lambda e: e.reciprocal(out=out.ap, in_=in_.ap), [in_.r], [out.r])

    def memset(self, eng, out, val):
        self.S.op(eng, lambda e: e.memset(out.ap, val), [], [out.r])

    def dma(self, q, out, in_, **kw):
        self.S.dma(q, out.ap, in_.ap, reads=[in_.r], writes=[out.r], **kw)

    def rstd(self, out, ss, inv_n, epsv, tmp):
        self.act(tmp, ss, AF.Ln, scale=inv_n, bias=epsv)
        self.act(out, tmp, AF.Exp, scale=-0.5)


def host_consts():
    c = {}
    idx = np.arange(128)
    same = (idx[:, None] // 64) == (idx[None, :] // 64)
    c["c_ident"] = np.eye(128, dtype=np.float32)
    c["c_tri"] = (same & (idx[:, None] <= idx[None, :])).astype(np.float32)
    c["c_su"] = (same & (idx[:, None] > idx[None, :])).astype(np.float32)
    c["c_onesbd"] = same.astype(np.float32)
    mbt = np.where(same & (idx[:, None] <= idx[None, :]), 0.0, NEG).astype(np.float32)
    mbs = np.where(same & (idx[None, :] < idx[:, None]), 0.0, NEG).astype(np.float32)
    c["c_mbt4"] = np.tile(mbt, (1, 4))
    c["c_mbs4"] = np.tile(mbs, (1, 4))
    c["c_ident4"] = np.tile(np.eye(128, dtype=np.float32), (1, 4))
    c["c_bo"] = same.astype(np.float32)
    chi = np.zeros((128, 2), np.float32)
    chi[:64, 0] = 1
    chi[64:, 1] = 1
    c["c_chi"] = chi
    chj = np.zeros((2, 128, 128), np.float32)
    chj[0, :64, :] = 1
    chj[1, 64:, :] = 1
    c["c_chj"] = chj
    p = np.arange(64)
    dloc = p % 32
    perm = np.where(dloc < 16, p + 16, p - 16)
    prot = np.zeros((64, 64), np.float32)
    prot[perm, p] = 1.0
    c["c_prot"] = prot
    inv_freq = (10000.0 ** (-(np.arange(0, 32, 2, dtype=np.float32)) / np.float32(32))).astype(np.float32)
    ang = (np.arange(S_LEN, dtype=np.float32)[:, None] * inv_freq[None, :]).astype(np.float32)
    cos = np.cos(ang).astype(np.float32)
    sin = np.sin(ang).astype(np.float32)
    cosT = np.zeros((64, S_LEN), np.float32)
    sinT = np.zeros((64, S_LEN), np.float32)
    for pp in range(64):
        dl = pp % 32
        cosT[pp] = cos[:, dl % 16]
        sinT[pp] = -sin[:, dl] if dl < 16 else sin[:, dl - 16]
    c["c_cos"] = cosT
    c["c_sin"] = sinT
    return c


W_SPECS = [
    ("ffn1_pre_g", [2, 1024]), ("ffn1_w_gate", [2, 1024, 2816]), ("ffn1_w_up", [2, 1024, 2816]),
    ("ffn1_w_down", [2, 2816, 1024]), ("ffn1_post_g", [2, 1024]), ("mix_pre_g", [2, 1024]),
    ("w_in", [2, 1024, 3860]), ("gdn_conv_w", [2, 4, 768]), ("gdn_a_log", [2, 4]), ("gdn_dt_bias", [2, 4]),
    ("gdn_norm_g", [2, 64]), ("diff_lam_q1", [2, 32]), ("diff_lam_k1", [2, 32]), ("diff_lam_q2", [2, 32]),
    ("diff_lam_k2", [2, 32]), ("diff_norm_g", [2, 64]), ("ssd_conv_w", [2, 4, 768]), ("ssd_conv_b", [2, 768]),
    ("ssd_a_log", [2, 4]), ("ssd_dt_bias", [2, 4]), ("ssd_d", [2, 4]), ("ssd_norm_g", [2, 256]),
    ("mlstm_i_bias", [2, 4]), ("mlstm_f_bias", [2, 4]), ("mlstm_norm_g", [2, 256]), ("w_out", [2, 1024, 1024]),
    ("mix_post_g", [2, 1024]), ("ffn2_pre_g", [2, 1024]), ("ffn2_w_gate", [2, 1024, 2816]),
    ("ffn2_w_up", [2, 1024, 2816]), ("ffn2_w_down", [2, 2816, 1024]), ("ffn2_post_g", [2, 1024]),
]


def build(nlayers=2, stages=("ffn1", "mixa", "mixb", "ffn2"), dbg=False, nblk=NB, upto=99):
    nc = bass.Bass("TRN2", target_bir_lowering=False)
    es0 = ExitStack()
    kb = KB(nc, es0)
    S = kb.S
    x_in = kb.dram("x", [S_LEN, D], F32, "ExternalInput")
    out = kb.dram("out", [S_LEN, D], F32, "ExternalOutput")
    W = {n: kb.dram(n, s, F32, "ExternalInput") for n, s in W_SPECS}
    hc = host_consts()
    C = {n: kb.dram(n, list(a.shape), F32, "ExternalInput") for n, a in hc.items()}
    mixA = kb.dram("mixA", [S_LEN, 768], BF16)
    dbg_mixed = kb.dram("dbg_mixed", [S_LEN, D], F32, "ExternalOutput") if dbg else None

    Wb = {}
    for l in range(nlayers):
        for f in (1, 2):
            for nm, shp in (("w_gate", [D, DFF]), ("w_up", [D, DFF]), ("w_down", [DFF, D])):
                Wb[(l, "ffn%d_%s" % (f, nm))] = kb.dram("wb_%d_ffn%d_%s" % (l, f, nm), shp, BF16)
        Wb[(l, "w_in")] = kb.dram("wb_%d_w_in" % l, [D, INC], BF16)
        Wb[(l, "w_out")] = kb.dram("wb_%d_w_out" % l, [D, D], BF16)

    def cast_weight(l, name):
        src = W[name]
        dst = Wb[(l, name)]
        rows = src.ap.shape[1]
        for r0 in range(0, rows, 256):
            r1 = min(rows, r0 + 256)
            kb.dma("gpsimd", dst[r0:r1, :], V(src.ap[l], src.r)[r0:r1, :])

    for l in range(nlayers):
        for name in ("ffn1_w_gate", "ffn1_w_up", "ffn1_w_down", "w_in", "w_out", "ffn2_w_gate", "ffn2_w_up", "ffn2_w_down"):
            cast_weight(l, name)

    cs = {}
    for n in ("c_ident", "c_tri", "c_su", "c_onesbd", "c_bo"):
        cs[n] = kb.sb(es0, n, [128, 128], F32)
        kb.dma("sync", cs[n], C[n])
    for n in ("c_mbt4", "c_mbs4", "c_ident4"):
        cs[n] = kb.sb(es0, n, [128, 512], F32)
        kb.dma("sync", cs[n], C[n])
    cs["c_chi"] = kb.sb(es0, "c_chi", [128, 2], F32)
    kb.dma("sync", cs["c_chi"], C["c_chi"])
    cs["c_chj"] = kb.sb(es0, "c_chj", [128, 2, 128], F32)
    kb.dma("sync", cs["c_chj"], C["c_chj"].re("j k c -> k j c"))
    identb = kb.sb(es0, "identb", [128, 128], BF16)
    kb.cp("vector", identb, cs["c_ident"])
    protf = kb.sb(es0, "protf", [64, 64], F32)
    kb.dma("sync", protf, C["c_prot"])
    protb = kb.sb(es0, "protb", [64, 64], BF16)
    kb.cp("vector", protb, protf)
    epsv = kb.sb(es0, "epsv", [128, 1], F32)
    kb.memset("vector", epsv, EPS)
    ident = cs["c_ident"]

    def bload(es, name, src_ap_v, n, scale=None):
        t = kb.sb(es, name, [128, n], F32)
        kb.S.dma("sync", t.ap, src_ap_v.ap.partition_broadcast(128), reads=[src_ap_v.r], writes=[t.r])
        if scale is not None:
            kb.ts("vector", t, t, scale, None, ALU.mult)
        return t

    def wl(name, l):
        w = W[name]
        return V(w.ap[l], w.r)

    def prenorm_T(xt, g1b, xn, hT_dst, pT, ss, rs, tmp1, junk):
        kb.memset("gpsimd", ss, 0.0)
        kb.act(junk, xt, AF.Square, accum=ss)
        kb.rstd(rs, ss, 1.0 / D, epsv, tmp1)
        kb.stt(xn, xt, rs[:, 0:1], g1b, ALU.mult, ALU.mult)
        for k in range(8):
            kb.tp(pT[:, k * 128:(k + 1) * 128], xn[:, k * 128:(k + 1) * 128], identb)
        kb.cp("scalar", hT_dst, pT.re("p (k n) -> p k n", k=8))

    def post_res(ya, yb, xt, g2b, tmp, q2, rs2, tmp1, junk):
        kb.memset("gpsimd", q2, 0.0)
        kb.act(junk[:, 0:512], ya, AF.Square, accum=q2[:, 0:1])
        kb.act(junk[:, 512:1024], yb, AF.Square, accum=q2[:, 1:2])
        kb.tt("vector", q2[:, 0:1], q2[:, 0:1], q2[:, 1:2], ALU.add)
        kb.rstd(rs2, q2[:, 0:1], 1.0 / D, epsv, tmp1)
        kb.tt("vector", tmp[:, 0:512], ya, g2b[:, 0:512], ALU.mult)
        kb.tt("vector", tmp[:, 512:1024], yb, g2b[:, 512:1024], ALU.mult)
        kb.stt(xt, tmp, rs2[:, 0:1], xt, ALU.mult, ALU.add)

    def ffn_phase(l, f, src, dst):
        pre = "ffn%d_" % f
        with ExitStack() as es:
            g1b = bload(es, "g1b", wl(pre + "pre_g", l), D)
            g2b = bload(es, "g2b", wl(pre + "post_g", l), D, scale=0.5)
            xt = [[kb.sb(es, "xt", [128, D], F32) for s in range(4)] for par in range(2)]
            xn = kb.sb(es, "xn", [128, D], BF16)
            junk = kb.sb(es, "junk", [128, D], BF16)
            tmp = kb.sb(es, "tmp", [128, D], F32)
            hT = kb.sb(es, "hT", [128, 8, 512], BF16)
            actb = kb.sb(es, "actb", [128, 22, 512], BF16)
            slabs = [kb.sb(es, "slab", [128, 11264], BF16) for i in range(4)]
            sg = [kb.sb(es, "sg", [128, 512], F32) for i in range(2)]
            ysb = [kb.sb(es, "ysb", [128, 512], F32) for i in range(4)]
            ss = kb.sb(es, "ss", [128, 1], F32)
            rs = kb.sb(es, "rs", [128, 1], F32)
            t1 = kb.sb(es, "t1", [128, 1], F32)
            q2 = kb.sb(es, "q2", [128, 2], F32)
            pT = [kb.ps(es, "pT", [128, 1024], BF16) for i in range(2)]
            pG = [kb.ps(es, "pG", [128, 512], F32) for i in range(2)]
            pU = [kb.ps(es, "pU", [128, 512], F32) for i in range(2)]
            pY = [kb.ps(es, "pY", [128, 512], F32) for i in range(2)]
            wg = Wb[(l, pre + "w_gate")].re("(k p) n -> p k n", p=128)
            wu = Wb[(l, pre + "w_up")].re("(k p) n -> p k n", p=128)
            wd = Wb[(l, pre + "w_down")].re("(f p) n -> p f n", p=128)
            nslab = [0]

            def load_slab(kind, half):
                sl = slabs[nslab[0] % 4]
                nslab[0] += 1
                if kind == "g":
                    v = sl.re("p (k n) -> p k n", k=8)
                    kb.dma("sync", v, wg[:, :, half * 1408:(half + 1) * 1408])
                elif kind == "u":
                    v = sl.re("p (k n) -> p k n", k=8)
                    kb.dma("sync", v, wu[:, :, half * 1408:(half + 1) * 1408])
                else:
                    v = sl.re("p (f n) -> p f n", f=22)
                    kb.dma("sync", v, wd[:, :, half * 512:(half + 1) * 512])
                return v

            def load_x(t):
                for s in range(4):
                    r0 = (t * 4 + s) * 128
                    kb.dma("scalar", xt[t % 2][s], src[r0:r0 + 128, :])

            hTs = [hT, kb.sb(es, "hT2", [128, 8, 512], BF16)]
            junk2 = kb.sb(es, "junk2", [128, D], BF16)
            ssb = kb.sb(es, "ssb", [128, 1], F32); rsb = kb.sb(es, "rsb", [128, 1], F32); t1b = kb.sb(es, "t1b", [128, 1], F32)

            def prenorm_tile(t):
                for s in range(4):
                    prenorm_T(xt[t % 2][s], g1b, xn, hTs[t % 2][:, :, s * 128:(s + 1) * 128], pT[s % 2], ss, rs, t1, junk)

            load_x(0)
            NT = S_LEN // 512
            prenorm_tile(0)
            for t in range(NT):
                par = t % 2
                hTt = hTs[par]
                sg0 = load_slab("g", 0)
                su0 = load_slab("u", 0)
                if t + 1 < NT:
                    load_x(t + 1)
                sg1 = load_slab("g", 1)
                su1 = load_slab("u", 1)
                for half, (sgw, suw) in enumerate(((sg0, su0), (sg1, su1))):
                    for fi in range(11):
                        fc = half * 11 + fi
                        pg = pG[fc % 2]
                        pu = pU[fc % 2]
                        for k in range(8):
                            kb.mm(pg, sgw[:, k, fi * 128:(fi + 1) * 128], hTt[:, k, :], start=(k == 0), stop=(k == 7))
                        for k in range(8):
                            kb.mm(pu, suw[:, k, fi * 128:(fi + 1) * 128], hTt[:, k, :], start=(k == 0), stop=(k == 7))
                        kb.act(sg[fc % 2], pg, AF.Silu)
                        kb.tt("vector", actb[:, fc, :], sg[fc % 2], pu, ALU.mult)
                    if half == 0:
                        sd0 = load_slab("d", 0)
                sd1 = load_slab("d", 1)
                lists = []
                S.rec = []
                for s in range(4):
                    py = pY[s % 2]
                    for fc in range(22):
                        kb.mm(py, actb[:, fc, s * 128:(s + 1) * 128], sd0[:, fc, :], start=(fc == 0), stop=(fc == 21))
                    kb.cp("scalar", ysb[s], py)
                for s in range(4):
                    py = pY[s % 2]
                    for fc in range(22):
                        kb.mm(py, actb[:, fc, s * 128:(s + 1) * 128], sd1[:, fc, :], start=(fc == 0), stop=(fc == 21))
                    post_res(ysb[s], py, xt[par][s], g2b, tmp, q2, rsb, t1b, junk2)
                    r0 = (t * 4 + s) * 128
                    kb.dma("scalar", dst[r0:r0 + 128, :], xt[par][s])
                lists.append(S.rec)
                S.rec = None
                if t + 1 < NT:
                    S.rec = []
                    prenorm_tile(t + 1)
                    lists.append(S.rec)
                    S.rec = None
                S.interleave(lists)
            S.barrier()

    def mixa_phase(l, xs):
        with ExitStack() as es:
            sb = lambda n, shp, dt=F32: kb.sb(es, n, shp, dt)
            g1b = bload(es, "g1b", wl("mix_pre_g", l), D)
            winb = sb("winb", [128, 8, INC], BF16)
            wsrc = Wb[(l, "w_in")].re("(k p) n -> p k n", p=128)
            for k in range(8):
                kb.dma("sync", winb[:, k, :], wsrc[:, k, :])
            gcw = sb("gcw", [128, 6, 4])
            scw = sb("scw", [128, 6, 4])
            scb = sb("scb", [128, 6])
            for j in range(4):
                kb.dma("sync", gcw[:, :, j], wl("gdn_conv_w", l)[j].re("(c p) -> p c", p=128), allow_slow_non_contiguous=True)
                kb.dma("sync", scw[:, :, j], wl("ssd_conv_w", l)[j].re("(c p) -> p c", p=128), allow_slow_non_contiguous=True)
            kb.dma("sync", scb, wl("ssd_conv_b", l).re("(c p) -> p c", p=128), allow_slow_non_contiguous=True)
            negA_g = bload(es, "negA_g", wl("gdn_a_log", l), 4)
            kb.act(negA_g, negA_g, AF.Exp)
            kb.ts("vector", negA_g, negA_g, -1.0, None, ALU.mult)
            negA_s = bload(es, "negA_s", wl("ssd_a_log", l), 4)
            kb.act(negA_s, negA_s, AF.Exp)
            kb.ts("vector", negA_s, negA_s, -1.0, None, ALU.mult)
            dtb_g = bload(es, "dtb_g", wl("gdn_dt_bias", l), 4)
            dtb_s = bload(es, "dtb_s", wl("ssd_dt_bias", l), 4)
            dsk = bload(es, "dsk", wl("ssd_d", l), 4)
            ibias = bload(es, "ibias", wl("mlstm_i_bias", l), 4)
            fbias = bload(es, "fbias", wl("mlstm_f_bias", l), 4)
            gng = bload(es, "gng", wl("gdn_norm_g", l), 64)
            gssd = bload(es, "gssd", wl("ssd_norm_g", l), 256)
            gml = bload(es, "gml", wl("mlstm_norm_g", l), 256)

            TRI, SU, ONESBD, BO = cs["c_tri"], cs["c_su"], cs["c_onesbd"], cs["c_bo"]
            NI4 = sb("NI4", [128, 512])
            kb.ts("vector", NI4, cs["c_ident4"], -1.0, 1.0, ALU.mult, ALU.add)
            MBT4, ID4, CHI, CHJ = cs["c_mbt4"], cs["c_ident4"], cs["c_chi"], cs["c_chj"]

            xt = sb("xt", [128, D])
            xn = sb("xn", [128, D], BF16)
            junk = sb("junk", [128, D], BF16)
            hTs = [sb("hT", [128, 8, 128], BF16) for i in range(2)]
            ss = sb("ss", [128, 1]); rs = sb("rs", [128, 1]); t1s = sb("t1s", [128, 1])
            mixos = [sb("mixo", [128, 768], BF16) for i in range(2)]
            ptp = kb.ps(es, "ptp", [128, 1024], BF16)

            def bfv(bank):
                return V(bank.ap.bitcast(BF16), bank.r)

            v3 = lambda v, h=4: v.re("p (h d) -> p h d", h=h)
            hs = lambda h: slice(h * 128, (h + 1) * 128)
            hp = lambda h: slice((h % 2) * 64, (h % 2) * 64 + 64)

            def proj_fm(pout, col0, hT, M=128):
                for k in range(8):
                    kb.mm(pout, winb[:, k, col0:col0 + M], hT[:, k, :], start=(k == 0), stop=(k == 7))

            def proj_tm(pout, col0, n, hT):
                for k in range(8):
                    kb.mm(pout, hT[:, k, :], winb[:, k, col0:col0 + n], start=(k == 0), stop=(k == 7))

            def conv_chunk(praw, raw, w, bias, ac, outv):
                kb.cp("scalar", raw[:, 3:131], praw)
                if bias is None:
                    kb.ts("vector", ac, raw[:, 3:131], w[:, 3:4], None, ALU.mult)
                else:
                    kb.ts("vector", ac, raw[:, 3:131], w[:, 3:4], bias, ALU.mult, ALU.add)
                for j in (2, 1, 0):
                    kb.stt(ac, raw[:, j:j + 128], w[:, j:j + 1], ac, ALU.mult, ALU.add)
                kb.cp("gpsimd", raw[:, 0:3], raw[:, 128:131])
                kb.act(outv, ac, AF.Silu)

            def decay_prep(glog, G_all, psmall, pbig):
                kb.mm(psmall[:, 0:4], TRI, glog)
                kb.mm(psmall[:, 4:8], SU, glog)
                kb.mm(psmall[:, 8:12], ONESBD, glog)
                kb.tt("vector", v3(G_all), TRI.bc(1, [128, 4, 128]), glog.bc(2, [128, 4, 128]), ALU.mult)
                kb.mm(pbig, SU, G_all, start=True, stop=False)
                kb.mm(pbig, ident, MBT4, start=False, stop=True)

            class G:
                pass
            g = G()
            g.A = kb.ps(es, "gA", [128, 512], F32); g.B = kb.ps(es, "gB", [128, 512], F32); g.C = kb.ps(es, "gC", [128, 512], F32)
            g.raw = sb("rawG", [128, 6, 131]); kb.memset("vector", g.raw, 0.0)
            g.acc = [sb("gacc", [128, 128]) for i in range(2)]
            g.gF = sb("gF", [128, 4, 128]); g.sq4 = sb("sq4", [128, 4, 128]); g.rn4 = sb("rn4", [128, 4, 128])
            g.qn = sb("qn", [128, 2, 128], BF16); g.kn = sb("kn", [128, 2, 128], BF16); g.vFb = sb("vFb", [128, 2, 128], BF16)
            g.sgate = sb("sgate", [128, 256]); g.beta = sb("beta", [128, 4]); g.a4 = sb("ga4", [128, 4]); g.glog = sb("gglog", [128, 4])
            g.cbg = sb("cbg", [128, 4]); g.E = sb("gE", [128, 12]); g.G_all = sb("gG_all", [128, 512]); g.DT_all = sb("gDT_all", [128, 512])
            g.Kbg = sb("Kbg", [128, 256]); g.Vb = sb("Vb", [128, 256]); g.kdec = sb("gkdec", [128, 256], BF16)
            g.Lt = sb("Lt", [128, 512])
            g.P = [sb("P", [128, 512]) for j in range(6)]
            g.PT = [sb("PT", [128, 512]) for j in range(5)]
            g.Y = sb("Y", [128, 512])
            g.U_sb = sb("U_sb", [128, 256]); g.WT_sb = sb("WT_sb", [128, 2, 128], BF16); g.attnT = sb("attnT", [128, 512], BF16)
            g.g_rep = sb("gg_rep", [128, 256]); g.eP = sb("geP", [128, 4]); g.vnew = sb("vnew", [128, 256], BF16)
            g.S32 = sb("S32", [128, 2, 64]); g.Sbf = sb("Sbf", [128, 2, 64], BF16)
            kb.memset("vector", g.S32, 0.0); kb.memset("vector", g.Sbf, 0.0)
            g.t1 = sb("gt1", [128, 256]); g.o_ = sb("go_", [128, 256]); g.sq = sb("gsq", [128, 256])
            g.ss4 = sb("gss4", [128, 4]); g.rs4 = sb("grs4", [128, 4]); g.tm4 = sb("gtm4", [128, 4])

            g.U2 = [g.U_sb, sb("U_sb2", [128, 256])]
            g.WT2 = [g.WT_sb, sb("WT_sb2", [128, 2, 128], BF16)]
            g.at2 = [g.attnT, sb("attnT2", [128, 512], BF16)]
            g.kd2 = [g.kdec, sb("gkdec2", [128, 256], BF16)]
            g.qn2 = [g.qn, sb("qn2", [128, 2, 128], BF16)]
            g.E2 = [g.E, sb("gE2", [128, 12])]
            g.eP2 = [g.eP, sb("geP2", [128, 4])]
            g.sg2 = [g.sgate, sb("sgate2", [128, 256])]

            def gdn_pre(b, hT):
                A, B, C = g.A, g.B, g.C
                g.U_sb, g.WT_sb, g.attnT, g.kdec, g.qn, g.E, g.eP, g.sgate = (g.U2[b % 2], g.WT2[b % 2], g.at2[b % 2], g.kd2[b % 2],
                                                                            g.qn2[b % 2], g.E2[b % 2], g.eP2[b % 2], g.sg2[b % 2])
                Bb = bfv(B)
                gF, kn, qn, vFb, beta, glog, E, DT_all, P, PT, Y = g.gF, g.kn, g.qn, g.vFb, g.beta, g.glog, g.E, g.DT_all, g.P, g.PT, g.Y
                for c in range(6):
                    pj = A if c % 2 == 0 else B
                    proj_fm(pj[:, 0:128], c * 128, hT)
                    outv = gF[:, c, :] if c < 4 else vFb[:, c - 4, :]
                    conv_chunk(pj[:, 0:128], g.raw[:, c, :], gcw[:, c, :], None, g.acc[c % 2], outv)
                kb.tt("gpsimd", g.sq4, gF, gF, ALU.mult)
                for i in range(4):
                    kb.mm(A[:, i * 128:(i + 1) * 128], BO, g.sq4[:, i, :])
                rn4f = g.rn4.re("p a b -> p (a b)")
                kb.act(rn4f, A, AF.Ln, bias=epsv)
                kb.act(rn4f, rn4f, AF.Exp, scale=-0.5)
                kb.stt(qn, gF[:, 0:2, :], 0.125, g.rn4[:, 0:2, :], ALU.mult, ALU.mult)
                kb.tt("vector", kn, gF[:, 2:4, :], g.rn4[:, 2:4, :], ALU.mult)
                kb.tp(Bb[:, 0:128], kn[:, 0, :], identb)
                kb.tp(Bb[:, 128:256], kn[:, 1, :], identb)
                kb.tp(Bb[:, 256:384], vFb[:, 0, :], identb)
                kb.tp(Bb[:, 384:512], vFb[:, 1, :], identb)
                kTM = v3(Bb[:, 0:256]); vTM = v3(Bb[:, 256:512])
                proj_tm(A[:, 0:264], 768, 264, hT)
                kb.act(g.sgate, A[:, 0:256], AF.Silu)
                kb.act(beta, A[:, 256:260], AF.Sigmoid)
                kb.tt("vector", g.a4, A[:, 260:264], dtb_g, ALU.add)
                kb.act(g.a4, g.a4, AF.Exp)
                kb.act(g.a4, g.a4, AF.Ln, bias=1.0)
                kb.tt("vector", glog, g.a4, negA_g, ALU.mult)
                decay_prep(glog, g.G_all, C, A)
                kb.act(E, C[:, 0:12], AF.Exp)
                kb.act(DT_all, A, AF.Exp)
                kb.tt("vector", g.cbg, beta, E[:, 0:4], ALU.mult)
                kb.tt("vector", v3(g.Kbg), kTM, g.cbg.bc(2, [128, 4, 64]), ALU.mult)
                kb.tt("vector", v3(g.Vb), vTM, beta.bc(2, [128, 4, 64]), ALU.mult)
                kb.tt("vector", v3(g.kdec), kTM, E[:, 4:8].bc(2, [128, 4, 64]), ALU.mult)
                for h in range(4):
                    kb.mm(B[:, hs(h)], kn[hp(h), h // 2, :], kn[hp(h), h // 2, :])
                kb.tt("vector", g.Lt, B, DT_all, ALU.mult)
                kb.tt("gpsimd", g.Lt, g.Lt, NI4, ALU.mult)
                for h in range(4):
                    kb.tp(C[:, hs(h)], g.Lt[:, hs(h)], ident)
                kb.tt("vector", v3(P[0]), v3(C), beta.bc(2, [128, 4, 128]), ALU.mult)
                for h in range(4):
                    kb.tp(A[:, hs(h)], P[0][:, hs(h)], ident)
                kb.cp("scalar", PT[0], A)
                kb.tt("vector", Y, ID4, PT[0], ALU.subtract)
                for j in range(5):
                    for h in range(4):
                        kb.mm(B[:, hs(h)], PT[j][:, hs(h)], P[j][:, hs(h)])
                    if j < 4:
                        for h in range(4):
                            kb.mm(C[:, hs(h)], P[j][:, hs(h)], PT[j][:, hs(h)])
                    kb.cp("scalar", P[j + 1], B)
                    if j < 4:
                        kb.cp("vector", PT[j + 1], C)
                    for h in range(4):
                        kb.mm(A[:, hs(h)], P[j + 1][:, hs(h)], Y[:, hs(h)])
                    kb.tt("vector", Y, Y, A, ALU.add)
                for h in range(4):
                    kb.mm(B[:, h * 64:(h + 1) * 64], Y[:, hs(h)], g.Vb[:, h * 64:(h + 1) * 64])
                for h in range(4):
                    kb.mm(C[hp(h), (h // 2) * 128:(h // 2) * 128 + 128], g.Kbg[:, h * 64:(h + 1) * 64], Y[:, hs(h)])
                kb.cp("scalar", g.U_sb, B[:, 0:256])
                kb.cp("vector", g.WT_sb.re("p a b -> p (a b)"), C[:, 0:256])
                for h in range(4):
                    kb.mm(A[:, hs(h)], kn[hp(h), h // 2, :], qn[hp(h), h // 2, :])
                kb.tt("vector", g.attnT, A, DT_all, ALU.mult)
                kb.cp("gpsimd", v3(g.g_rep), glog.bc(2, [128, 4, 64]))
                for pair in range(2):
                    kb.mm(B[:, pair * 2:pair * 2 + 2], g.g_rep[:, pair * 128:(pair + 1) * 128], CHI)
                kb.act(g.eP, B[:, 0:4], AF.Exp)

            def gdn_scan(b, mixo):
                X, Yb = m_.A, m_.B
                U_sb, WT_sb, attnT, kdec, qn, E, eP, sgate = (g.U2[b % 2], g.WT2[b % 2], g.at2[b % 2], g.kd2[b % 2],
                                                              g.qn2[b % 2], g.E2[b % 2], g.eP2[b % 2], g.sg2[b % 2])
                for j in range(2):
                    r = slice(j * 64, j * 64 + 64)
                    for h in range(4):
                        kb.mm(X[r, h * 64:(h + 1) * 64], WT_sb[hp(h), h // 2, r], g.Sbf[hp(h), h // 2, :])
                    kb.tt("vector", g.vnew[r, :], U_sb[r, :], X[r, 0:256], ALU.subtract)
                    for h in range(4):
                        kb.mm(X[r, 256 + h * 64:256 + (h + 1) * 64], qn[hp(h), h // 2, r], g.Sbf[hp(h), h // 2, :])
                    for h in range(4):
                        kb.mm(Yb[hp(h), (h // 2) * 64:(h // 2) * 64 + 64], kdec[r, h * 64:(h + 1) * 64], g.vnew[r, h * 64:(h + 1) * 64])
                    for pair in range(2):
                        kb.stt(g.S32[:, pair, :], g.S32[:, pair, :], eP[:, pair * 2 + j:pair * 2 + j + 1],
                               Yb[:, pair * 64:(pair + 1) * 64], ALU.mult, ALU.add)
                    kb.cp("scalar", g.Sbf, g.S32)
                for h in range(4):
                    kb.mm(Yb[:, 128 + h * 64:128 + (h + 1) * 64], attnT[:, hs(h)], g.vnew[:, h * 64:(h + 1) * 64])
                kb.tt("vector", v3(g.t1), v3(X[:, 256:512]), E[:, 0:4].bc(2, [128, 4, 64]), ALU.mult)
                kb.tt("vector", g.o_, g.t1, Yb[:, 128:384], ALU.add)
                kb.tt("gpsimd", g.sq, g.o_, g.o_, ALU.mult)
                kb.red(g.ss4, v3(g.sq))
                kb.rstd(g.rs4, g.ss4, 1.0 / 64, epsv, g.tm4)
                kb.tt("vector", v3(g.o_), v3(g.o_), g.rs4.bc(2, [128, 4, 64]), ALU.mult)
                kb.tt("gpsimd", v3(g.o_), v3(g.o_), gng.bc(1, [128, 4, 64]), ALU.mult)
                kb.tt("vector", mixo[:, 0:256], g.o_, sgate, ALU.mult)

            s_ = G()
            s_.A = kb.ps(es, "sA", [128, 512], F32); s_.B = kb.ps(es, "sB", [128, 512], F32)
            s_.raw = sb("rawS", [128, 6, 131]); kb.memset("vector", s_.raw, 0.0)
            s_.acc = [sb("sacc", [128, 128]) for i in range(2)]
            s_.sF = sb("sF", [128, 6, 128], BF16)
            s_.xs_tm = sb("xs_tm", [128, 256]); s_.B_tm = sb("B_tm", [128, 256], BF16)
            s_.sz = sb("sz", [128, 256]); s_.a4 = sb("sa4", [128, 4]); s_.dt4 = sb("dt4", [128, 4]); s_.cdt = sb("cdt", [128, 4])
            s_.glog = sb("sglog", [128, 4]); s_.E = sb("sE", [128, 12]); s_.G_all = sb("sG_all", [128, 512]); s_.DT_all = sb("sDT_all", [128, 512])
            s_.etotN = sb("etotN", [128, 8])
            s_.xdt = sb("xdt", [128, 256], BF16); s_.xdec = sb("xdec", [128, 256], BF16); s_.xskip = sb("xskip", [128, 256])
            s_.MT = sb("MT", [128, 512], BF16); s_.y_d = sb("y_d", [128, 256])
            s_.ST32 = sb("ST32", [128, 256]); s_.STbf = sb("STbf", [128, 256], BF16); s_.tmpS = sb("tmpS", [128, 256])
            kb.memset("vector", s_.ST32, 0.0); kb.memset("vector", s_.STbf, 0.0)
            s_.t1 = sb("st1", [128, 256]); s_.y_ = sb("y_", [128, 256]); s_.sq = sb("ssq", [128, 256])
            s_.ss2 = sb("ss2", [128, 2]); s_.rs2 = sb("rs2", [128, 2]); s_.tm2 = sb("tm2", [128, 2])

            def ssd_block(b, hT, mixo):
                A, B = s_.A, s_.B
                Ab = bfv(A)
                sF, E, DT_all, glog = s_.sF, s_.E, s_.DT_all, s_.glog
                for c in range(6):
                    pj = A if c % 2 == 0 else B
                    proj_fm(pj[:, 0:128], 2056 + c * 128, hT)
                    conv_chunk(pj[:, 0:128], s_.raw[:, c, :], scw[:, c, :], scb[:, c:c + 1], s_.acc[c % 2], sF[:, c, :])
                for i in range(4):
                    kb.tp(Ab[:, i * 128:(i + 1) * 128], sF[:, i, :], identb)
                kb.cp("scalar", s_.xs_tm, Ab[:, 0:256])
                kb.cp("vector", s_.B_tm, Ab[:, 256:512])
                proj_tm(B[:, 0:256], 1800, 256, hT)
                proj_tm(B[:, 256:260], 2824, 4, hT)
                kb.act(s_.sz, B[:, 0:256], AF.Silu)
                kb.tt("vector", s_.a4, B[:, 256:260], dtb_s, ALU.add)
                kb.act(s_.a4, s_.a4, AF.Exp)
                kb.act(s_.dt4, s_.a4, AF.Ln, bias=1.0)
                kb.tt("vector", glog, s_.dt4, negA_s, ALU.mult)
                decay_prep(glog, s_.G_all, B, A)
                for j in range(2):
                    kb.mm(B[:, 16 + j * 4:16 + (j + 1) * 4], CHJ[:, j, :], glog)
                kb.act(E, B[:, 0:12], AF.Exp)
                kb.act(s_.etotN, B[:, 16:24], AF.Exp)
                kb.act(DT_all, A, AF.Exp)
                xsTM = v3(s_.xs_tm)
                kb.tt("vector", s_.cdt, s_.dt4, E[:, 4:8], ALU.mult)
                kb.tt("vector", v3(s_.xdt), xsTM, s_.dt4.bc(2, [128, 4, 64]), ALU.mult)
                kb.tt("gpsimd", v3(s_.xdec), xsTM, s_.cdt.bc(2, [128, 4, 64]), ALU.mult)
                kb.tt("gpsimd", v3(s_.xskip), xsTM, dsk.bc(2, [128, 4, 64]), ALU.mult)
                for gi in range(2):
                    kb.mm(A[:, gi * 128:(gi + 1) * 128], sF[:, 2 + gi, :], sF[:, 4 + gi, :])
                MT3 = v3(s_.MT); DT3 = v3(DT_all)
                for gi in range(2):
                    kb.tt("vector", MT3[:, 2 * gi:2 * gi + 2, :], A[:, gi * 128:(gi + 1) * 128].bc(1, [128, 2, 128]),
                          DT3[:, 2 * gi:2 * gi + 2, :], ALU.mult)
                for h in range(4):
                    kb.mm(B[:, h * 64:(h + 1) * 64], s_.MT[:, hs(h)], s_.xdt[:, h * 64:(h + 1) * 64])
                kb.cp("scalar", s_.y_d, B[:, 0:256])
                for j in range(2):
                    r = slice(j * 64, j * 64 + 64)
                    for h in range(4):
                        kb.mm(A[r, h * 64:(h + 1) * 64], sF[:, 4 + h // 2, r], s_.STbf[:, h * 64:(h + 1) * 64])
                    for h in range(4):
                        kb.mm(B[:, h * 64:(h + 1) * 64], s_.B_tm[r, (h // 2) * 128:(h // 2) * 128 + 128], s_.xdec[r, h * 64:(h + 1) * 64])
                    kb.tt("gpsimd", v3(s_.tmpS), v3(s_.ST32), s_.etotN[:, j * 4:(j + 1) * 4].bc(2, [128, 4, 64]), ALU.mult)
                    kb.tt("vector", s_.ST32, s_.tmpS, B[:, 0:256], ALU.add)
                    kb.cp("scalar", s_.STbf, s_.ST32)
                kb.tt("vector", v3(s_.t1), v3(A[:, 0:256]), E[:, 0:4].bc(2, [128, 4, 64]), ALU.mult)
                kb.tt("gpsimd", s_.y_, s_.t1, s_.y_d, ALU.add)
                kb.tt("gpsimd", s_.y_, s_.y_, s_.xskip, ALU.add)
                kb.tt("vector", s_.y_, s_.y_, s_.sz, ALU.mult)
                kb.tt("gpsimd", s_.sq, s_.y_, s_.y_, ALU.mult)
                kb.red(s_.ss2, v3(s_.sq, 2))
                kb.rstd(s_.rs2, s_.ss2, 1.0 / 128, epsv, s_.tm2)
                kb.tt("vector", v3(s_.y_, 2), v3(s_.y_, 2), s_.rs2.bc(2, [128, 2, 128]), ALU.mult)
                kb.tt("vector", mixo[:, 256:512], s_.y_, gssd, ALU.mult)

            m_ = G()
            m_.A = kb.ps(es, "mA", [128, 512], F32); m_.B = kb.ps(es, "mB", [128, 512], F32)
            m_.mqk = sb("mqk", [128, 4, 128], BF16); m_.kv = sb("kv_tm", [128, 512])
            m_.so = sb("so", [128, 256]); m_.li4 = sb("li4", [128, 4]); m_.eli8 = sb("eli8", [128, 4]); m_.f4 = sb("f4", [128, 4])
            m_.glog = sb("mglog", [128, 4]); m_.E = sb("mE", [128, 12]); m_.G_all = sb("mG_all", [128, 512]); m_.DT_all = sb("mDT_all", [128, 512])
            m_.vli = sb("vli", [128, 4, 66], BF16); m_.kdec = sb("mkdec", [128, 256], BF16); m_.STm = sb("STm", [128, 512], BF16)
            m_.intra = sb("intra", [128, 272])
            m_.g_rep = sb("mg_rep", [128, 256]); m_.eP = sb("meP", [128, 4])
            m_.CS32 = sb("CS32", [128, 2, 66]); m_.CSbf = sb("CSbf", [128, 2, 66], BF16)
            kb.memset("vector", m_.CS32, 0.0); kb.memset("vector", m_.CSbf, 0.0)
            m_.t2 = sb("t2", [128, 4, 65]); m_.d4 = sb("d4", [128, 4]); m_.hm = sb("hm", [128, 256]); m_.sq = sb("msq", [128, 256])
            m_.ss4 = sb("mss4", [128, 4]); m_.rs4 = sb("mrs4", [128, 4]); m_.tm4 = sb("mtm4", [128, 4])

            def mlstm_block(b, hT, mixo):
                A, B = m_.A, m_.B
                mqk, E, DT_all, glog, vli, t2, hm = m_.mqk, m_.E, m_.DT_all, m_.glog, m_.vli, m_.t2, m_.hm
                for i in range(2):
                    proj_fm(A[:, i * 128:(i + 1) * 128], 2828 + i * 128, hT)
                    proj_fm(A[:, 256 + i * 128:256 + (i + 1) * 128], 3084 + i * 128, hT)
                kb.cp("scalar", mqk.re("p a b -> p (a b)"), A)
                proj_tm(B, 3084, 512, hT)
                kb.cp("scalar", m_.kv, B)
                proj_tm(A[:, 0:264], 3596, 264, hT)
                kb.act(m_.so, A[:, 0:256], AF.Sigmoid)
                kb.tt("vector", m_.li4, A[:, 256:260], ibias, ALU.add)
                kb.act(m_.eli8, m_.li4, AF.Exp)
                kb.ts("vector", m_.eli8, m_.eli8, 0.125, None, ALU.mult)
                kb.tt("vector", m_.f4, A[:, 260:264], fbias, ALU.add)
                kb.act(m_.f4, m_.f4, AF.Exp, scale=-1.0)
                kb.act(m_.f4, m_.f4, AF.Ln, bias=1.0)
                kb.ts("vector", glog, m_.f4, -1.0, None, ALU.mult)
                decay_prep(glog, m_.G_all, A, B)
                kb.act(E, A[:, 0:12], AF.Exp)
                kb.act(DT_all, B, AF.Exp)
                kb.tt("gpsimd", vli[:, :, 0:64], v3(m_.kv[:, 256:512]), m_.eli8.bc(2, [128, 4, 64]), ALU.mult)
                kb.cp("vector", vli[:, :, 64:65], m_.eli8.bc(2, [128, 4, 1]))
                kb.tt("gpsimd", v3(m_.kdec), v3(m_.kv[:, 0:256]), E[:, 4:8].bc(2, [128, 4, 64]), ALU.mult)
                for h in range(4):
                    kb.mm(A[:, hs(h)], mqk[hp(h), 2 + h // 2, :], mqk[hp(h), h // 2, :])
                kb.tt("vector", m_.STm, A, DT_all, ALU.mult)
                for h in range(4):
                    kb.mm(B[:, h * 68:h * 68 + 65], m_.STm[:, hs(h)], vli[:, h, 0:65])
                kb.cp("scalar", m_.intra, B[:, 0:272])
                kb.cp("gpsimd", v3(m_.g_rep), glog.bc(2, [128, 4, 64]))
                for pair in range(2):
                    kb.mm(A[:, pair * 2:pair * 2 + 2], m_.g_rep[:, pair * 128:(pair + 1) * 128], CHI)
                kb.act(m_.eP, A[:, 0:4], AF.Exp)
                for j in range(2):
                    r = slice(j * 64, j * 64 + 64)
                    for h in range(4):
                        kb.mm(A[r, h * 68:h * 68 + 65], mqk[hp(h), h // 2, r], m_.CSbf[hp(h), h // 2, 0:65])
                    for h in range(4):
                        kb.mm(B[hp(h), (h // 2) * 68:(h // 2) * 68 + 65], m_.kdec[r, h * 64:(h + 1) * 64], vli[r, h, 0:65])
                    for pair in range(2):
                        kb.stt(m_.CS32[:, pair, 0:65], m_.CS32[:, pair, 0:65], m_.eP[:, pair * 2 + j:pair * 2 + j + 1],
                               B[:, pair * 68:pair * 68 + 65], ALU.mult, ALU.add)
                    kb.cp("scalar", m_.CSbf, m_.CS32)
                kb.tt("vector", t2, A[:, 0:272].re("p (h d) -> p h d", h=4)[:, :, 0:65], E[:, 0:4].bc(2, [128, 4, 65]), ALU.mult)
                kb.tt("vector", t2, t2, m_.intra.re("p (h d) -> p h d", h=4)[:, :, 0:65], ALU.add)
                kb.tt("vector", m_.d4, t2[:, :, 64], t2[:, :, 64], ALU.mult)
                kb.ts("vector", m_.d4, m_.d4, 1.0, None, ALU.max)
                kb.act(m_.d4, m_.d4, AF.Ln)
                kb.act(m_.d4, m_.d4, AF.Exp, scale=-0.5)
                kb.tt("vector", v3(hm), t2[:, :, 0:64], m_.d4.bc(2, [128, 4, 64]), ALU.mult)
                kb.tt("gpsimd", hm, hm, m_.so, ALU.mult)
                kb.tt("gpsimd", m_.sq, hm, hm, ALU.mult)
                kb.red(m_.ss4, v3(m_.sq))
                kb.rstd(m_.rs4, m_.ss4, 1.0 / 64, epsv, m_.tm4)
                kb.tt("vector", v3(hm), v3(hm), m_.rs4.bc(2, [128, 4, 64]), ALU.mult)
                kb.tt("vector", mixo[:, 512:768], hm, gml, ALU.mult)

            hTs.append(sb("hT3", [128, 8, 128], BF16))

            def pre_block(b):
                r0 = b * 128
                kb.dma("sync", xt, xs[r0:r0 + 128, :])
                prenorm_T(xt, g1b, xn, hTs[b % 3], ptp, ss, rs, t1s, junk)

            def rec_of(fn, *a):
                S.rec = []
                fn(*a)
                l_ = S.rec
                S.rec = None
                return l_

            def scan_mlstm(b, hT, mixo):
                gdn_scan(b, mixo)
                mlstm_block(b, hT, mixo)

            pre_block(0)
            if nblk > 1:
                pre_block(1)
            gdn_pre(0, hTs[0])
            for b in range(nblk):
                r0 = b * 128
                hT = hTs[b % 3]
                mixo = mixos[b % 2]
                lists = []
                if b + 1 < nblk:
                    lists.append(rec_of(gdn_pre, b + 1, hTs[(b + 1) % 3]))
                lists.append(rec_of(scan_mlstm, b, hT, mixo))
                lists.append(rec_of(ssd_block, b, hT, mixo))
                if b + 2 < nblk:
                    lists.append(rec_of(pre_block, b + 2))
                S.interleave(lists)
                kb.dma("sync", mixA[r0:r0 + 128, :], mixo)
            S.barrier()

    def mixb_phase(l, xs):
        lambda_init = 0.8 - 0.6 * math.exp(-0.3 * l)
        with ExitStack() as es:
            sb = lambda n, shp, dt=F32: kb.sb(es, n, shp, dt)
            g1b = bload(es, "g1b", wl("mix_pre_g", l), D)
            g2b = bload(es, "g2b", wl("mix_post_g", l), D)
            gdf = bload(es, "gdf", wl("diff_norm_g", l), 64, scale=(1.0 - lambda_init))
            winb = sb("winb", [128, 8, 768], BF16)
            wsrc = Wb[(l, "w_in")].re("(k p) n -> p k n", p=128)
            woutb = sb("woutb", [128, 8, D], BF16)
            wosrc = Wb[(l, "w_out")].re("(k p) n -> p k n", p=128)
            for k in range(8):
                kb.dma("sync", winb[:, k, :], wsrc[:, k, 1032:1800])
                kb.dma("sync", woutb[:, k, :], wosrc[:, k, :])
            lq1 = bload(es, "lq1", wl("diff_lam_q1", l), 32); lk1 = bload(es, "lk1", wl("diff_lam_k1", l), 32)
            lq2 = bload(es, "lq2", wl("diff_lam_q2", l), 32); lk2 = bload(es, "lk2", wl("diff_lam_k2", l), 32)
            lam2 = sb("lam2", [128, 2]); neglam = sb("neglam", [128, 1])
            kb.tt("vector", lq1, lq1, lk1, ALU.mult)
            kb.tt("vector", lq2, lq2, lk2, ALU.mult)
            kb.red(lam2[:, 0:1], lq1)
            kb.red(lam2[:, 1:2], lq2)
            kb.act(lam2, lam2, AF.Exp)
            kb.tt("vector", neglam, lam2[:, 1:2], lam2[:, 0:1], ALU.subtract)
            kb.ts("vector", neglam, neglam, -lambda_init, None, ALU.add)

            xt = sb("xt", [128, D]); xn = sb("xn", [128, D], BF16); junk = sb("junk", [128, D], BF16)
            tmp = sb("tmp", [128, D])
            hT = sb("hT", [128, 8, 128], BF16)
            ss = sb("ss", [128, 1]); rs = sb("rs", [128, 1]); t1s = sb("t1s", [128, 1]); q2 = sb("q2", [128, 2])
            KT = sb("KT", [64, 4, S_LEN], BF16)
            Vaug = sb("Vaug", [128, NB, 4, 66], BF16)
            kb.memset("vector", Vaug.re("p a b c -> p (a b c)"), 1.0)
            cosb = sb("cosb", [64, 128]); sinb = sb("sinb", [64, 128])
            qraw = sb("qraw", [64, 512], BF16)
            r1 = sb("r1", [64, 512]); r2 = sb("r2", [64, 512])
            qr = sb("qr", [64, 4, 128], BF16)
            PTt = [sb("PTt", [128, 512], BF16) for i in range(2)]
            rec8 = sb("rec8", [128, 8]); on = sb("on", [128, 8, 64]); od = sb("od", [128, 256]); sq = sb("sq", [128, 256])
            ss4 = sb("ss4", [128, 4]); rs4 = sb("rs4", [128, 4]); tm4 = sb("tm4", [128, 4])
            mT = sb("mT", [128, 8, 128], BF16)
            mixeds = [sb("mixed", [128, D], BF16) for i in range(3)]
            xts = [xt, sb("xt2", [128, D]), sb("xt3", [128, D])]
            qrs = [qr, sb("qr2", [64, 4, 128], BF16)]
            junk2 = sb("junk2", [128, D], BF16)
            rsb = sb("rsb", [128, 1]); t1b = sb("t1b", [128, 1])
            mdbg = sb("mdbg", [128, D]) if dbg else None
            pP = kb.ps(es, "pP", [128, 1024], BF16)
            pPf = V(pP.ap.bitcast(F32), pP.r)
            pq = kb.ps(es, "pq", [128, 512], F32)
            pS = [kb.ps(es, "pS", [128, 512], F32) for i in range(2)]
            pA = [kb.ps(es, "pA", [128, 512], F32) for i in range(2)]
            pY = [kb.ps(es, "pY", [128, 512], F32) for i in range(2)]
            pYb = V(pY[0].ap.bitcast(BF16), pY[0].r)
            v3 = lambda v, h=4: v.re("p (h d) -> p h d", h=h)
            sc = 32.0 ** -0.5
            nexp = [0]

            def stage_P(b):
                r0 = b * 128
                xtb = xts[b % 3]; mixed = mixeds[b % 3]; qrb = qrs[b % 2]
                kb.dma("sync", xtb, xs[r0:r0 + 128, :])
                kb.dma("sync", cosb, C["c_cos"][:, r0:r0 + 128])
                kb.dma("sync", sinb, C["c_sin"][:, r0:r0 + 128])
                kb.dma("sync", mixed[:, 0:256], mixA[r0:r0 + 128, 0:256])
                kb.dma("sync", mixed[:, 512:1024], mixA[r0:r0 + 128, 256:768])
                prenorm_T(xtb, g1b, xn, hT, pP, ss, rs, t1s, junk)
                for which in range(2):
                    for h in range(4):
                        for k in range(8):
                            kb.mm(pq[0:64, h * 128:(h + 1) * 128], winb[:, k, which * 256 + h * 64:which * 256 + (h + 1) * 64],
                                  hT[:, k, :], start=(k == 0), stop=(k == 7))
                    kb.cp("scalar", qraw, pq[0:64, :])
                    kb.mm(pPf[0:64, :], protb, qraw)
                    kb.tt("vector", v3(r1), v3(qraw), cosb.bc(1, [64, 4, 128]), ALU.mult)
                    kb.tt("vector", v3(r2), v3(pPf[0:64, :]), sinb.bc(1, [64, 4, 128]), ALU.mult)
                    dst = qrb if which == 0 else KT[:, :, r0:r0 + 128]
                    kb.tt("gpsimd", dst, v3(r1), v3(r2), ALU.add)
                for k in range(8):
                    kb.mm(pq[:, 0:256], hT[:, k, :], winb[:, k, 512:768], start=(k == 0), stop=(k == 7))
                kb.cp("scalar", Vaug[:, b, :, 0:64], v3(pq[:, 0:256]))

            def stage_A(b):
                mixed = mixeds[b % 3]; qrb = qrs[b % 2]
                kb.memset("vector", pA[0], 0.0)
                kb.memset("vector", pA[1], 0.0)
                for g0 in range(0, b + 1, 4):
                    kbs = list(range(g0, min(b + 1, g0 + 4)))
                    n = len(kbs)
                    for hmi in range(8):
                        h, mp = hmi // 2, hmi % 2
                        ps_ = pS[nexp[0] % 2]
                        pt_ = PTt[nexp[0] % 2]
                        nexp[0] += 1
                        for i, kbi in enumerate(kbs):
                            kb.mm(ps_[:, i * 128:(i + 1) * 128], KT[mp * 32:(mp + 1) * 32, h, kbi * 128:(kbi + 1) * 128],
                                  qrb[mp * 32:(mp + 1) * 32, h, :])
                        kb.act(pt_[:, 0:n * 128], ps_[:, 0:n * 128], AF.Exp, scale=sc)
                        if kbs[-1] == b:
                            i = n - 1
                            kb.memset("gpsimd", pt_[64:128, i * 128:i * 128 + 64], 0.0)
                        for i, kbi in enumerate(kbs):
                            kb.mm(pA[hmi // 4][:, (hmi % 4) * 68:(hmi % 4) * 68 + 65], pt_[:, i * 128:(i + 1) * 128],
                                  Vaug[:, kbi, h, 0:65], start=False, stop=(kbi == b), skip=True)
                for i in range(2):
                    a3 = pA[i][:, 0:272].re("p (h d) -> p h d", h=4)
                    kb.recip(rec8[:, i * 4:(i + 1) * 4], a3[:, :, 64])
                    kb.tt("vector", on[:, i * 4:(i + 1) * 4, :], a3[:, :, 0:64], rec8[:, i * 4:(i + 1) * 4].bc(2, [128, 4, 64]), ALU.mult)
                on4 = on.re("p (h m) d -> p h m d", m=2)
                kb.stt(v3(od), on4[:, :, 1, :], neglam[:, 0:1], on4[:, :, 0, :], ALU.mult, ALU.add)
                kb.tt("gpsimd", sq, od, od, ALU.mult)
                kb.red(ss4, v3(sq))
                kb.rstd(rs4, ss4, 1.0 / 64, epsv, tm4)
                kb.tt("vector", v3(od), v3(od), rs4.bc(2, [128, 4, 64]), ALU.mult)
                kb.tt("vector", v3(mixed[:, 256:512]), v3(od), gdf.bc(1, [128, 4, 64]), ALU.mult)

            def stage_O(b):
                r0 = b * 128
                xtb = xts[b % 3]; mixed = mixeds[b % 3]
                if dbg:
                    kb.cp("vector", mdbg, mixed)
                    kb.dma("sync", dbg_mixed[r0:r0 + 128, :], mdbg)
                for k in range(8):
                    kb.tp(pYb[:, k * 128:(k + 1) * 128], mixed[:, k * 128:(k + 1) * 128], identb)
                kb.cp("scalar", mT, pYb.re("p (k n) -> p k n", k=8))
                for c in range(2):
                    for k in range(8):
                        kb.mm(pY[c], mT[:, k, :], woutb[:, k, c * 512:(c + 1) * 512], start=(k == 0), stop=(k == 7))
                post_res(pY[0], pY[1], xtb, g2b, tmp, q2, rsb, t1b, junk2)
                kb.dma("sync", xs[r0:r0 + 128, :], xtb)

            def rec_of(fn, b):
                S.rec = []
                fn(b)
                l_ = S.rec
                S.rec = None
                return l_

            stage_P(0)
            for b in range(nblk):
                lists = [rec_of(stage_A, b)]
                if b + 1 < nblk:
                    lists.append(rec_of(stage_P, b + 1))
                if b > 0:
                    lists.append(rec_of(stage_O, b - 1))
                S.interleave(lists)
            stage_O(nblk - 1)
            S.barrier()

    if "ffn1" not in stages:
        for r0 in range(0, S_LEN, 512):
            kb.dma("sync", out[r0:r0 + 512, :], x_in[r0:r0 + 512, :])
    for l in range(nlayers):
        src = x_in if l == 0 else out
        if "ffn1" in stages:
            ffn_phase(l, 1, src, out)
        if "mixa" in stages:
            mixa_phase(l, out)
        if "mixb" in stages:
            mixb_phase(l, out)
        if "ffn2" in stages:
            ffn_phase(l, 2, out, out)
    S.wait_res("sync", [out.r] + ([dbg_mixed.r] if dbg else []))
    S.barrier(skip_queues=())
    done = es0.enter_context(nc.semaphore("s_done"))
    for e in ("tensor", "vector", "scalar", "sync"):
        S.eng[e].sem_inc(done, 1)
    S.eng["gpsimd"].wait_ge(done, 4)
    for e in S.ENG:
        S.eng["gpsimd"].sem_clear(S.sem[e])
    for q in S.dq:
        for sm in S.dq[q]["sems"]:
            S.eng["gpsimd"].sem_clear(sm)
    S.eng["gpsimd"].sem_clear(done)
    es0.close()
    return nc, hc


_CACHE = {}


def kernel(**inputs):
    x = np.ascontiguousarray(np.asarray(inputs["x"], dtype=np.float32))
    nb = x.shape[0]
    if "nc" not in _CACHE:
        _CACHE["nc"] = build()
    nc, hc = _CACHE["nc"]
    shared = {n: np.ascontiguousarray(np.asarray(inputs[n], dtype=np.float32)) for n, _ in W_SPECS}
    shared.update(hc)
    in_maps = []
    for b in range(nb):
        m = dict(shared)
        m["x"] = x[b]
        in_maps.append(m)
    res = run_bass_kernel_spmd(nc, in_maps, core_ids=list(range(nb)))
    return np.stack([np.asarray(r["out"], dtype=np.float32) for r in res.results], axis=0)
```
